# Optimizing a Trainium2 kernel written in Bass

```python
import math
import jax
import jax.numpy as jnp
from jax import lax
import numpy as np

D_MODEL = 2048
BATCH = 16
SEQ = 2048
DEPTH = 2

N_BRANCH = 4
BRANCH_WIDTH = D_MODEL // 4
S5_GROUP = 16
S5_GROUPS = BRANCH_WIDTH // S5_GROUP
S5_STATE = 64
S5_MIN_NEG = 1e-4
HG_HEADS = 4
HG_DIM = BRANCH_WIDTH // HG_HEADS
HG_CHUNK = 64
HG_F_MIN = 1e-6
SB_HEADS = 4
SB_DIM = BRANCH_WIDTH // SB_HEADS
SB_BLOCK = 128
POOL_WINDOWS = (2, 4, 8, 16)
POOL_GROUP = BRANCH_WIDTH // len(POOL_WINDOWS)
IN_SLICES = 9
IN_COLS = IN_SLICES * BRANCH_WIDTH
PEER_HEADS = 8
PEER_NKEYS = 128
PEER_EXPERTS = PEER_NKEYS * PEER_NKEYS
PEER_TOPK = 16
PEER_QDIM = 256
PEER_CHUNK = 128
ADA_CHUNKS = 6
EPS = 1e-6

kernel_name = 'hybrid_s5_hgrn2_stickbreak_pool_peer'


def _rmsnorm(x, gain):
    xf = x.astype(jnp.float32)
    y = xf * lax.rsqrt(jnp.mean(xf * xf, axis=-1, keepdims=True) + EPS)
    return (y * gain.astype(jnp.float32)).astype(x.dtype)


def _modulate(h, shift, scale):
    return h * (1.0 + scale[:, None, :]) + shift[:, None, :]


def _split_heads(t, n_heads, head_dim):
    bsz, seq, _ = t.shape
    return t.astype(jnp.float32).reshape(bsz, seq, n_heads, head_dim).transpose(0, 2, 1, 3)


def _complex_affine_combine(e1, e2):
    a1r, a1i, b1r, b1i = e1
    a2r, a2i, b2r, b2i = e2
    return (a2r * a1r - a2i * a1i,
            a2r * a1i + a2i * a1r,
            a2r * b1r - a2i * b1i + b2r,
            a2r * b1i + a2i * b1r + b2i)


def _s5_mixer(u, lam_re, lam_im, log_step, b_re, b_im, c_re, c_im, d_skip, w_glu, b_glu):
    f32 = jnp.float32
    bsz, seq, _ = u.shape
    uf = u.astype(f32).reshape(bsz, seq, S5_GROUPS, S5_GROUP)
    lr = jnp.minimum(lam_re.astype(f32), -S5_MIN_NEG)
    li = lam_im.astype(f32)
    step = jnp.exp(log_step.astype(f32))[:, None]
    mag = jnp.exp(lr * step)
    ab_re = mag * jnp.cos(li * step)
    ab_im = mag * jnp.sin(li * step)
    den = lr * lr + li * li
    nr = ab_re - 1.0
    fr = (nr * lr + ab_im * li) / den
    fi = (ab_im * lr - nr * li) / den
    br, bi = b_re.astype(f32), b_im.astype(f32)
    bb_re = fr[..., None] * br - fi[..., None] * bi
    bb_im = fr[..., None] * bi + fi[..., None] * br
    bu_re = jnp.einsum('gph,btgh->btgp', bb_re, uf)
    bu_im = jnp.einsum('gph,btgh->btgp', bb_im, uf)
    a_re = jnp.broadcast_to(ab_re, bu_re.shape)
    a_im = jnp.broadcast_to(ab_im, bu_im.shape)
    _, _, xs_re, xs_im = lax.associative_scan(
        _complex_affine_combine, (a_re, a_im, bu_re, bu_im), axis=1)
    y = (jnp.einsum('ghp,btgp->btgh', c_re.astype(f32), xs_re)
         - jnp.einsum('ghp,btgp->btgh', c_im.astype(f32), xs_im)
         + d_skip.astype(f32).reshape(S5_GROUPS, S5_GROUP) * uf)
    y = jax.nn.gelu(y.reshape(bsz, seq, BRANCH_WIDTH), approximate=False)
    out = y * jax.nn.sigmoid(y @ w_glu.astype(f32) + b_glu.astype(f32))
    return out.astype(u.dtype)


def _hgrn2_mixer(q, f, i, g, lower_bound, norm_gain):
    f32 = jnp.float32
    bsz, seq, _ = q.shape
    lb = lower_bound.astype(f32).reshape(HG_HEADS, 1, HG_DIM)
    qh = jax.nn.silu(_split_heads(q, HG_HEADS, HG_DIM))
    vh = _split_heads(i, HG_HEADS, HG_DIM)
    f_gate = lb + (1.0 - lb) * jax.nn.sigmoid(_split_heads(f, HG_HEADS, HG_DIM))
    log_f = jnp.log(jnp.maximum(f_gate, HG_F_MIN))
    kh = 1.0 - f_gate
    n_chunks = seq // HG_CHUNK

    def to_chunks(t):
        return t.reshape(bsz, HG_HEADS, n_chunks, HG_CHUNK, HG_DIM).transpose(2, 0, 1, 3, 4)

    causal = jnp.tril(jnp.ones((HG_CHUNK, HG_CHUNK), dtype=bool))[:, :, None]

    def chunk_step(state, inp):
        qc, kc, vc, gc = inp
        bcum = jnp.cumsum(gc, axis=2)
        o_inter = jnp.einsum('bhtd,bhde->bhte', qc * jnp.exp(bcum), state)
        diff = bcum[:, :, :, None, :] - bcum[:, :, None, :, :]
        decay = jnp.where(causal, jnp.exp(jnp.where(causal, diff, 0.0)), 0.0)
        scores = jnp.einsum('bhtd,bhsd,bhtsd->bhts', qc, kc, decay)
        o_intra = jnp.einsum('bhts,bhse->bhte', scores, vc)
        b_last = bcum[:, :, -1:, :]
        k_dec = kc * jnp.exp(b_last - bcum)
        new_state = (jnp.exp(b_last[:, :, 0, :, None]) * state
                     + jnp.einsum('bhsd,bhse->bhde', k_dec, vc))
        return new_state, o_inter + o_intra

    state0 = jnp.zeros((bsz, HG_HEADS, HG_DIM, HG_DIM), f32)
    _, o = lax.scan(chunk_step, state0,
                    (to_chunks(qh), to_chunks(kh), to_chunks(vh), to_chunks(log_f)))
    o = o.transpose(1, 0, 3, 2, 4).reshape(bsz, seq, HG_HEADS, HG_DIM)
    o = (o * lax.rsqrt(jnp.mean(o * o, axis=-1, keepdims=True) + EPS)
         * norm_gain.astype(f32).reshape(HG_HEADS, HG_DIM))
    o = o.reshape(bsz, seq, BRANCH_WIDTH) * jax.nn.silu(g.astype(f32))
    return o.astype(q.dtype)


def _stick_breaking_mixer(q, k, v):
    bsz, seq, _ = q.shape
    qh = _split_heads(q, SB_HEADS, SB_DIM)
    kh = _split_heads(k, SB_HEADS, SB_DIM)
    vh = _split_heads(v, SB_HEADS, SB_DIM)
    scale = 1.0 / math.sqrt(SB_DIM)
    outs = []
    for blk in range(seq // SB_BLOCK):
        start = blk * SB_BLOCK
        end = start + SB_BLOCK
        z = jnp.einsum('bhtd,bhsd->bhts', qh[:, :, start:end], kh[:, :, :end]) * scale
        t_idx = start + jnp.arange(SB_BLOCK)[:, None]
        s_idx = jnp.arange(end)[None, :]
        mask = s_idx < t_idx
        log_not = jnp.where(mask, jax.nn.log_sigmoid(-z), 0.0)
        suffix = jnp.flip(jnp.cumsum(jnp.flip(log_not, -1), axis=-1), -1) - log_not
        weight = jnp.where(mask, jnp.exp(jax.nn.log_sigmoid(z) + suffix), 0.0)
        outs.append(jnp.einsum('bhts,bhse->bhte', weight, vh[:, :, :end]))
    o = jnp.concatenate(outs, axis=2)
    return o.transpose(0, 2, 1, 3).reshape(bsz, seq, BRANCH_WIDTH).astype(q.dtype)


def _pool_mixer(p, pool_w, pool_scale):
    f32 = jnp.float32
    bsz, seq, _ = p.shape
    pf = p.astype(f32).reshape(bsz, seq, len(POOL_WINDOWS), POOL_GROUP)
    csum = jnp.cumsum(pf, axis=1)
    pooled = []
    for gi, w in enumerate(POOL_WINDOWS):
        cs = csum[:, :, gi]
        lagged = jnp.pad(cs, ((0, 0), (w, 0), (0, 0)))[:, :seq]
        count = jnp.minimum(jnp.arange(1, seq + 1), w).astype(f32)[None, :, None]
        pooled.append((cs - lagged) / count - pf[:, :, gi])
    pooled = jnp.stack(pooled, axis=2)
    mixed = jnp.einsum('btgc,gcd->btgd', pooled, pool_w.astype(f32))
    return (mixed.reshape(bsz, seq, BRANCH_WIDTH) * pool_scale.astype(f32)).astype(p.dtype)


def _hybrid_mixer(h, w_in, s5_lambda_re, s5_lambda_im, s5_log_step, s5_b_re, s5_b_im,
                  s5_c_re, s5_c_im, s5_d, s5_w_glu, s5_b_glu, hg_lower_bound, hg_norm_gain,
                  pool_w, pool_scale, w_gate, w_branch, w_out):
    proj = h @ w_in
    s5_u, hg_q, hg_f, hg_i, hg_g, sb_q, sb_k, sb_v, pool_in = jnp.split(proj, IN_SLICES, axis=-1)
    branches = (
        _s5_mixer(s5_u, s5_lambda_re, s5_lambda_im, s5_log_step, s5_b_re, s5_b_im,
                  s5_c_re, s5_c_im, s5_d, s5_w_glu, s5_b_glu),
        _hgrn2_mixer(hg_q, hg_f, hg_i, hg_g, hg_lower_bound, hg_norm_gain),
        _stick_breaking_mixer(sb_q, sb_k, sb_v),
        _pool_mixer(pool_in, pool_w, pool_scale),
    )
    terms = [jax.nn.sigmoid(h @ w_gate[n]) * (yb @ w_branch[n]) for n, yb in enumerate(branches)]
    merged = terms[0] + terms[1] + terms[2] + terms[3]
    return merged @ w_out


def _peer(h, w_query, sub_keys, expert_u, expert_v):
    f32 = jnp.float32
    bsz, seq, d = h.shape
    tokens = h.reshape(-1, PEER_CHUNK, d)
    keys = sub_keys.astype(f32)

    def per_block(xc):
        q = (xc @ w_query).astype(f32).reshape(PEER_CHUNK, PEER_HEADS, 2, PEER_QDIM // 2)
        scores = jnp.einsum('thpd,hpkd->thpk', q, keys)
        top_val, top_idx = lax.top_k(scores, PEER_TOPK)
        cand = top_val[:, :, 0, :, None] + top_val[:, :, 1, None, :]
        cand_idx = top_idx[:, :, 0, :, None] * PEER_NKEYS + top_idx[:, :, 1, None, :]
        cand = cand.reshape(PEER_CHUNK, PEER_HEADS, PEER_TOPK * PEER_TOPK)
        cand_idx = cand_idx.reshape(PEER_CHUNK, PEER_HEADS, PEER_TOPK * PEER_TOPK)
        best_val, best_pos = lax.top_k(cand, PEER_TOPK)
        expert_idx = jnp.take_along_axis(cand_idx, best_pos, axis=-1)
        gate = jax.nn.softmax(best_val, axis=-1)
        u = jnp.take(expert_u, expert_idx, axis=0)
        v = jnp.take(expert_v, expert_idx, axis=0)
        act = jax.nn.gelu(jnp.einsum('td,thkd->thk', xc, u), approximate=False)
        return jnp.einsum('thk,thkd->td', (gate * act).astype(v.dtype), v)

    out = lax.map(per_block, tokens)
    return out.reshape(bsz, seq, d).astype(h.dtype)


def setup_inputs(seed: int = 0) -> dict:
    key = jax.random.key(seed)
    ks = jax.random.split(key, 32)
    f32 = jnp.float32

    def nrm(k, shape, scale):
        return jax.random.normal(k, shape, f32) * scale

    L, G, P, H = DEPTH, S5_GROUPS, S5_STATE, S5_GROUP
    return {
        'x': nrm(ks[0], (BATCH, SEQ, D_MODEL), 1.0),
        'c': nrm(ks[1], (BATCH, D_MODEL), 1.0),
        'ada_w': nrm(ks[2], (L, D_MODEL, ADA_CHUNKS * D_MODEL), 0.5 * D_MODEL ** -0.5),
        'ada_b': nrm(ks[3], (L, ADA_CHUNKS * D_MODEL), 0.02),
        'norm_mix_gain': 1.0 + nrm(ks[4], (L, D_MODEL), 0.02),
        'norm_ffn_gain': 1.0 + nrm(ks[5], (L, D_MODEL), 0.02),
        'w_in': nrm(ks[6], (L, D_MODEL, IN_COLS), D_MODEL ** -0.5),
        's5_lambda_re': -0.5 + nrm(ks[7], (L, G, P), 0.01),
        's5_lambda_im': math.pi * jnp.arange(P, dtype=f32) + nrm(ks[8], (L, G, P), 0.01),
        's5_log_step': jax.random.uniform(ks[9], (L, G), f32, math.log(1e-3), math.log(1e-1)),
        's5_b_re': nrm(ks[10], (L, G, P, H), (2.0 * H) ** -0.5),
        's5_b_im': nrm(ks[11], (L, G, P, H), (2.0 * H) ** -0.5),
        's5_c_re': nrm(ks[12], (L, G, H, P), P ** -0.5),
        's5_c_im': nrm(ks[13], (L, G, H, P), P ** -0.5),
        's5_d': nrm(ks[14], (L, BRANCH_WIDTH), 1.0),
        's5_w_glu': nrm(ks[15], (L, BRANCH_WIDTH, BRANCH_WIDTH), BRANCH_WIDTH ** -0.5),
        's5_b_glu': nrm(ks[16], (L, BRANCH_WIDTH), 0.02),
        'hg_lb_logits': 1.0 + nrm(ks[17], (L, BRANCH_WIDTH), 0.1),
        'hg_norm_gain': 1.0 + nrm(ks[18], (L, BRANCH_WIDTH), 0.02),
        'pool_w': nrm(ks[19], (L, len(POOL_WINDOWS), POOL_GROUP, POOL_GROUP), POOL_GROUP ** -0.5),
        'pool_scale': 1.0 + nrm(ks[20], (L, BRANCH_WIDTH), 0.02),
        'w_gate': nrm(ks[21], (L, N_BRANCH, D_MODEL, D_MODEL), D_MODEL ** -0.5),
        'w_branch': nrm(ks[22], (L, N_BRANCH, BRANCH_WIDTH, D_MODEL), BRANCH_WIDTH ** -0.5),
        'w_out': nrm(ks[23], (L, D_MODEL, D_MODEL), D_MODEL ** -0.5),
        'peer_w_query': nrm(ks[24], (L, D_MODEL, PEER_HEADS * PEER_QDIM), D_MODEL ** -0.5),
        'peer_sub_keys': nrm(ks[25], (L, PEER_HEADS, 2, PEER_NKEYS, PEER_QDIM // 2), (PEER_QDIM // 2) ** -0.5),
        'peer_u': nrm(ks[26], (L, PEER_EXPERTS, D_MODEL), D_MODEL ** -0.5),
        'peer_v': nrm(ks[27], (L, PEER_EXPERTS, D_MODEL), 0.5),
        'final_gain': 1.0 + nrm(ks[28], (D_MODEL,), 0.02),
    }


def reference(x, c, ada_w, ada_b, norm_mix_gain, norm_ffn_gain, w_in,
              s5_lambda_re, s5_lambda_im, s5_log_step, s5_b_re, s5_b_im, s5_c_re, s5_c_im,
              s5_d, s5_w_glu, s5_b_glu, hg_lb_logits, hg_norm_gain, pool_w, pool_scale,
              w_gate, w_branch, w_out, peer_w_query, peer_sub_keys, peer_u, peer_v, final_gain):
    lb_soft = jax.nn.softmax(hg_lb_logits.astype(jnp.float32), axis=0)
    lower_bounds = jnp.cumsum(lb_soft, axis=0) - lb_soft[0:1]
    cond = jax.nn.silu(c)
    for l in range(DEPTH):
        mod = cond @ ada_w[l] + ada_b[l]
        sh1, sc1, g1, sh2, sc2, g2 = jnp.split(mod, ADA_CHUNKS, axis=-1)
        h = _modulate(_rmsnorm(x, norm_mix_gain[l]), sh1, sc1)
        y = _hybrid_mixer(h, w_in[l], s5_lambda_re[l], s5_lambda_im[l], s5_log_step[l],
                          s5_b_re[l], s5_b_im[l], s5_c_re[l], s5_c_im[l], s5_d[l],
                          s5_w_glu[l], s5_b_glu[l], lower_bounds[l], hg_norm_gain[l],
                          pool_w[l], pool_scale[l], w_gate[l], w_branch[l], w_out[l])
        x = x + g1[:, None, :] * y
        h = _modulate(_rmsnorm(x, norm_ffn_gain[l]), sh2, sc2)
        x = x + g2[:, None, :] * _peer(h, peer_w_query[l], peer_sub_keys[l], peer_u[l], peer_v[l])
    return _rmsnorm(x, final_gain)
```

```python
import numpy as np
from contextlib import ExitStack
import concourse.bass as bass
import concourse.mybir as mybir
from concourse.bass_utils import run_bass_kernel_spmd

F32 = mybir.dt.float32
BF16 = mybir.dt.bfloat16
AF = mybir.ActivationFunctionType
ALU = mybir.AluOpType

D = 2048
T = 2048
NB = 2
DEPTH = 2
EPS = 1e-6


class Res:
    __slots__ = ("w", "rs")

    def __init__(self):
        self.w = {}
        self.rs = {}


def mkres(n):
    return [Res() for _ in range(n)]


class KB:
    def __init__(self, nc):
        self.nc = nc
        self.eng = {"pe": nc.tensor, "act": nc.scalar, "dve": nc.vector, "pool": nc.gpsimd, "sp": nc.sync}
        self.sems = {}
        self.cnt = {}
        for e in self.eng:
            self.sems[e] = nc.alloc_semaphore(name=f"c_{e}")
            self.cnt[e] = 0
        self.waited = {e: {} for e in self.eng}
        self.dq = {}
        for q, n in (("sp", 12), ("pool", 2), ("act", 4)):
            lst = []
            for i in range(n):
                key = f"d_{q}{i}"
                self.sems[key] = nc.alloc_semaphore(name=key)
                lst.append([key, 0])
            self.dq[q] = [lst, 0]
        self.ninst = 0

    def _wait(self, e, deps):
        wd = self.waited[e]
        for key, val in deps.items():
            if key == e and e == "pe":
                continue
            if wd.get(key, 0) >= val:
                continue
            self.eng[e].wait_ge(self.sems[key], val)
            wd[key] = val
            self.ninst += 1

    @staticmethod
    def _deps(reads, writes):
        deps = {}
        for r in reads:
            for k, v in r.w.items():
                if deps.get(k, 0) < v:
                    deps[k] = v
        for w in writes:
            for k, v in w.w.items():
                if deps.get(k, 0) < v:
                    deps[k] = v
            for k, v in w.rs.items():
                if deps.get(k, 0) < v:
                    deps[k] = v
        return deps

    @staticmethod
    def _mark(ev, reads, writes):
        k, v = ev
        for r in reads:
            if r.rs.get(k, 0) < v:
                r.rs[k] = v
        for w in writes:
            w.w = {k: v}
            w.rs = {}

    def op(self, e, fn, reads=(), writes=()):
        self._wait(e, self._deps(reads, writes))
        self.cnt[e] += 1
        ins = fn(self.eng[e])
        ins.then_inc(self.sems[e], 1)
        self.ninst += 1
        self._mark((e, self.cnt[e]), reads, writes)

    def dma(self, q, out, in_, reads=(), writes=(), **kw):
        deps = self._deps(reads, writes)
        lst, idx = self.dq[q]
        slot = lst[idx]
        self.dq[q][1] = (idx + 1) % len(lst)
        if slot[1] > 0:
            deps[slot[0]] = max(deps.get(slot[0], 0), slot[1])
        self._wait(q, deps)
        slot[1] += 16
        self.eng[q].dma_start(out=out, in_=in_, **kw).then_inc(self.sems[slot[0]], 16)
        self.ninst += 1
        self._mark((slot[0], slot[1]), reads, writes)

    def barrier(self):
        deps = {}
        for e in self.eng:
            if self.cnt[e] > 0:
                deps[e] = self.cnt[e]
        for q in self.dq:
            for key, val in self.dq[q][0]:
                if val > 0:
                    deps[key] = val
        for e in self.eng:
            d = dict(deps)
            d.pop(e, None) if e != "pe" else None
            self._wait(e, d)

    def wait_all(self, e, ress):
        deps = {}
        for r in ress:
            for k, v in r.w.items():
                if deps.get(k, 0) < v:
                    deps[k] = v
        self._wait(e, deps)


def _pk(v):
    v = np.asarray(v)
    n = v.shape[-1] // 128
    return np.ascontiguousarray(np.swapaxes(v.reshape(v.shape[:-1] + (n, 128)), -1, -2))


def host_consts():
    c = {}
    c["ident_bf"] = np.eye(128, dtype=np.float32)
    c["ident_f32"] = np.eye(128, dtype=np.float32)
    inv = np.zeros((128, 16), np.float32)
    inv[:, :] = 1.0 / (np.arange(16, dtype=np.float32) + 1.0)
    c["invc"] = inv
    si = np.arange(128)[:, None]
    ti = np.arange(128)[None, :]
    c["mtri"] = (si < ti).astype(np.float32)
    c["triu"] = (si > ti).astype(np.float32)
    c["mle"] = (si <= ti).astype(np.float32)
    c["rmask"] = (np.arange(128)[:, None] // 32 == np.arange(4)[None, :]).astype(np.float32)
    c["ones128"] = np.ones((128, 128), np.float32)
    c["zeros128"] = np.zeros((128, 128), np.float32)
    return c


class Ctx:
    pass


class scope:
    def __init__(self, g):
        self.g = g
        self.es = ExitStack()

    def __enter__(self):
        self.es.__enter__()
        return self.es

    def __exit__(self, *a):
        if a[0] is None:
            self.g.kb.barrier()
        return self.es.__exit__(*a)


_UNIQ = [0]


def uniq(name):
    _UNIQ[0] += 1
    return f"{name}_u{_UNIQ[0]}"


def build(debug=None, nlayers=DEPTH):
    nc = bass.Bass("TRN2", target_bir_lowering=False)
    kb = KB(nc)
    g = Ctx()
    g.nc, g.kb, g.debug = nc, kb, debug

    def din(name, shape, dt=F32):
        return nc.dram_tensor(name, list(shape), dt, kind="ExternalInput").ap()

    I = {}
    I["x"] = din("x", [NB, T, D])
    I["cT"] = din("cT", [128, 16, NB])
    adaw = [din(f"ada_w{l}", [D, 6 * D]) for l in range(DEPTH)]
    I["ada_w"] = adaw
    I["ada_b"] = din("ada_b", [DEPTH, 1, 6 * D])
    I["gmix"] = din("gmix", [128, DEPTH, 16])
    I["gffn"] = din("gffn", [128, DEPTH, 16])
    I["gfin"] = din("gfin", [128, 16])
    I["w_in"] = din("w_in", [DEPTH, D, 4608])
    I["pool_w"] = din("pool_w", [DEPTH, 4, 128, 128])
    I["w_gate"] = din("w_gate", [DEPTH, 4, D, D])
    I["w_branch"] = din("w_branch", [DEPTH, 4, 512, D])
    I["w_out"] = din("w_out", [DEPTH, D, D])
    I["w_query"] = din("w_query", [DEPTH, D, D])
    I["keysT"] = din("keysT", [DEPTH, 128, 16, 128])
    I["peer_uT"] = [din(f"peer_uT{l}", [D, 16384]) for l in range(DEPTH)]
    I["peer_v"] = [din(f"peer_v{l}", [16384, D]) for l in range(DEPTH)]
    I["pool_scale"] = din("pool_scale", [128, DEPTH, 4])
    I["hg_gain"] = din("hg_gain", [128, DEPTH, 4])
    for nm in ("s5_lre_s", "s5_lim_s", "s5_lst_s"):
        I[nm] = din(nm, [128, DEPTH, 16])
    for nm in ("s5_lre_b", "s5_lim_b", "s5_lst_b", "s5_bT_re", "s5_bT_im"):
        I[nm] = din(nm, [DEPTH, 128, 512])
    for nm in ("s5_cT_re", "s5_cT_im"):
        I[nm] = din(nm, [DEPTH, 128, 16, 128])
    I["s5_d"] = din("s5_d", [128, DEPTH, 4])
    I["s5_bglu"] = din("s5_bglu", [128, DEPTH, 4])
    I["s5_wglu"] = din("s5_wglu", [DEPTH, 512, 512])
    I["hg_lbl"] = din("hg_lbl", [128, DEPTH, 4])
    I["ident_bf"] = din("ident_bf", [128, 128])
    I["ident_f32"] = din("ident_f32", [128, 128])
    I["invc"] = din("invc", [128, 16])
    I["rmask"] = din("rmask", [128, 4])
    for nm in ("mtri", "triu", "mle", "ones128", "zeros128"):
        I[nm] = din(nm, [128, 128])
    g.I = I

    out = nc.dram_tensor("out", [NB, T, D], F32, kind="ExternalOutput").ap()
    g.out = out
    dbg_kind = "ExternalOutput" if debug else "Internal"
    g.ybs = nc.dram_tensor("ybs", [NB, D, T], BF16, kind=dbg_kind).ap()
    g.ybs_res = [mkres(16) for _ in range(NB)]
    g.hTs = nc.dram_tensor("hTs", [NB, D, T], BF16, kind=dbg_kind).ap()
    g.hTs_res = [mkres(16) for _ in range(NB)]
    g.pj = nc.dram_tensor("pj", [NB, 4608, T], BF16, kind=dbg_kind).ap()
    g.pj_res = [mkres(36) for _ in range(NB)]
    g.pjf = nc.dram_tensor("pjf", [NB, 512, T], F32, kind=dbg_kind).ap()
    g.pjf_res = [mkres(4) for _ in range(NB)]
    g.pv = nc.dram_tensor("pv", [NB, 2, T, 512], BF16, kind=dbg_kind).ap()
    g.pv_res = [[mkres(16) for _ in range(2)] for _ in range(NB)]
    g.gst = nc.dram_tensor("gst", [16384, NB * T], BF16, kind=dbg_kind).ap()
    g.gst_res = [mkres(16) for _ in range(NB)]
    g.gas = nc.dram_tensor("gas", [16384, NB * T], BF16, kind=dbg_kind).ap()
    g.gas_res = [Res() for _ in range(NB * T // 1024)]
    g.xres = nc.dram_tensor("xres", [NB, T, D], F32, kind=dbg_kind).ap()
    g.xres_res = [mkres(16) for _ in range(NB)]

    with ExitStack() as es:
        def sb(name, shape, dt):
            return es.enter_context(nc.sbuf_tensor(uniq(name), list(shape), dt))

        g.ps = [es.enter_context(nc.psum_tensor(f"ps{i}", [128, 512], F32)) for i in range(8)]
        g.ps_res = mkres(8)
        g.ident = sb("ident", [128, 128], BF16)
        g.identf = sb("identf", [128, 128], F32)
        g.invc = sb("invc", [128, 16], F32)
        g.cres = Res()
        kb.dma("pool", g.ident[:], I["ident_bf"], writes=[g.cres])
        kb.dma("sp", g.identf[:], I["ident_f32"], writes=[g.cres])
        kb.dma("sp", g.invc[:], I["invc"], writes=[g.cres])
        g.mtri = sb("mtri", [128, 128], F32)
        g.mtri_bf = sb("mtri_bf", [128, 128], BF16)
        g.triu = sb("triu", [128, 128], F32)
        g.mle = sb("mle", [128, 128], F32)
        g.ones128 = sb("ones128", [128, 128], F32)
        g.ones_bf = sb("ones_bf", [128, 128], BF16)
        g.zeros_bf = sb("zeros_bf", [128, 128], BF16)
        kb.dma("sp", g.mtri[:], I["mtri"], writes=[g.cres])
        kb.dma("sp", g.triu[:], I["triu"], writes=[g.cres])
        kb.dma("sp", g.mle[:], I["mle"], writes=[g.cres])
        kb.dma("sp", g.ones128[:], I["ones128"], writes=[g.cres])
        kb.dma("pool", g.mtri_bf[:], I["mtri"], writes=[g.cres])
        kb.dma("pool", g.ones_bf[:], I["ones128"], writes=[g.cres])
        kb.dma("pool", g.zeros_bf[:], I["zeros128"], writes=[g.cres])
        g.rmask = sb("rmask", [128, 4], F32)
        kb.dma("sp", g.rmask[:], I["rmask"], writes=[g.cres])
        g.epsT = sb("epsT", [128, 1], F32)
        g.onesT = sb("onesT", [128, 2], F32)
        kb.op("dve", lambda e: e.memset(g.epsT[:], EPS), writes=[g.cres])
        kb.op("dve", lambda e: e.memset(g.onesT[:], 1.0), writes=[g.cres])
        g.modT = sb("modT", [128, DEPTH, 96, NB], F32)
        g.mod_res = Res()
        g.A1 = sb("A1", [128, DEPTH, 16, NB], F32)
        g.A2 = sb("A2", [128, DEPTH, 16, NB], F32)
        g.gmix = sb("gmix", [128, DEPTH, 16], F32)
        g.gffn = sb("gffn", [128, DEPTH, 16], F32)
        g.gfin = sb("gfin", [128, 16], F32)
        kb.dma("sp", g.gmix[:], I["gmix"], writes=[g.cres])
        kb.dma("sp", g.gffn[:], I["gffn"], writes=[g.cres])
        kb.dma("sp", g.gfin[:], I["gfin"], writes=[g.cres])

        g.branches = [branch_s5, branch_hg, branch_sb, branch_pool]
        g.do_merge = True
        g.do_mixer = debug != "peer"
        g.do_peer = debug in (None, "peer", "all")
        if debug and debug.startswith("br:"):
            names = debug[3:].split(",")
            g.branches = [globals()["branch_" + n] for n in names]
            g.do_merge = False
        phase_mod(g)
        if debug == "mod":
            finish_debug(g, es)
            return nc
        for l in range(nlayers):
            if g.do_mixer:
                for b in range(NB):
                    with scope(g) as es2:
                        mixer_part(g, es2, l, b)
            if g.do_peer:
                phase_peer(g, l)
        if debug is None:
            with scope(g) as esf:
                phase_final(g, esf)
        finish(g)
    return nc


def dump(g, tag, ap, res, dt=None):
    if not g.debug:
        return
    if not hasattr(g, "dumps"):
        g.dumps = {}
    if tag in g.dumps:
        return
    shp = list(ap.shape)
    d = g.nc.dram_tensor("dbg_" + tag, shp, dt or ap.dtype, kind="ExternalOutput").ap()
    r = Res()
    g.kb.dma("sp", d, ap, reads=res, writes=[r])
    g.dumps[tag] = r


def finish(g):
    kb = g.kb
    if hasattr(g, "dumps"):
        kb.wait_all("sp", list(g.dumps.values()))
    allres = []
    for lst in (g.ybs_res, g.hTs_res, g.xres_res, g.pj_res, g.pjf_res, g.pv_res[0], g.pv_res[1], g.gst_res):
        for r in lst:
            allres += r
    allres += g.gas_res
    if hasattr(g, "out_res"):
        allres += g.out_res
    kb.wait_all("sp", allres)


def finish_debug(g, es):
    kb, nc = g.kb, g.nc
    dbg = nc.dram_tensor("dbg_mod", [128, DEPTH * 96 * NB], F32, kind="ExternalOutput").ap()
    r = Res()
    kb.dma("sp", dbg, g.modT[:].rearrange("p l c b -> p (l c b)"), reads=[g.mod_res], writes=[r])
    kb.wait_all("sp", [r])


def phase_mod(g):
    nc, kb, I = g.nc, g.kb, g.I
    with scope(g) as es:
        def sb(name, shape, dt):
            return es.enter_context(nc.sbuf_tensor(uniq(name), list(shape), dt))
        condT = sb("condT", [128, 16, NB], F32)
        r_cond = Res()
        kb.dma("sp", condT[:], I["cT"], writes=[r_cond])
        kb.op("act", lambda e: e.activation(out=condT[:], in_=condT[:], func=AF.Silu), reads=[r_cond], writes=[r_cond])
        wb = [sb(f"adaw{i}", [128, 16, 512], F32) for i in range(2)]
        wb_res = mkres(2)
        ab = sb("adab", [1, DEPTH * 6 * D], F32)
        r_ab = Res()
        kb.dma("sp", ab[:], I["ada_b"].rearrange("l o c -> o (l c)"), writes=[r_ab])
        nblk = DEPTH * 24
        def load(i):
            l, blk = divmod(i, 24)
            src = I["ada_w"][l][:, blk * 512:(blk + 1) * 512].rearrange("(k p) c -> p k c", p=128)
            kb.dma("sp", wb[i % 2][:], src, writes=[wb_res[i % 2]])
        load(0)
        for i in range(nblk):
            if i + 1 < nblk:
                load(i + 1)
            l, blk = divmod(i, 24)
            w = wb[i % 2]
            bank = i % 2
            ps = g.ps[bank]
            for j in range(4):
                ct = blk * 4 + j
                for k in range(16):
                    kb.op("pe", lambda e, k=k, j=j: e.matmul(ps[:, 2 * j:2 * j + 2], w[:, k, j * 128:(j + 1) * 128], condT[:, k, :],
                                                         start=(k == 0), stop=False),
                          reads=[wb_res[i % 2], r_cond], writes=[g.ps_res[bank]])
                c0 = l * 6 * D + ct * 128
                kb.op("pe", lambda e, j=j, c0=c0: e.matmul(ps[:, 2 * j:2 * j + 2], ab[0:1, c0:c0 + 128], g.onesT[0:1, :],
                                                        start=False, stop=True),
                      reads=[r_ab, g.cres], writes=[g.ps_res[bank]])
            kb.op("act", lambda e: e.activation(out=g.modT[:, l, blk * 4:blk * 4 + 4, :],
                                                in_=ps[:, 0:8].rearrange("p (j b) -> p j b", b=NB), func=AF.Identity),
                  reads=[g.ps_res[bank]], writes=[g.mod_res])
        for l in range(DEPTH):
            kb.op("dve", lambda e, l=l: e.scalar_tensor_tensor(out=g.A1[:, l], in0=g.modT[:, l, 16:32, :], scalar=1.0,
                                                              in1=g.gmix[:, l, :].unsqueeze(2).to_broadcast([128, 16, NB]),
                                                              op0=ALU.add, op1=ALU.mult),
                  reads=[g.mod_res, g.cres], writes=[g.mod_res])
            kb.op("dve", lambda e, l=l: e.scalar_tensor_tensor(out=g.A2[:, l], in0=g.modT[:, l, 64:80, :], scalar=1.0,
                                                              in1=g.gffn[:, l, :].unsqueeze(2).to_broadcast([128, 16, NB]),
                                                              op0=ALU.add, op1=ALU.mult),
                  reads=[g.mod_res, g.cres], writes=[g.mod_res])


def norm_to_hT(g, es, xsrc, xsrc_res, hT, hT_res, Asc, Bsh):
    nc, kb = g.nc, g.kb
    def sb(name, shape, dt):
        return es.enter_context(nc.sbuf_tensor(uniq(name), list(shape), dt))
    xt = [sb(f"nx{i}", [128, D], F32) for i in range(2)]
    xt_res = mkres(2)
    xn = [sb(f"nxn{i}", [128, D], BF16) for i in range(2)]
    xn_res = mkres(2)
    junk = sb("njunk", [128, D], BF16)
    junk_res = Res()
    ss = sb("nss", [128, 4], F32)
    ss_res = mkres(2)
    def load(tt):
        kb.dma("sp", xt[tt % 2][:], xsrc[tt * 128:(tt + 1) * 128, :], reads=[xsrc_res[tt]] if xsrc_res else [], writes=[xt_res[tt % 2]])
    load(0)
    for tt in range(16):
        if tt + 1 < 16:
            load(tt + 1)
        i = tt % 2
        kb.op("act", lambda e: e.activation(out=junk[:], in_=xt[i][:], func=AF.Square, accum_out=ss[:, i:i + 1]),
              reads=[xt_res[i]], writes=[junk_res, ss_res[i]])
        kb.op("act", lambda e: e.activation(out=ss[:, 2 + i:3 + i], in_=ss[:, i:i + 1], func=AF.Sqrt, scale=1.0 / D, bias=g.epsT[:]),
              reads=[ss_res[i], g.cres], writes=[ss_res[i]])
        kb.op("dve", lambda e: e.reciprocal(out=ss[:, 2 + i:3 + i], in_=ss[:, 2 + i:3 + i]), reads=[ss_res[i]], writes=[ss_res[i]])
        kb.op("dve", lambda e: e.tensor_scalar(out=xn[i][:], in0=xt[i][:], scalar1=ss[:, 2 + i:3 + i], scalar2=None, op0=ALU.mult),
              reads=[xt_res[i], ss_res[i]], writes=[xn_res[i]])
        for half in range(2):
            bank = 6 + half
            psb = g.ps[bank][:].bitcast(BF16)
            for j in range(8):
                k = half * 8 + j
                kb.op("pe", lambda e, j=j, k=k: e.transpose(psb[:, j * 128:(j + 1) * 128], xn[i][:, k * 128:(k + 1) * 128], g.ident[:]),
                      reads=[xn_res[i], g.cres], writes=[g.ps_res[bank]])
            for j in range(8):
                k = half * 8 + j
                kb.op("act", lambda e, j=j, k=k: e.activation(out=hT[:, k, tt * 128:(tt + 1) * 128], in_=psb[:, j * 128:(j + 1) * 128],
                                                               func=AF.Identity, scale=Asc(k), bias=Bsh(k)),
                      reads=[g.ps_res[bank], g.mod_res], writes=[hT_res])


def load_wblock(g, q, dst, dst_res, src2d):
    g.kb.dma(q, dst, src2d.rearrange("(k p) c -> p k c", p=128), writes=[dst_res])


def proj_fm(g, wblk, wblk_res, hT, hT_res, ncol_tiles, evac):
    kb = g.kb
    n = 0
    for j in range(ncol_tiles):
        for tc in range(4):
            bank = n % 4
            n += 1
            ps = g.ps[bank]
            for k in range(16):
                kb.op("pe", lambda e, k=k: e.matmul(ps[:], wblk[:, k, j * 128:(j + 1) * 128], hT[:, k, tc * 512:(tc + 1) * 512],
                                                  start=(k == 0), stop=(k == 15)),
                      reads=[wblk_res, hT_res], writes=[g.ps_res[bank]])
            evac(j, tc, ps, g.ps_res[bank])


def mixer_part(g, es, l, b):
    nc, kb, I = g.nc, g.kb, g.I
    with scope(g) as es1:
        phase_proj(g, es1, l, b)
    for br in g.branches:
        with scope(g) as es2:
            br(g, es2, l, b)
    if g.do_merge:
        with scope(g) as es3:
            phase_merge(g, es3, l, b)


def phase_proj(g, es, l, b):
    nc, kb, I = g.nc, g.kb, g.I
    def sb(name, shape, dt):
        return es.enter_context(nc.sbuf_tensor(uniq(name), list(shape), dt))
    hT = sb("hT", [128, 16, T], BF16)
    hT_res = Res()
    if l == 0:
        xsrc, xsrc_res = I["x"][b], None
    else:
        xsrc, xsrc_res = g.xres[b], g.xres_res[b]
    with scope(g) as es2:
        norm_to_hT(g, es2, xsrc, xsrc_res, hT, hT_res,
                   lambda k: g.A1[:, l, k, b:b + 1], lambda k: g.modT[:, l, k, b:b + 1])
    for k in range(16):
        kb.dma("sp", g.hTs[b, k * 128:(k + 1) * 128, :], hT[:, k, :], reads=[hT_res], writes=[g.hTs_res[b][k]])
    wblk = [sb(f"wblk{i}", [128, 16, 512], BF16) for i in range(2)]
    wblk_res = mkres(2)
    stg = [sb(f"pstg{i}", [128, T], BF16) for i in range(2)]
    stg_res = mkres(2)
    stgf = [sb(f"pstgf{i}", [128, T], F32) for i in range(2)]
    stgf_res = mkres(2)
    stgv = [sb(f"pstgv{i}", [128, 512], BF16) for i in range(2)]
    stgv_res = mkres(2)
    def wload(cb):
        load_wblock(g, "pool", wblk[cb % 2][:], wblk_res[cb % 2], I["w_in"][l, :, cb * 512:(cb + 1) * 512])
    wload(0)
    nst = 0
    for cb in range(9):
        if cb + 1 < 9:
            wload(cb + 1)
        w, w_res = wblk[cb % 2], wblk_res[cb % 2]
        if cb in (3, 7):
            which = 0 if cb == 3 else 1
            for tt in range(16):
                bank = tt % 4
                ps = g.ps[bank]
                for k in range(16):
                    kb.op("pe", lambda e, k=k: e.matmul(ps[:], hT[:, k, tt * 128:(tt + 1) * 128], w[:, k, :], start=(k == 0), stop=(k == 15)),
                          reads=[w_res, hT_res], writes=[g.ps_res[bank]])
                sv, sv_res = stgv[tt % 2], stgv_res[tt % 2]
                kb.op("act", lambda e: e.activation(out=sv[:], in_=ps[:], func=AF.Identity), reads=[g.ps_res[bank]], writes=[sv_res])
                kb.dma("sp", g.pv[b, which, tt * 128:(tt + 1) * 128, :], sv[:], reads=[sv_res], writes=[g.pv_res[b][which][tt]])
            continue
        for j in range(4):
            if cb == 2:
                st, st_res = stgf[j % 2], stgf_res[j % 2]
            else:
                st, st_res = stg[nst % 2], stg_res[nst % 2]
                nst += 1
            for tc in range(4):
                bank = (j * 4 + tc) % 4
                ps = g.ps[bank]
                for k in range(16):
                    kb.op("pe", lambda e, k=k: e.matmul(ps[:], w[:, k, j * 128:(j + 1) * 128], hT[:, k, tc * 512:(tc + 1) * 512],
                                                      start=(k == 0), stop=(k == 15)),
                          reads=[w_res, hT_res], writes=[g.ps_res[bank]])
                kb.op("act", lambda e: e.activation(out=st[:, tc * 512:(tc + 1) * 512], in_=ps[:], func=AF.Identity),
                      reads=[g.ps_res[bank]], writes=[st_res])
            if cb == 2:
                kb.dma("sp", g.pjf[b, j * 128:(j + 1) * 128, :], st[:], reads=[st_res], writes=[g.pjf_res[b][j]])
            else:
                ct = cb * 4 + j
                kb.dma("sp", g.pj[b, ct * 128:(ct + 1) * 128, :], st[:], reads=[st_res], writes=[g.pj_res[b][ct]])


def phase_final(g, es):
    nc, kb, I = g.nc, g.kb, g.I
    def sb(name, shape, dt):
        return es.enter_context(nc.sbuf_tensor(uniq(name), list(shape), dt))
    gbc = sb("fn_g", [128, D], F32)
    gbc_res = Res()
    build_rowbcast(g, sb, gbc, gbc_res, lambda k: g.gfin[:, k:k + 1])
    xt = [sb(f"fn_x{i}", [128, D], F32) for i in range(2)]
    xt_res = mkres(2)
    yt = [sb(f"fn_y{i}", [128, D], F32) for i in range(2)]
    yt_res = mkres(2)
    junk = sb("fn_junk", [128, D], BF16)
    junk_res = Res()
    ss = sb("fn_ss", [128, 4], F32)
    ss_res = mkres(2)
    g.out_res = mkres(NB * 16)
    n = 0
    for b in range(NB):
        for tt in range(16):
            i = n % 2
            n += 1
            rows = slice(tt * 128, (tt + 1) * 128)
            kb.dma("sp", xt[i][:], g.xres[b, rows, :], reads=[g.xres_res[b][tt]], writes=[xt_res[i]])
            kb.op("act", lambda e: e.activation(out=junk[:], in_=xt[i][:], func=AF.Square, accum_out=ss[:, i:i + 1]),
                  reads=[xt_res[i]], writes=[junk_res, ss_res[i]])
            kb.op("act", lambda e: e.activation(out=ss[:, 2 + i:3 + i], in_=ss[:, i:i + 1], func=AF.Sqrt, scale=1.0 / D, bias=g.epsT[:]),
                  reads=[ss_res[i], g.cres], writes=[ss_res[i]])
            kb.op("dve", lambda e: e.reciprocal(out=ss[:, 2 + i:3 + i], in_=ss[:, 2 + i:3 + i]), reads=[ss_res[i]], writes=[ss_res[i]])
            kb.op("dve", lambda e: e.scalar_tensor_tensor(out=yt[i][:], in0=xt[i][:], scalar=ss[:, 2 + i:3 + i], in1=gbc[:],
                                                          op0=ALU.mult, op1=ALU.mult),
                  reads=[xt_res[i], ss_res[i], gbc_res], writes=[yt_res[i]])
            kb.dma("sp", g.out[b, rows, :], yt[i][:], reads=[yt_res[i]], writes=[g.out_res[b * 16 + tt]])


def build_rowbcast(g, sb, dst, dst_res, colfn):
    kb = g.kb
    dg = [sb(f"rb_dg{i}", [128, 128], F32) for i in range(2)]
    dg_res = mkres(2)
    for k in range(16):
        i = k % 2
        kb.op("dve", lambda e: e.tensor_scalar(out=dg[i][:], in0=g.identf[:], scalar1=colfn(k), scalar2=None, op0=ALU.mult),
              reads=[g.cres, g.mod_res], writes=[dg_res[i]])
        kk = k % 4
        kb.op("pe", lambda e: e.matmul(g.ps[6][:, kk * 128:(kk + 1) * 128], g.ones128[:], dg[i][:], start=True, stop=True),
              reads=[g.cres, dg_res[i]], writes=[g.ps_res[6]])
        if kk == 3:
            k0 = k - 3
            kb.op("act", lambda e: e.activation(out=dst[:, k0 * 128:(k0 + 4) * 128], in_=g.ps[6][:], func=AF.Identity),
                  reads=[g.ps_res[6]], writes=[dst_res])


def phase_merge(g, es, l, b):
    nc, kb, I = g.nc, g.kb, g.I
    def sb(name, shape, dt):
        return es.enter_context(nc.sbuf_tensor(uniq(name), list(shape), dt))
    TCH = 1024
    NTC = TCH // 512
    g1bc = sb("mg_g1", [128, D], F32)
    g1_res = Res()
    build_rowbcast(g, sb, g1bc, g1_res, lambda k: g.modT[:, l, 32 + k, b:b + 1])
    hTc = sb("mg_hT", [128, 16, TCH], BF16)
    ybc = sb("mg_yb", [128, 16, TCH], BF16)
    mrg = sb("mg_mrg", [128, 16, TCH], BF16)
    acc = sb("mg_acc", [128, 4, TCH], F32)
    hTc_res, ybc_res = Res(), Res()
    mrg_res = mkres(16)
    acc_res = mkres(4)
    wg = [sb(f"mg_wg{i}", [128, 16, 512], BF16) for i in range(2)]
    wg_res = mkres(2)
    wbr = [sb(f"mg_wb{i}", [128, 4, 512], BF16) for i in range(2)]
    wbr_res = mkres(2)
    sgm = [sb(f"mg_sg{i}", [128, 512], F32) for i in range(2)]
    sgm_res = mkres(2)
    tmp = [sb(f"mg_tmp{i}", [128, 512], F32) for i in range(2)]
    tmp_res = mkres(2)
    xt = [sb(f"mg_xt{i}", [128, 512], F32) for i in range(2)]
    xt_res = mkres(2)
    xsrc = I["x"][b] if l == 0 else g.xres[b]
    for ch in range(T // TCH):
        tsl = slice(ch * TCH, (ch + 1) * TCH)
        kb.dma("sp", hTc[:], g.hTs[b, :, tsl].rearrange("(k p) t -> p k t", p=128), reads=g.hTs_res[b], writes=[hTc_res])
        kb.dma("sp", ybc[:], g.ybs[b, :, tsl].rearrange("(k p) t -> p k t", p=128), reads=g.ybs_res[b], writes=[ybc_res])
        nw = 0
        def wload(cb, n, i):
            kb.dma("pool", wg[i][:], I["w_gate"][l, n, :, cb * 512:(cb + 1) * 512].rearrange("(k p) c -> p k c", p=128), writes=[wg_res[i]])
            kb.dma("pool", wbr[i][:], I["w_branch"][l, n, :, cb * 512:(cb + 1) * 512].rearrange("(k p) c -> p k c", p=128), writes=[wbr_res[i]])
        seq = [(cb, n) for cb in range(4) for n in range(4)]
        wload(seq[0][0], seq[0][1], 0)
        cnt = 0
        for si, (cb, n) in enumerate(seq):
            wi = si % 2
            if si + 1 < len(seq):
                wload(seq[si + 1][0], seq[si + 1][1], (si + 1) % 2)
            for j in range(4):
                dt_ = cb * 4 + j
                for tcc in range(NTC):
                    cs = slice(tcc * 512, (tcc + 1) * 512)
                    i = cnt % 2
                    cnt += 1
                    psg, psb = g.ps[i], g.ps[2 + i]
                    for k in range(16):
                        kb.op("pe", lambda e, k=k: e.matmul(psg[:], wg[wi][:, k, j * 128:(j + 1) * 128], hTc[:, k, cs], start=(k == 0), stop=(k == 15)),
                              reads=[wg_res[wi], hTc_res], writes=[g.ps_res[i]])
                    for kk in range(4):
                        kb.op("pe", lambda e, kk=kk: e.matmul(psb[:], wbr[wi][:, kk, j * 128:(j + 1) * 128], ybc[:, 4 * n + kk, cs],
                                                            start=(kk == 0), stop=(kk == 3)),
                              reads=[wbr_res[wi], ybc_res], writes=[g.ps_res[2 + i]])
                    kb.op("act", lambda e: e.activation(out=sgm[i][:], in_=psg[:], func=AF.Sigmoid), reads=[g.ps_res[i]], writes=[sgm_res[i]])
                    if n == 0:
                        kb.op("dve", lambda e: e.tensor_tensor(out=acc[:, j, cs], in0=sgm[i][:], in1=psb[:], op=ALU.mult),
                              reads=[sgm_res[i], g.ps_res[2 + i]], writes=[acc_res[j]])
                    else:
                        kb.op("dve", lambda e: e.tensor_tensor(out=tmp[i][:], in0=sgm[i][:], in1=psb[:], op=ALU.mult),
                              reads=[sgm_res[i], g.ps_res[2 + i]], writes=[tmp_res[i]])
                        if n < 3:
                            kb.op("pool", lambda e: e.tensor_tensor(out=acc[:, j, cs], in0=acc[:, j, cs], in1=tmp[i][:], op=ALU.add),
                                  reads=[tmp_res[i], acc_res[j]], writes=[acc_res[j]])
                        else:
                            kb.op("pool", lambda e: e.tensor_tensor(out=mrg[:, dt_, cs], in0=acc[:, j, cs], in1=tmp[i][:], op=ALU.add),
                                  reads=[tmp_res[i], acc_res[j]], writes=[mrg_res[dt_]])
        if ch == 0 and b == 0:
            dump(g, "mg_mrg", mrg[:], mrg_res)
        def woload(dc, i):
            kb.dma("pool", wg[i][:], I["w_out"][l, :, dc * 512:(dc + 1) * 512].rearrange("(k p) c -> p k c", p=128), writes=[wg_res[i]])
        woload(0, 0)
        cnt = 0
        for dc in range(4):
            if dc + 1 < 4:
                woload(dc + 1, (dc + 1) % 2)
            wi = dc % 2
            dsl = slice(dc * 512, (dc + 1) * 512)
            for tt in range(TCH // 128):
                i = cnt % 2
                cnt += 1
                tg = ch * (TCH // 128) + tt
                rows = slice(tg * 128, (tg + 1) * 128)
                kb.dma("sp", xt[i][:], xsrc[rows, dsl], reads=([g.xres_res[b][tg]] if l > 0 else []), writes=[xt_res[i]])
                ps = g.ps[4 + i]
                for k in range(16):
                    kb.op("pe", lambda e, k=k: e.matmul(ps[:], mrg[:, k, tt * 128:(tt + 1) * 128], wg[wi][:, k, :], start=(k == 0), stop=(k == 15)),
                          reads=[mrg_res[k], wg_res[wi]], writes=[g.ps_res[4 + i]])
                kb.op("dve", lambda e: e.tensor_tensor(out=tmp[i][:], in0=ps[:], in1=g1bc[:, dsl], op=ALU.mult),
                      reads=[g.ps_res[4 + i], g1_res], writes=[tmp_res[i]])
                kb.op("dve", lambda e: e.tensor_tensor(out=xt[i][:], in0=xt[i][:], in1=tmp[i][:], op=ALU.add),
                      reads=[tmp_res[i], xt_res[i]], writes=[xt_res[i]])
                kb.dma("sp", g.xres[b, rows, dsl], xt[i][:], reads=[xt_res[i]], writes=[g.xres_res[b][tg]])


def phase_peer(g, l):
    for b in range(NB):
        with scope(g) as es:
            peer_route(g, es, l, b)
    for tg in range(NB * T // 1024):
        with scope(g) as es:
            peer_x(g, es, l, tg)
    with scope(g) as es:
        peer_y(g, es, l)


def peer_route(g, es, l, b):
    nc, kb, I = g.nc, g.kb, g.I
    def sb(name, shape, dt):
        return es.enter_context(nc.sbuf_tensor(uniq(name), list(shape), dt))
    NEG = -1e30
    qT = sb("pr_qT", [128, 16, T], BF16)
    qT_res = mkres(16)
    xsrc, xsrc_res = g.xres[b], g.xres_res[b]
    if g.debug == "peer":
        xsrc, xsrc_res = I["x"][b], None
    with scope(g) as es1:
        def sb1(name, shape, dt):
            return es1.enter_context(nc.sbuf_tensor(uniq(name), list(shape), dt))
        hT = sb1("pr_hT", [128, 16, T], BF16)
        hT_res = Res()
        with scope(g) as es2:
            norm_to_hT(g, es2, xsrc, xsrc_res, hT, hT_res,
                       lambda k: g.A2[:, l, k, b:b + 1], lambda k: g.modT[:, l, 48 + k, b:b + 1])
        for k in range(16):
            kb.dma("sp", g.hTs[b, k * 128:(k + 1) * 128, :], hT[:, k, :], reads=[hT_res], writes=[g.hTs_res[b][k]])
        wblk = [sb1(f"pr_w{i}", [128, 16, 512], BF16) for i in range(2)]
        wblk_res = mkres(2)
        def wload(cb):
            load_wblock(g, "pool", wblk[cb % 2][:], wblk_res[cb % 2], I["w_query"][l, :, cb * 512:(cb + 1) * 512])
        wload(0)
        for cb in range(4):
            if cb + 1 < 4:
                wload(cb + 1)
            w, w_res = wblk[cb % 2], wblk_res[cb % 2]
            for j in range(4):
                ct = cb * 4 + j
                for tc in range(4):
                    bank = (j * 4 + tc) % 4
                    ps = g.ps[bank]
                    for k in range(16):
                        kb.op("pe", lambda e, k=k: e.matmul(ps[:], w[:, k, j * 128:(j + 1) * 128], hT[:, k, tc * 512:(tc + 1) * 512],
                                                          start=(k == 0), stop=(k == 15)),
                              reads=[w_res, hT_res], writes=[g.ps_res[bank]])
                    kb.op("act", lambda e: e.activation(out=qT[:, ct, tc * 512:(tc + 1) * 512], in_=ps[:], func=AF.Identity),
                          reads=[g.ps_res[bank]], writes=[qT_res[ct]])
    keysT = sb("pr_keys", [128, 16, 128], BF16)
    keys_res = Res()
    kb.dma("pool", keysT[:], I["keysT"][l], writes=[keys_res])
    sc = sb("pr_sc", [128, 16, 128], F32)
    sc2 = sb("pr_sc2", [128, 16, 128], F32)
    top = sb("pr_top", [128, 16, 16], F32)
    cand = sb("pr_cand", [128, 8, 256], F32)
    cand2 = sb("pr_cand2", [128, 8, 256], F32)
    best = sb("pr_best", [128, 8, 24], F32)
    sm = sb("pr_sm", [128, 8, 8], F32)
    ex = sb("pr_ex", [128, 8, 16], F32)
    r_sc, r_sc2, r_e, r_top, r_cand, r_cand2, r_best, r_sm, r_ex = mkres(9)
    NPB = 4
    P = [sb(f"pr_P{i}", [128, 8, 128], F32) for i in range(NPB)]
    P_res = mkres(NPB)
    Gh = [sb(f"pr_G{i}", [128, 1024], BF16) for i in range(NPB)]
    Gh_res = mkres(NPB)
    GT = [sb(f"pr_GT{i}", [128, 8, 128], BF16) for i in range(2)]
    GT_res = mkres(2)
    sc4 = sc[:].rearrange("p (h two) k -> p h two k", two=2)
    E = [sb(f"pr_E{i}", [128, 1024], BF16) for i in range(4)]
    E_res = mkres(4)
    top4 = top[:].rearrange("p (h two) k -> p h two k", two=2)
    for tt in range(16):
        tsl = slice(tt * 128, (tt + 1) * 128)
        for q4 in range(4):
            bank = q4 % 2
            ps = g.ps[bank]
            for jj in range(4):
                hp = q4 * 4 + jj
                kb.op("pe", lambda e, hp=hp, jj=jj: e.matmul(ps[:, jj * 128:(jj + 1) * 128], qT[:, hp, tsl], keysT[:, hp, :], start=True, stop=True),
                      reads=[qT_res[hp], keys_res], writes=[g.ps_res[bank]])
            kb.op("act", lambda e: e.activation(out=sc[:, q4 * 4:(q4 + 1) * 4, :], in_=ps[:].rearrange("p (a k) -> p a k", k=128), func=AF.Identity),
                  reads=[g.ps_res[bank]], writes=[r_sc])
        for hp in range(16):
            kb.op("dve", lambda e, hp=hp: e.max(out=top[:, hp, 0:8], in_=sc[:, hp, :]), reads=[r_sc], writes=[r_top])
            kb.op("dve", lambda e, hp=hp: e.match_replace(out=sc2[:, hp, :], in_to_replace=top[:, hp, 0:8], in_values=sc[:, hp, :], imm_value=NEG),
                  reads=[r_sc, r_top], writes=[r_sc2])
            kb.op("dve", lambda e, hp=hp: e.max(out=top[:, hp, 8:16], in_=sc2[:, hp, :]), reads=[r_sc2], writes=[r_top])
        kb.op("dve", lambda e: e.tensor_tensor(out=cand[:].rearrange("p h (i j) -> p h i j", j=16),
                                               in0=top4[:, :, 0, :].unsqueeze(3).to_broadcast([128, 8, 16, 16]),
                                               in1=top4[:, :, 1, :].unsqueeze(2).to_broadcast([128, 8, 16, 16]), op=ALU.add),
              reads=[r_top], writes=[r_cand])
        for h in range(8):
            kb.op("dve", lambda e, h=h: e.max(out=best[:, h, 0:8], in_=cand[:, h, :]), reads=[r_cand], writes=[r_best])
            kb.op("dve", lambda e, h=h: e.match_replace(out=cand2[:, h, :], in_to_replace=best[:, h, 0:8], in_values=cand[:, h, :], imm_value=NEG),
                  reads=[r_cand, r_best], writes=[r_cand2])
            kb.op("dve", lambda e, h=h: e.max(out=best[:, h, 8:16], in_=cand2[:, h, :]), reads=[r_cand2], writes=[r_best])
        kb.op("dve", lambda e: e.tensor_tensor(out=ex[:], in0=best[:, :, 0:16], in1=best[:, :, 0:1].to_broadcast([128, 8, 16]), op=ALU.subtract),
              reads=[r_best], writes=[r_ex])
        kb.op("act", lambda e: e.activation(out=ex[:], in_=ex[:], func=AF.Exp), reads=[r_ex], writes=[r_ex])
        kb.op("dve", lambda e: e.tensor_reduce(out=sm[:, :, 0], in_=ex[:], axis=mybir.AxisListType.X, op=ALU.add), reads=[r_ex], writes=[r_sm])
        kb.op("act", lambda e: e.activation(out=sm[:, :, 0], in_=sm[:, :, 0], func=AF.Ln), reads=[r_sm], writes=[r_sm])
        kb.op("dve", lambda e: e.scalar_tensor_tensor(out=sm[:, :, 2], in0=sm[:, :, 0], scalar=-1.0, in1=best[:, :, 0], op0=ALU.mult, op1=ALU.subtract),
              reads=[r_best, r_sm], writes=[r_sm])
        kb.op("dve", lambda e: e.tensor_copy(out=sm[:, :, 1], in_=best[:, :, 15]), reads=[r_best, r_sm], writes=[r_sm])
        if tt == 0 and b == 0:
            dump(g, "pr_sc", sc[:], [r_sc]); dump(g, "pr_best", best[:], [r_best]); dump(g, "pr_sm", sm[:], [r_sm])
        n = 0
        pending = []
        for ib in range(16):
            gi = ib % 2
            banks = (2 + 2 * gi, 3 + 2 * gi)
            for bk in banks:
                kb.op("pe", lambda e, bk=bk: e.matmul(g.ps[bk][:], g.zeros_bf[:], qT[:, 0, 0:512], start=True, stop=True),
                      reads=[g.cres, qT_res[0]], writes=[g.ps_res[bk]])
            for h in range(8):
                i = n % NPB
                n += 1
                kb.op("pool", lambda e: e.tensor_tensor(out=P[i][:], in0=sc4[:, h, 0, ib * 8:(ib + 1) * 8].unsqueeze(2).to_broadcast([128, 8, 128]),
                                                        in1=sc4[:, h, 1, :].unsqueeze(1).to_broadcast([128, 8, 128]), op=ALU.add),
                      reads=[r_sc], writes=[P_res[i]])
                P2 = P[i][:].rearrange("p a k -> p (a k)")
                kb.op("act", lambda e: e.activation(out=E[i][:], in_=P2, func=AF.Exp, bias=sm[:, h, 2:3]), reads=[P_res[i], r_sm], writes=[E_res[i]])
                kb.op("dve", lambda e: e.scalar_tensor_tensor(out=Gh[i][:], in0=P2, scalar=sm[:, h, 1:2], in1=E[i][:], op0=ALU.is_ge, op1=ALU.mult),
                      reads=[P_res[i], E_res[i], r_sm], writes=[Gh_res[i]])
                for ii in range(8):
                    bk = banks[ii // 4]
                    kb.op("pe", lambda e, ii=ii, bk=bk: e.matmul(g.ps[bk][:, (ii % 4) * 128:(ii % 4 + 1) * 128], Gh[i][:, ii * 128:(ii + 1) * 128],
                                                               g.ident[:], start=False, stop=True),
                          reads=[Gh_res[i], g.cres], writes=[g.ps_res[bk]])
                if h == 2 and pending:
                    pending.pop(0)()
            def evac(ib=ib, gi=gi, banks=banks):
                for half, bk in enumerate(banks):
                    kb.op("act", lambda e, half=half, bk=bk: e.activation(out=GT[gi][:, half * 4:(half + 1) * 4, :],
                                                                          in_=g.ps[bk][:].rearrange("p (a t) -> p a t", t=128), func=AF.Identity),
                          reads=[g.ps_res[bk]], writes=[GT_res[gi]])
                dst = g.gst[ib * 1024:(ib + 1) * 1024, b * T + tt * 128:b * T + (tt + 1) * 128].rearrange("(a j) t -> j a t", j=128)
                kb.dma("sp", dst, GT[gi][:], reads=[GT_res[gi]], writes=[g.gst_res[b][tt]])
            pending.append(evac)
        while pending:
            pending.pop(0)()


def peer_x(g, es, l, tg):
    nc, kb, I = g.nc, g.kb, g.I
    def sb(name, shape, dt):
        return es.enter_context(nc.sbuf_tensor(uniq(name), list(shape), dt))
    b, half = divmod(tg, 2)
    tsl = slice(half * 1024, (half + 1) * 1024)
    gsl = slice(tg * 1024, (tg + 1) * 1024)
    h2c = sb("px_h2", [128, 16, 1024], BF16)
    h2_res = Res()
    kb.dma("sp", h2c[:], g.hTs[b, :, tsl].rearrange("(k p) t -> p k t", p=128), reads=g.hTs_res[b], writes=[h2_res])
    ublk = [sb(f"px_u{i}", [128, 16, 512], BF16) for i in range(2)]
    ublk_res = mkres(2)
    gt = [sb(f"px_gt{i}", [128, 1024], BF16) for i in range(2)]
    gt_res = mkres(2)
    ga = [sb(f"px_ga{i}", [128, 512], F32) for i in range(2)]
    ga_res = mkres(2)
    go = [sb(f"px_go{i}", [128, 1024], BF16) for i in range(2)]
    go_res = mkres(2)
    gst_deps = g.gst_res[b][half * 8:(half + 1) * 8]
    def uload(eb):
        load_wblock(g, "pool", ublk[eb % 2][:], ublk_res[eb % 2], I["peer_uT"][l][:, eb * 512:(eb + 1) * 512])
    uload(0)
    n = 0
    for eb in range(32):
        if eb + 1 < 32:
            uload(eb + 1)
        u, u_res = ublk[eb % 2], ublk_res[eb % 2]
        for ec in range(4):
            e0 = (eb * 4 + ec) * 128
            i = (eb * 4 + ec) % 2
            kb.dma("sp", gt[i][:], g.gst[e0:e0 + 128, gsl], reads=gst_deps, writes=[gt_res[i]])
            for tcc in range(2):
                cs = slice(tcc * 512, (tcc + 1) * 512)
                j = n % 2
                n += 1
                ps = g.ps[j]
                for k in range(16):
                    kb.op("pe", lambda e, k=k: e.matmul(ps[:], u[:, k, ec * 128:(ec + 1) * 128], h2c[:, k, cs], start=(k == 0), stop=(k == 15)),
                          reads=[u_res, h2_res], writes=[g.ps_res[j]])
                kb.op("act", lambda e: e.activation(out=ga[j][:], in_=ps[:], func=AF.Gelu), reads=[g.ps_res[j]], writes=[ga_res[j]])
                kb.op("dve", lambda e: e.tensor_tensor(out=go[i][:, cs], in0=ga[j][:], in1=gt[i][:, cs], op=ALU.mult),
                      reads=[ga_res[j], gt_res[i]], writes=[go_res[i]])
            kb.dma("sp", g.gas[e0:e0 + 128, gsl], go[i][:], reads=[go_res[i]], writes=[g.gas_res[tg]])


def peer_y(g, es, l):
    nc, kb, I = g.nc, g.kb, g.I
    def sb(name, shape, dt):
        return es.enter_context(nc.sbuf_tensor(uniq(name), list(shape), dt))
    EC = 8
    g2bc = [sb(f"py_g2{b}", [128, D], F32) for b in range(NB)]
    g2_res = mkres(NB)
    with scope(g) as es1:
        def sb1(name, shape, dt):
            return es1.enter_context(nc.sbuf_tensor(uniq(name), list(shape), dt))
        for b in range(NB):
            build_rowbcast(g, sb1, g2bc[b], g2_res[b], lambda k, b=b: g.modT[:, l, 80 + k, b:b + 1])
    gab = [sb(f"py_ga{i}", [128, EC, 512], BF16) for i in range(2)]
    gab_res = mkres(2)
    vb = [sb(f"py_v{i}", [128, EC, 512], BF16) for i in range(2)]
    vb_res = mkres(2)
    xt = [sb(f"py_xt{i}", [128, 512], F32) for i in range(2)]
    xt_res = mkres(2)
    tmp = [sb(f"py_tmp{i}", [128, 512], F32) for i in range(2)]
    tmp_res = mkres(2)
    NBLK = 128 // EC
    for grp in range(NB * T // 512):
        b = grp // 4
        gsl = slice(grp * 512, (grp + 1) * 512)
        for dq in range(4):
            dsl = slice(dq * 512, (dq + 1) * 512)
            def load(blk):
                i = blk % 2
                e0 = blk * EC * 128
                kb.dma("sp", gab[i][:], g.gas[e0:e0 + EC * 128, gsl].rearrange("(c p) t -> p c t", p=128), reads=[g.gas_res[grp // 2]], writes=[gab_res[i]])
                kb.dma("pool", vb[i][:], I["peer_v"][l][e0:e0 + EC * 128, dsl].rearrange("(c p) d -> p c d", p=128), writes=[vb_res[i]])
            load(0)
            for blk in range(NBLK):
                if blk + 1 < NBLK:
                    load(blk + 1)
                i = blk % 2
                for c in range(EC):
                    first = (blk == 0 and c == 0)
                    last = (blk == NBLK - 1 and c == EC - 1)
                    for tt in range(4):
                        kb.op("pe", lambda e, tt=tt, c=c: e.matmul(g.ps[tt][:], gab[i][:, c, tt * 128:(tt + 1) * 128], vb[i][:, c, :], start=first, stop=last),
                              reads=[gab_res[i], vb_res[i]], writes=[g.ps_res[tt]])
            for tt in range(4):
                tg_ = (grp % 4) * 4 + tt
                rows = slice(tg_ * 128, (tg_ + 1) * 128)
                i = tt % 2
                kb.dma("sp", xt[i][:], g.xres[b, rows, dsl], reads=[g.xres_res[b][tg_]], writes=[xt_res[i]])
                kb.op("dve", lambda e: e.tensor_tensor(out=tmp[i][:], in0=g.ps[tt][:], in1=g2bc[b][:, dsl], op=ALU.mult),
                      reads=[g.ps_res[tt], g2_res[b]], writes=[tmp_res[i]])
                kb.op("dve", lambda e: e.tensor_tensor(out=xt[i][:], in0=xt[i][:], in1=tmp[i][:], op=ALU.add),
                      reads=[tmp_res[i], xt_res[i]], writes=[xt_res[i]])
                kb.dma("sp", g.xres[b, rows, dsl], xt[i][:], reads=[xt_res[i]], writes=[g.xres_res[b][tg_]])


def branch_pool(g, es, l, b):
    nc, kb, I = g.nc, g.kb, g.I
    def sb(name, shape, dt):
        return es.enter_context(nc.sbuf_tensor(uniq(name), list(shape), dt))
    pT = sb("pl_p", [128, 4, T], BF16)
    pT_res = mkres(4)
    pw = sb("pl_w", [128, 4, 128], BF16)
    pw_res = Res()
    psc = sb("pl_sc", [128, 4], F32)
    kb.dma("pool", pw[:], I["pool_w"][l].rearrange("g c d -> c g d"), writes=[pw_res])
    kb.dma("sp", psc[:], I["pool_scale"][:, l, :], writes=[pw_res])
    for gi in range(4):
        kb.dma("sp", pT[:, gi, :], g.pj[b, (32 + gi) * 128:(33 + gi) * 128, :], reads=[g.pj_res[b][32 + gi]], writes=[pT_res[gi]])
    dump(g, "pl_pT", pT[:], pT_res)
    dump(g, "pl_pw", pw[:], [pw_res])
    dump(g, "pl_psc", psc[:], [pw_res])
    sa = sb("pl_sa", [128, T], F32)
    sbb = sb("pl_sb", [128, T], F32)
    s_res = mkres(2)
    pooled = sb("pl_pooled", [128, T], BF16)
    pooled_res = Res()
    tmp = sb("pl_tmp", [128, 16], F32)
    tmp_res = Res()
    yo = [sb(f"pl_yo{i}", [128, T], BF16) for i in range(2)]
    yo_res = mkres(2)
    for gi in range(4):
        wlen = 2 ** (gi + 1)
        cur, cur_res = pT[:, gi, :], pT_res[gi]
        bufs = [(sa, s_res[0]), (sbb, s_res[1])]
        for lev in range(gi + 1):
            d = 2 ** lev
            dst, dst_res = bufs[lev % 2]
            kb.op("dve", lambda e, cur=cur, dst=dst, d=d: e.tensor_tensor(out=dst[:, d:], in0=cur[:, d:], in1=cur[:, :T - d], op=ALU.add),
                  reads=[cur_res], writes=[dst_res])
            kb.op("dve", lambda e, cur=cur, dst=dst, d=d: e.tensor_copy(out=dst[:, :d], in_=cur[:, :d]),
                  reads=[cur_res], writes=[dst_res])
            cur, cur_res = dst[:], dst_res
        kb.op("dve", lambda e, cur=cur: e.scalar_tensor_tensor(out=pooled[:], in0=cur, scalar=1.0 / wlen, in1=pT[:, gi, :],
                                                              op0=ALU.mult, op1=ALU.subtract),
              reads=[cur_res, pT_res[gi]], writes=[pooled_res])
        n = wlen - 1
        kb.op("dve", lambda e, cur=cur, n=n: e.tensor_tensor(out=tmp[:, :n], in0=cur[:, :n], in1=g.invc[:, :n], op=ALU.mult),
              reads=[cur_res, g.cres], writes=[tmp_res])
        kb.op("dve", lambda e, n=n: e.tensor_tensor(out=pooled[:, :n], in0=tmp[:, :n], in1=pT[:, gi, :n], op=ALU.subtract),
              reads=[tmp_res, pT_res[gi]], writes=[pooled_res])
        dump(g, "pl_pooled", pooled[:], [pooled_res])
        dump(g, "pl_cur", cur, [cur_res])
        y, y_res = yo[gi % 2], yo_res[gi % 2]
        for tc in range(4):
            bank = 4 + tc % 2
            ps = g.ps[bank]
            kb.op("pe", lambda e, tc=tc, ps=ps: e.matmul(ps[:], pw[:, gi, :], pooled[:, tc * 512:(tc + 1) * 512], start=True, stop=True),
                  reads=[pw_res, pooled_res], writes=[g.ps_res[bank]])
            kb.op("act", lambda e, tc=tc, ps=ps, y=y: e.activation(out=y[:, tc * 512:(tc + 1) * 512], in_=ps[:], func=AF.Identity,
                                                                   scale=psc[:, gi:gi + 1]),
                  reads=[g.ps_res[bank], pw_res], writes=[y_res])
        ct = 12 + gi
        kb.dma("sp", g.ybs[b, ct * 128:(ct + 1) * 128, :], y[:], reads=[y_res], writes=[g.ybs_res[b][ct]])


def branch_sb(g, es, l, b):
    nc, kb, I = g.nc, g.kb, g.I
    def sb(name, shape, dt):
        return es.enter_context(nc.sbuf_tensor(uniq(name), list(shape), dt))
    scale = 1.0 / float(np.sqrt(128.0))
    qT = sb("sb_q", [128, T], BF16)
    kT = sb("sb_k", [128, T], BF16)
    v = sb("sb_v", [128, 16, 128], BF16)
    in_res = mkres(3)
    C = sb("sb_C", [128, T], F32)
    C_res = Res()
    E = [sb(f"sb_E{i}", [128, 512], F32) for i in range(2)]
    L = [sb(f"sb_L{i}", [128, 512], F32) for i in range(2)]
    A = [sb(f"sb_A{i}", [128, 512], F32) for i in range(2)]
    W = [sb(f"sb_W{i}", [128, 512], BF16) for i in range(2)]
    E_res, L_res, A_res, W_res = mkres(2), mkres(2), mkres(2), mkres(2)
    yo = sb("sb_yo", [128, T], BF16)
    yo_res = Res()
    for h in range(4):
        kb.dma("sp", qT[:], g.pj[b, (20 + h) * 128:(21 + h) * 128, :], reads=[g.pj_res[b][20 + h]], writes=[in_res[0]])
        kb.dma("sp", kT[:], g.pj[b, (24 + h) * 128:(25 + h) * 128, :], reads=[g.pj_res[b][24 + h]], writes=[in_res[1]])
        kb.dma("sp", v[:], g.pv[b, 1, :, h * 128:(h + 1) * 128].rearrange("(tt s) e -> s tt e", s=128),
               reads=g.pv_res[b][1], writes=[in_res[2]])
        kb.op("pool", lambda e: e.memset(C[:], 0.0), writes=[C_res])
        for c in range(4):
            kb.op("pe", lambda e, c=c: e.matmul(g.ps[c][:], g.zeros_bf[:], qT[:, 0:512], start=True, stop=True),
                  reads=[g.cres, in_res[0]], writes=[g.ps_res[c]])
        n = 0
        for kbk in range(15, -1, -1):
            for c in range(kbk // 4, 4):
                diag = (c == kbk // 4)
                col0 = (kbk % 4) * 128 if diag else 0
                w = 512 - col0
                t0 = c * 512 + col0
                i = n % 2
                n += 1
                zb = 4 + i
                psz, pst, pso = g.ps[zb], g.ps[6], g.ps[7]
                kb.op("pe", lambda e: e.matmul(psz[:, :w], kT[:, kbk * 128:(kbk + 1) * 128], qT[:, t0:t0 + w], start=True, stop=True),
                      reads=[in_res[0], in_res[1]], writes=[g.ps_res[zb]])
                kb.op("act", lambda e: e.activation(out=E[i][:, :w], in_=psz[:, :w], func=AF.Exp, scale=scale),
                      reads=[g.ps_res[zb]], writes=[E_res[i]])
                kb.op("act", lambda e: e.activation(out=L[i][:, :w], in_=E[i][:, :w], func=AF.Ln, bias=g.onesT[:, 0:1]),
                      reads=[E_res[i], g.cres], writes=[L_res[i]])
                kb.op("dve", lambda e: e.scalar_tensor_tensor(out=A[i][:, :w], in0=psz[:, :w], scalar=scale, in1=L[i][:, :w],
                                                              op0=ALU.mult, op1=ALU.subtract),
                      reads=[g.ps_res[zb], L_res[i]], writes=[A_res[i]])
                if diag:
                    kb.op("dve", lambda e: e.tensor_tensor(out=L[i][:, 0:128], in0=L[i][:, 0:128], in1=g.mtri[:], op=ALU.mult),
                          reads=[L_res[i], g.cres, A_res[i]], writes=[L_res[i]])
                kb.op("pe", lambda e: e.matmul(pst[:, :w], g.triu[:], L[i][:, :w], start=True, stop=True),
                      reads=[g.cres, L_res[i]], writes=[g.ps_res[6]])
                kb.op("pe", lambda e: e.matmul(pso[:, :w], g.ones128[:], L[i][:, :w], start=True, stop=True),
                      reads=[g.cres, L_res[i]], writes=[g.ps_res[7]])
                kb.op("dve", lambda e: e.tensor_tensor(out=A[i][:, :w], in0=A[i][:, :w], in1=pst[:, :w], op=ALU.subtract),
                      reads=[A_res[i], g.ps_res[6]], writes=[A_res[i]])
                kb.op("dve", lambda e: e.tensor_tensor(out=A[i][:, :w], in0=A[i][:, :w], in1=C[:, t0:t0 + w], op=ALU.subtract),
                      reads=[A_res[i], C_res], writes=[A_res[i]])
                kb.op("act", lambda e: e.activation(out=W[i][:, :w], in_=A[i][:, :w], func=AF.Exp),
                      reads=[A_res[i]], writes=[W_res[i]])
                if diag:
                    kb.op("dve", lambda e: e.tensor_tensor(out=W[i][:, 0:128], in0=W[i][:, 0:128], in1=g.mtri_bf[:], op=ALU.mult),
                          reads=[W_res[i], g.cres], writes=[W_res[i]])
                kb.op("dve", lambda e: e.tensor_tensor(out=C[:, t0:t0 + w], in0=C[:, t0:t0 + w], in1=pso[:, :w], op=ALU.add),
                      reads=[C_res, g.ps_res[7]], writes=[C_res])
                kb.op("pe", lambda e: e.matmul(g.ps[c][:, col0:512], v[:, kbk, :], W[i][:, :w], start=False, stop=True),
                      reads=[in_res[2], W_res[i]], writes=[g.ps_res[c]])
        for c in range(4):
            kb.op("act", lambda e, c=c: e.activation(out=yo[:, c * 512:(c + 1) * 512], in_=g.ps[c][:], func=AF.Identity),
                  reads=[g.ps_res[c]], writes=[yo_res])
        ct = 8 + h
        kb.dma("sp", g.ybs[b, ct * 128:(ct + 1) * 128, :], yo[:], reads=[yo_res], writes=[g.ybs_res[b][ct]])


def branch_hg(g, es, l, b):
    nc, kb, I = g.nc, g.kb, g.I
    def sb(name, shape, dt):
        return es.enter_context(nc.sbuf_tensor(uniq(name), list(shape), dt))
    CH, NCH = 32, T // 32
    MID, LAST, CPB = CH // 2 - 1, CH - 1, 512 // CH
    prm = sb("hg_prm", [128, 4, 4], F32)
    prm_res = Res()
    lbl = sb("hg_lbl", [128, DEPTH, 4], F32)
    kb.dma("sp", prm[:, 0, :], I["hg_gain"][:, l, :], writes=[prm_res])
    kb.dma("sp", lbl[:], I["hg_lbl"], writes=[prm_res])
    if l == 0:
        kb.op("dve", lambda e: e.memset(prm[:, 1, :], 0.0), writes=[prm_res])
    else:
        kb.op("dve", lambda e: e.tensor_tensor(out=prm[:, 3, :], in0=lbl[:, 1, :], in1=lbl[:, 0, :], op=ALU.subtract),
              reads=[prm_res], writes=[prm_res])
        kb.op("act", lambda e: e.activation(out=prm[:, 1, :], in_=prm[:, 3, :], func=AF.Sigmoid), reads=[prm_res], writes=[prm_res])
    kb.op("dve", lambda e: e.tensor_scalar(out=prm[:, 2, :], in0=prm[:, 1, :], scalar1=-1.0, scalar2=1.0, op0=ALU.mult, op1=ALU.add),
          reads=[prm_res], writes=[prm_res])
    qr = sb("hg_qr", [128, T], BF16)
    fr = sb("hg_fr", [128, T], F32)
    gr = sb("hg_gr", [128, T], BF16)
    v = sb("hg_v", [CH, NCH, 128], BF16)
    in_res = mkres(4)
    fg = sb("hg_fg", [128, T], F32)
    kk = sb("hg_kk", [128, T], F32)
    bb = sb("hg_bb", [128, T], F32)
    d1 = sb("hg_d1", [128, T], F32)
    e1 = sb("hg_e1", [128, T], F32)
    e2 = sb("hg_e2", [128, T], F32)
    qs = sb("hg_qs", [128, T], F32)
    qe = sb("hg_qe", [128, T], BF16)
    ke = sb("hg_ke", [128, T], BF16)
    qb = sb("hg_qb", [128, T], BF16)
    kd = sb("hg_kd", [128, T], BF16)
    sm = sb("hg_sm", [128, 4, NCH], F32)
    r_fg, r_kk, r_bb, r_d1, r_e1, r_e2, r_qs, r_qe, r_ke, r_qb, r_kd, r_sm = mkres(12)
    PT = [sb(f"hg_PT{i}", [CH, CH], BF16) for i in range(2)]
    PT_res = mkres(2)
    kdT = [sb(f"hg_kdT{i}", [CH, 128], BF16) for i in range(2)]
    kdT_res = mkres(2)
    S = sb("hg_S", [128, 128], F32)
    Sb = sb("hg_Sb", [128, 128], BF16)
    S_res, Sb_res = Res(), Res()
    sq = sb("hg_sq", [128, 512], BF16)
    rt = sb("hg_rt", [128, 512], F32)
    on = sb("hg_on", [128, 512], F32)
    sgl = sb("hg_sgl", [128, 512], F32)
    r_sq, r_rt, r_on, r_sgl = mkres(4)
    yo = sb("hg_yo", [128, T], BF16)
    yo_res = Res()
    def v3(t):
        return t[:].rearrange("p (c s) -> p c s", s=CH)
    for h in range(4):
        kb.dma("sp", qr[:], g.pj[b, (4 + h) * 128:(5 + h) * 128, :], reads=[g.pj_res[b][4 + h]], writes=[in_res[0]])
        kb.dma("sp", fr[:], g.pjf[b, h * 128:(h + 1) * 128, :], reads=[g.pjf_res[b][h]], writes=[in_res[1]])
        kb.dma("sp", gr[:], g.pj[b, (16 + h) * 128:(17 + h) * 128, :], reads=[g.pj_res[b][16 + h]], writes=[in_res[2]])
        kb.dma("sp", v[:], g.pv[b, 0, :, h * 128:(h + 1) * 128].rearrange("(c s) e -> s c e", s=CH),
               reads=g.pv_res[b][0], writes=[in_res[3]])
        kb.op("act", lambda e: e.activation(out=fg[:], in_=fr[:], func=AF.Sigmoid), reads=[in_res[1]], writes=[r_fg])
        kb.op("dve", lambda e: e.tensor_scalar(out=fg[:], in0=fg[:], scalar1=prm[:, 2, h:h + 1], scalar2=prm[:, 1, h:h + 1],
                                               op0=ALU.mult, op1=ALU.add), reads=[r_fg, prm_res], writes=[r_fg])
        kb.op("dve", lambda e: e.tensor_scalar(out=kk[:], in0=fg[:], scalar1=-1.0, scalar2=1.0, op0=ALU.mult, op1=ALU.add),
              reads=[r_fg], writes=[r_kk])
        kb.op("dve", lambda e: e.tensor_scalar(out=fg[:], in0=fg[:], scalar1=1e-6, scalar2=None, op0=ALU.max),
              reads=[r_fg, r_kk], writes=[r_fg])
        kb.op("act", lambda e: e.activation(out=fg[:], in_=fg[:], func=AF.Ln), reads=[r_fg], writes=[r_fg])
        for c in range(NCH):
            kb.op("dve", lambda e, c=c: e.tensor_tensor_scan(out=bb[:, c * CH:(c + 1) * CH], data0=g.ones128[:, 0:CH],
                                                            data1=fg[:, c * CH:(c + 1) * CH], initial=0.0, op0=ALU.mult, op1=ALU.add),
                  reads=[r_fg, g.cres], writes=[r_bb])
        bb3 = v3(bb)
        kb.op("dve", lambda e: e.tensor_tensor(out=v3(d1), in0=bb3, in1=bb3[:, :, MID:MID + 1].to_broadcast([128, NCH, CH]), op=ALU.subtract),
              reads=[r_bb], writes=[r_d1])
        kb.op("act", lambda e: e.activation(out=e1[:], in_=d1[:], func=AF.Exp), reads=[r_d1], writes=[r_e1])
        kb.op("act", lambda e: e.activation(out=e2[:], in_=d1[:], func=AF.Exp, scale=-1.0), reads=[r_d1], writes=[r_e2])
        kb.op("act", lambda e: e.activation(out=qs[:], in_=qr[:], func=AF.Silu), reads=[in_res[0]], writes=[r_qs])
        kb.op("dve", lambda e: e.tensor_tensor(out=qe[:], in0=qs[:], in1=e1[:], op=ALU.mult), reads=[r_qs, r_e1], writes=[r_qe])
        kb.op("dve", lambda e: e.tensor_tensor(out=ke[:], in0=kk[:], in1=e2[:], op=ALU.mult), reads=[r_kk, r_e2], writes=[r_ke])
        kb.op("act", lambda e: e.activation(out=sm[:, 0, :].unsqueeze(2), in_=bb3[:, :, MID:MID + 1], func=AF.Exp), reads=[r_bb], writes=[r_sm])
        kb.op("dve", lambda e: e.tensor_tensor(out=sm[:, 1, :].unsqueeze(2), in0=bb3[:, :, LAST:LAST + 1], in1=bb3[:, :, MID:MID + 1], op=ALU.subtract),
              reads=[r_bb, r_sm], writes=[r_sm])
        kb.op("act", lambda e: e.activation(out=sm[:, 1, :], in_=sm[:, 1, :], func=AF.Exp), reads=[r_sm], writes=[r_sm])
        kb.op("act", lambda e: e.activation(out=sm[:, 2, :].unsqueeze(2), in_=bb3[:, :, LAST:LAST + 1], func=AF.Exp), reads=[r_bb, r_sm], writes=[r_sm])
        kb.op("dve", lambda e: e.tensor_tensor(out=v3(qb), in0=v3(qe), in1=sm[:, 0, :].unsqueeze(2).to_broadcast([128, NCH, CH]), op=ALU.mult),
              reads=[r_qe, r_sm], writes=[r_qb])
        kb.op("dve", lambda e: e.tensor_tensor(out=v3(kd), in0=v3(ke), in1=sm[:, 1, :].unsqueeze(2).to_broadcast([128, NCH, CH]), op=ALU.mult),
              reads=[r_ke, r_sm], writes=[r_kd])
        dump(g, "hg_logf", fg[:], [r_fg]); dump(g, "hg_bb", bb[:], [r_bb]); dump(g, "hg_qe", qe[:], [r_qe]); dump(g, "hg_ke", ke[:], [r_ke])
        dump(g, "hg_qb", qb[:], [r_qb]); dump(g, "hg_kd", kd[:], [r_kd]); dump(g, "hg_sm", sm[:], [r_sm]); dump(g, "hg_v", v[:], [in_res[3]])
        for c in range(NCH):
            i = c % 2
            ob = (c // CPB) % 2
            col = (c % CPB) * CH
            pso = g.ps[ob]
            kb.op("pe", lambda e: e.matmul(g.ps[2][0:CH, 0:CH], ke[:, c * CH:(c + 1) * CH], qe[:, c * CH:(c + 1) * CH], start=True, stop=True),
                  reads=[r_ke, r_qe], writes=[g.ps_res[2]])
            kb.op("dve", lambda e: e.scalar_tensor_tensor(out=PT[i][:], in0=g.ps[2][0:CH, 0:CH], scalar=1e30, in1=g.mle[0:CH, 0:CH],
                                                          op0=ALU.min, op1=ALU.mult),
                  reads=[g.ps_res[2], g.cres], writes=[PT_res[i]])
            kb.op("pe", lambda e: e.matmul(pso[:, col:col + CH], v[:, c, :], PT[i][:], start=True, stop=(c == 0)),
                  reads=[in_res[3], PT_res[i]], writes=[g.ps_res[ob]])
            if c > 0:
                kb.op("pe", lambda e: e.matmul(pso[:, col:col + CH], Sb[:], qb[:, c * CH:(c + 1) * CH], start=False, stop=True),
                      reads=[Sb_res, r_qb], writes=[g.ps_res[ob]])
            if c < NCH - 1:
                pst = g.ps[3][:].bitcast(BF16)
                kb.op("pe", lambda e: e.transpose(pst[0:CH, 0:128], kd[:, c * CH:(c + 1) * CH], g.ident[:]),
                      reads=[r_kd, g.cres], writes=[g.ps_res[3]])
                kb.op("act", lambda e: e.activation(out=kdT[i][:], in_=pst[0:CH, 0:128], func=AF.Identity),
                      reads=[g.ps_res[3]], writes=[kdT_res[i]])
                kb.op("pe", lambda e: e.matmul(g.ps[4][:, 0:128], kdT[i][:], v[:, c, :], start=True, stop=True),
                      reads=[kdT_res[i], in_res[3]], writes=[g.ps_res[4]])
                if c == 0:
                    kb.op("dve", lambda e: e.tensor_copy(out=S[:], in_=g.ps[4][:, 0:128]), reads=[g.ps_res[4]], writes=[S_res])
                else:
                    kb.op("dve", lambda e: e.scalar_tensor_tensor(out=S[:], in0=S[:], scalar=sm[:, 2, c:c + 1], in1=g.ps[4][:, 0:128],
                                                                  op0=ALU.mult, op1=ALU.add),
                          reads=[S_res, r_sm, g.ps_res[4]], writes=[S_res])
                kb.op("act", lambda e: e.activation(out=Sb[:], in_=S[:], func=AF.Identity), reads=[S_res], writes=[Sb_res])
            if c % CPB == CPB - 1:
                blk = c // CPB
                cs = slice(blk * 512, (blk + 1) * 512)
                kb.op("act", lambda e: e.activation(out=sq[:], in_=pso[:], func=AF.Square), reads=[g.ps_res[ob]], writes=[r_sq])
                kb.op("pe", lambda e: e.matmul(g.ps[5][:], g.ones_bf[:], sq[:], start=True, stop=True),
                      reads=[g.cres, r_sq], writes=[g.ps_res[5]])
                kb.op("act", lambda e: e.activation(out=rt[:], in_=g.ps[5][:], func=AF.Sqrt, scale=1.0 / 128.0, bias=g.epsT[:]),
                      reads=[g.ps_res[5], g.cres], writes=[r_rt])
                kb.op("dve", lambda e: e.reciprocal(out=rt[:], in_=rt[:]), reads=[r_rt], writes=[r_rt])
                kb.op("dve", lambda e: e.tensor_tensor(out=on[:], in0=pso[:], in1=rt[:], op=ALU.mult),
                      reads=[g.ps_res[ob], r_rt], writes=[r_on])
                kb.op("act", lambda e: e.activation(out=sgl[:], in_=gr[:, cs], func=AF.Silu), reads=[in_res[2]], writes=[r_sgl])
                kb.op("dve", lambda e: e.scalar_tensor_tensor(out=yo[:, cs], in0=on[:], scalar=prm[:, 0, h:h + 1], in1=sgl[:],
                                                              op0=ALU.mult, op1=ALU.mult),
                      reads=[r_on, r_sgl, prm_res], writes=[yo_res])
        dump(g, "hg_S", S[:], [S_res]); dump(g, "hg_on", on[:], [r_on]); dump(g, "hg_rt", rt[:], [r_rt])
        ct = 4 + h
        kb.dma("sp", g.ybs[b, ct * 128:(ct + 1) * 128, :], yo[:], reads=[yo_res], writes=[g.ybs_res[b][ct]])


def s5_discretize(g, sb, lre, lim, lst, n, res, tag):
    kb = g.kb
    TWO_PI = float(2.0 * np.pi)
    t = {k: sb(f"s5{tag}_{k}", [128, n], F32) for k in ("lr", "step", "mag", "ang", "kf", "sh", "sq", "ch", "are", "aim")}
    ki = sb(f"s5{tag}_ki", [128, n], mybir.dt.int32)
    R = [res]
    def dve(fn):
        kb.op("dve", fn, reads=R, writes=R)
    def act(fn):
        kb.op("act", fn, reads=R, writes=R)
    dve(lambda e: e.tensor_scalar(out=t["lr"][:], in0=lre[:], scalar1=-1e-4, scalar2=None, op0=ALU.min))
    act(lambda e: e.activation(out=t["step"][:], in_=lst[:], func=AF.Exp))
    dve(lambda e: e.tensor_tensor(out=t["mag"][:], in0=t["lr"][:], in1=t["step"][:], op=ALU.mult))
    act(lambda e: e.activation(out=t["mag"][:], in_=t["mag"][:], func=AF.Exp))
    dve(lambda e: e.tensor_tensor(out=t["ang"][:], in0=lim[:], in1=t["step"][:], op=ALU.mult))
    dve(lambda e: e.tensor_scalar(out=t["kf"][:], in0=t["ang"][:], scalar1=1.0 / TWO_PI, scalar2=None, op0=ALU.mult))
    dve(lambda e: e.tensor_copy(out=ki[:], in_=t["kf"][:]))
    dve(lambda e: e.tensor_copy(out=t["kf"][:], in_=ki[:]))
    dve(lambda e: e.scalar_tensor_tensor(out=t["ang"][:], in0=t["kf"][:], scalar=-TWO_PI, in1=t["ang"][:], op0=ALU.mult, op1=ALU.add))
    act(lambda e: e.activation(out=t["sh"][:], in_=t["ang"][:], func=AF.Sin, scale=0.5))
    act(lambda e: e.activation(out=t["sq"][:], in_=t["ang"][:], func=AF.Sin, scale=0.25))
    dve(lambda e: e.tensor_tensor(out=t["ch"][:], in0=t["sq"][:], in1=t["sq"][:], op=ALU.mult))
    dve(lambda e: e.tensor_scalar(out=t["ch"][:], in0=t["ch"][:], scalar1=-2.0, scalar2=1.0, op0=ALU.mult, op1=ALU.add))
    dve(lambda e: e.tensor_tensor(out=t["aim"][:], in0=t["sh"][:], in1=t["ch"][:], op=ALU.mult))
    dve(lambda e: e.scalar_tensor_tensor(out=t["aim"][:], in0=t["aim"][:], scalar=2.0, in1=t["mag"][:], op0=ALU.mult, op1=ALU.mult))
    dve(lambda e: e.tensor_tensor(out=t["are"][:], in0=t["sh"][:], in1=t["sh"][:], op=ALU.mult))
    dve(lambda e: e.tensor_scalar(out=t["are"][:], in0=t["are"][:], scalar1=-2.0, scalar2=1.0, op0=ALU.mult, op1=ALU.add))
    dve(lambda e: e.tensor_tensor(out=t["are"][:], in0=t["are"][:], in1=t["mag"][:], op=ALU.mult))
    return t


def branch_s5(g, es, l, b):
    nc, kb, I = g.nc, g.kb, g.I
    def sb(name, shape, dt):
        return es.enter_context(nc.sbuf_tensor(uniq(name), list(shape), dt))
    NK = 11
    pres = Res()
    R = [pres, g.cres]
    pw = sb("s5pw", [128, 3, NK, 16], F32)
    bbr = sb("s5bbr", [128, 4, 128], F32)
    bbi = sb("s5bbi", [128, 4, 128], F32)
    bbpr = sb("s5bbpr", [128, 16, 128], BF16)
    bbpi = sb("s5bbpi", [128, 16, 128], BF16)
    with scope(g) as esp:
        def sbp(name, shape, dt):
            return esp.enter_context(nc.sbuf_tensor(uniq(name), list(shape), dt))
        raw_s = [sbp(f"s5rs{i}", [128, 16], F32) for i in range(3)]
        raw_b = [sbp(f"s5rb{i}", [128, 512], F32) for i in range(3)]
        bTr = sbp("s5bTr", [128, 512], F32)
        bTi = sbp("s5bTi", [128, 512], F32)
        for i, nm in enumerate(("s5_lre_s", "s5_lim_s", "s5_lst_s")):
            kb.dma("sp", raw_s[i][:], I[nm][:, l, :], writes=R)
        for i, nm in enumerate(("s5_lre_b", "s5_lim_b", "s5_lst_b")):
            kb.dma("sp", raw_b[i][:], I[nm][l], writes=R)
        kb.dma("sp", bTr[:], I["s5_bT_re"][l], writes=R)
        kb.dma("sp", bTi[:], I["s5_bT_im"][l], writes=R)
        ds = s5_discretize(g, sbp, raw_s[0], raw_s[1], raw_s[2], 16, pres, "s")
        db = s5_discretize(g, sbp, raw_b[0], raw_b[1], raw_b[2], 512, pres, "b")
        def dve(fn):
            kb.op("dve", fn, reads=R, writes=R)
        dve(lambda e: e.tensor_copy(out=pw[:, 0, 0, :], in_=ds["are"][:]))
        dve(lambda e: e.tensor_copy(out=pw[:, 1, 0, :], in_=ds["aim"][:]))
        tmp16 = sbp("s5tmp16", [128, 16], F32)
        for k in range(1, NK):
            dve(lambda e, k=k: e.tensor_tensor(out=tmp16[:], in0=pw[:, 1, k - 1, :], in1=pw[:, 1, k - 1, :], op=ALU.mult))
            dve(lambda e, k=k: e.tensor_tensor(out=pw[:, 0, k, :], in0=pw[:, 0, k - 1, :], in1=pw[:, 0, k - 1, :], op=ALU.mult))
            dve(lambda e, k=k: e.tensor_tensor(out=pw[:, 0, k, :], in0=pw[:, 0, k, :], in1=tmp16[:], op=ALU.subtract))
            dve(lambda e, k=k: e.scalar_tensor_tensor(out=pw[:, 1, k, :], in0=pw[:, 0, k - 1, :], scalar=2.0, in1=pw[:, 1, k - 1, :],
                                                      op0=ALU.mult, op1=ALU.mult))
        dve(lambda e: e.tensor_scalar(out=pw[:, 2, :, :], in0=pw[:, 1, :, :], scalar1=-1.0, scalar2=None, op0=ALU.mult))
        den = sbp("s5den", [128, 512], F32)
        nr = sbp("s5nr", [128, 512], F32)
        fr = sbp("s5fr", [128, 512], F32)
        fi = sbp("s5fi", [128, 512], F32)
        t1 = sbp("s5t1", [128, 512], F32)
        lr, li, are, aim = db["lr"], raw_b[1], db["are"], db["aim"]
        dve(lambda e: e.tensor_tensor(out=den[:], in0=lr[:], in1=lr[:], op=ALU.mult))
        dve(lambda e: e.tensor_tensor(out=t1[:], in0=li[:], in1=li[:], op=ALU.mult))
        dve(lambda e: e.tensor_tensor(out=den[:], in0=den[:], in1=t1[:], op=ALU.add))
        dve(lambda e: e.reciprocal(out=den[:], in_=den[:]))
        dve(lambda e: e.tensor_scalar(out=nr[:], in0=are[:], scalar1=-1.0, scalar2=None, op0=ALU.add))
        dve(lambda e: e.tensor_tensor(out=fr[:], in0=nr[:], in1=lr[:], op=ALU.mult))
        dve(lambda e: e.tensor_tensor(out=t1[:], in0=aim[:], in1=li[:], op=ALU.mult))
        dve(lambda e: e.tensor_tensor(out=fr[:], in0=fr[:], in1=t1[:], op=ALU.add))
        dve(lambda e: e.tensor_tensor(out=fr[:], in0=fr[:], in1=den[:], op=ALU.mult))
        dve(lambda e: e.tensor_tensor(out=fi[:], in0=aim[:], in1=lr[:], op=ALU.mult))
        dve(lambda e: e.tensor_tensor(out=t1[:], in0=nr[:], in1=li[:], op=ALU.mult))
        dve(lambda e: e.tensor_tensor(out=fi[:], in0=fi[:], in1=t1[:], op=ALU.subtract))
        dve(lambda e: e.tensor_tensor(out=fi[:], in0=fi[:], in1=den[:], op=ALU.mult))
        bbr2 = bbr[:].rearrange("p a b -> p (a b)")
        bbi2 = bbi[:].rearrange("p a b -> p (a b)")
        dve(lambda e: e.tensor_tensor(out=t1[:], in0=fr[:], in1=bTr[:], op=ALU.mult))
        dve(lambda e: e.tensor_tensor(out=nr[:], in0=fi[:], in1=bTi[:], op=ALU.mult))
        dve(lambda e: e.tensor_tensor(out=bbr2, in0=t1[:], in1=nr[:], op=ALU.subtract))
        dve(lambda e: e.tensor_tensor(out=t1[:], in0=fr[:], in1=bTi[:], op=ALU.mult))
        dve(lambda e: e.tensor_tensor(out=nr[:], in0=fi[:], in1=bTr[:], op=ALU.mult))
        dve(lambda e: e.tensor_tensor(out=bbi2, in0=t1[:], in1=nr[:], op=ALU.add))
        for st in range(16):
            dve(lambda e, st=st: e.tensor_scalar(out=bbpr[:, st, :], in0=bbr[:, st // 4, :], scalar1=g.rmask[:, st % 4:st % 4 + 1],
                                                 scalar2=None, op0=ALU.mult))
            dve(lambda e, st=st: e.tensor_scalar(out=bbpi[:, st, :], in0=bbi[:, st // 4, :], scalar1=g.rmask[:, st % 4:st % 4 + 1],
                                                 scalar2=None, op0=ALU.mult))
        dump(g, "s5_pw", pw[:], R)
        dump(g, "s5_bbr", bbr[:], R)
    cTr = sb("s5cTr", [128, 16, 128], BF16)
    cTi = sb("s5cTi", [128, 16, 128], BF16)
    wgl = sb("s5wgl", [128, 4, 512], BF16)
    dsk = sb("s5dsk", [128, 4], F32)
    bgl = sb("s5bgl", [128, 4], F32)
    kb.dma("pool", cTr[:], I["s5_cT_re"][l], writes=R)
    kb.dma("pool", cTi[:], I["s5_cT_im"][l], writes=R)
    kb.dma("pool", wgl[:], I["s5_wglu"][l].rearrange("(k p) c -> p k c", p=128), writes=R)
    kb.dma("sp", dsk[:], I["s5_d"][:, l, :], writes=R)
    kb.dma("sp", bgl[:], I["s5_bglu"][:, l, :], writes=R)
    kb.op("act", lambda e: e.activation(out=cTi[:], in_=cTi[:], func=AF.Identity, scale=-1.0), reads=R, writes=R)
    uT = sb("s5uT", [128, 4, T], BF16)
    uT_res = Res()
    kb.dma("sp", uT[:], g.pj[b, 0:512, :].rearrange("(k p) t -> p k t", p=128), reads=g.pj_res[b][0:4], writes=[uT_res])
    X = [[sb(f"s5X{i}{c}", [128, T], F32) for c in range(2)] for i in range(2)]
    X_res = [mkres(2) for _ in range(2)]
    xsb = [[sb(f"s5xb{i}{c}", [128, T], BF16) for c in range(2)] for i in range(4)]
    xsb_res = [mkres(2) for _ in range(4)]
    yg = sb("s5yg", [128, 4, T], BF16)
    yg_res = mkres(4)
    ytmp = [sb(f"s5yt{i}", [128, 512], F32) for i in range(2)]
    ytmp_res = mkres(2)
    for st in range(16):
        ut, po = st // 4, 32 * (st % 4)
        for tc in range(4):
            cs = slice(tc * 512, (tc + 1) * 512)
            for c, bbt in enumerate((bbpr, bbpi)):
                bank = 2 * c + tc % 2
                ps = g.ps[bank]
                kb.op("pe", lambda e: e.matmul(ps[:], bbt[:, st, :], uT[:, ut, cs], start=True, stop=True),
                      reads=[pres, uT_res], writes=[g.ps_res[bank]])
                kb.op("act", lambda e: e.activation(out=X[0][c][:, cs], in_=ps[:], func=AF.Identity),
                      reads=[g.ps_res[bank]], writes=[X_res[0][c]])
        cur = 0
        for k in range(NK):
            d = 2 ** k
            s0, s1 = X[cur], X[1 - cur]
            r0, r1 = X_res[cur], X_res[1 - cur]
            ar, ai, nai = pw[:, 0, k, st:st + 1], pw[:, 1, k, st:st + 1], pw[:, 2, k, st:st + 1]
            kb.op("dve", lambda e: e.scalar_tensor_tensor(out=s1[0][:, d:], in0=s0[0][:, :T - d], scalar=ar, in1=s0[0][:, d:],
                                                          op0=ALU.mult, op1=ALU.add), reads=[r0[0], pres], writes=[r1[0]])
            kb.op("dve", lambda e: e.scalar_tensor_tensor(out=s1[0][:, d:], in0=s0[1][:, :T - d], scalar=nai, in1=s1[0][:, d:],
                                                          op0=ALU.mult, op1=ALU.add), reads=[r0[1], pres, r1[0]], writes=[r1[0]])
            kb.op("dve", lambda e: e.scalar_tensor_tensor(out=s1[1][:, d:], in0=s0[1][:, :T - d], scalar=ar, in1=s0[1][:, d:],
                                                          op0=ALU.mult, op1=ALU.add), reads=[r0[1], pres], writes=[r1[1]])
            kb.op("dve", lambda e: e.scalar_tensor_tensor(out=s1[1][:, d:], in0=s0[0][:, :T - d], scalar=ai, in1=s1[1][:, d:],
                                                          op0=ALU.mult, op1=ALU.add), reads=[r0[0], pres, r1[1]], writes=[r1[1]])
            for c in range(2):
                kb.op("act", lambda e, c=c: e.activation(out=s1[c][:, :d], in_=s0[c][:, :d], func=AF.Identity),
                      reads=[r0[c]], writes=[r1[c]])
            cur = 1 - cur
        for c in range(2):
            kb.op("act", lambda e, c=c: e.activation(out=xsb[st % 4][c][:], in_=X[cur][c][:], func=AF.Identity),
                  reads=[X_res[cur][c]], writes=[xsb_res[st % 4][c]])
        if st == 0:
            dump(g, "s5_xs0", X[cur][0][:], [X_res[cur][0]])
        if st % 4 == 3:
            ct = st // 4
            for tc in range(4):
                cs = slice(tc * 512, (tc + 1) * 512)
                bank = 4 + tc % 2
                ps = g.ps[bank]
                n = 0
                for j in range(4):
                    for c, cT in enumerate((cTr, cTi)):
                        kb.op("pe", lambda e: e.matmul(ps[:], cT[:, 4 * ct + j, :], xsb[j][c][:, cs], start=(n == 0), stop=(n == 7)),
                              reads=[pres, xsb_res[j][c]], writes=[g.ps_res[bank]])
                        n += 1
                yt, yt_res = ytmp[tc % 2], ytmp_res[tc % 2]
                kb.op("dve", lambda e: e.scalar_tensor_tensor(out=yt[:], in0=uT[:, ct, cs], scalar=dsk[:, ct:ct + 1], in1=ps[:],
                                                              op0=ALU.mult, op1=ALU.add),
                      reads=[uT_res, pres, g.ps_res[bank]], writes=[yt_res])
                kb.op("act", lambda e: e.activation(out=yg[:, ct, cs], in_=yt[:], func=AF.Gelu), reads=[yt_res], writes=[yg_res[ct]])
    sg = [sb(f"s5sg{i}", [128, 512], BF16) for i in range(2)]
    sg_res = mkres(2)
    yo = [sb(f"s5yo{i}", [128, T], BF16) for i in range(2)]
    yo_res = mkres(2)
    n = 0
    for co in range(4):
        for tc in range(4):
            cs = slice(tc * 512, (tc + 1) * 512)
            bank = 6 + n % 2
            i = n % 2
            n += 1
            ps = g.ps[bank]
            for ci in range(4):
                kb.op("pe", lambda e: e.matmul(ps[:], wgl[:, ci, co * 128:(co + 1) * 128], yg[:, ci, cs], start=(ci == 0), stop=(ci == 3)),
                      reads=[pres, yg_res[ci]], writes=[g.ps_res[bank]])
            kb.op("act", lambda e: e.activation(out=sg[i][:], in_=ps[:], func=AF.Sigmoid, bias=bgl[:, co:co + 1]),
                  reads=[g.ps_res[bank], pres], writes=[sg_res[i]])
            kb.op("dve", lambda e: e.tensor_tensor(out=yo[co % 2][:, cs], in0=yg[:, co, cs], in1=sg[i][:], op=ALU.mult),
                  reads=[yg_res[co], sg_res[i]], writes=[yo_res[co % 2]])
        kb.dma("sp", g.ybs[b, co * 128:(co + 1) * 128, :], yo[co % 2][:], reads=[yo_res[co % 2]], writes=[g.ybs_res[b][co]])


def prep_inputs(inputs, core):
    f = lambda a: np.ascontiguousarray(np.asarray(a, dtype=np.float32))
    b0 = core * NB
    m = {}
    m["x"] = f(inputs["x"][b0:b0 + NB])
    m["cT"] = f(np.transpose(_pk(inputs["c"][b0:b0 + NB]), (1, 2, 0)))
    m["gmix"] = f(np.transpose(_pk(inputs["norm_mix_gain"]), (1, 0, 2)))
    m["gffn"] = f(np.transpose(_pk(inputs["norm_ffn_gain"]), (1, 0, 2)))
    m["gfin"] = f(_pk(inputs["final_gain"]))
    m["pool_scale"] = f(np.transpose(_pk(inputs["pool_scale"]), (1, 0, 2)))
    m["hg_gain"] = f(np.transpose(_pk(inputs["hg_norm_gain"]), (1, 0, 2)))
    m["hg_lbl"] = f(np.transpose(_pk(inputs["hg_lb_logits"]), (1, 0, 2)))
    return m


def s5_host_layouts(inputs):
    f = lambda a: np.ascontiguousarray(np.asarray(a, dtype=np.float32))
    L = DEPTH
    m = {}
    lre, lim, lst = f(inputs["s5_lambda_re"]), f(inputs["s5_lambda_im"]), f(inputs["s5_log_step"])
    lstf = np.broadcast_to(lst[:, :, None], (L, 32, 64))
    def s_lay(a):
        return f(a.reshape(L, 16, 2, 64).transpose(2, 3, 0, 1).reshape(128, L, 16))
    def b_lay(a):
        t = a.reshape(L, 4, 4, 2, 64).transpose(0, 2, 1, 3, 4)
        t = np.broadcast_to(t[:, :, None], (L, 4, 32, 4, 2, 64))
        return f(t.reshape(L, 128, 512))
    m["s5_lre_s"], m["s5_lim_s"], m["s5_lst_s"] = s_lay(lre), s_lay(lim), s_lay(lstf)
    m["s5_lre_b"], m["s5_lim_b"], m["s5_lst_b"] = b_lay(lre), b_lay(lim), b_lay(lstf)
    for nm, key in (("s5_bT_re", "s5_b_re"), ("s5_bT_im", "s5_b_im")):
        bb = f(inputs[key]).reshape(L, 4, 4, 2, 64, 16)
        t = bb.transpose(0, 2, 3, 5, 1, 4)
        arr = np.zeros((L, 4, 2, 16, 4, 2, 64), np.float32)
        for gp in range(2):
            arr[:, :, gp, :, :, gp, :] = t[:, :, gp]
        m[nm] = f(arr.reshape(L, 128, 512))
    for nm, key in (("s5_cT_re", "s5_c_re"), ("s5_cT_im", "s5_c_im")):
        cc = f(inputs[key]).reshape(L, 16, 2, 16, 64)
        arr = np.zeros((L, 2, 64, 16, 4, 2, 16), np.float32)
        for st in range(16):
            for gp in range(2):
                arr[:, gp, :, st, st % 4, gp, :] = cc[:, st, gp].transpose(0, 2, 1)
        m[nm] = f(arr.reshape(L, 128, 16, 128))
    m["s5_d"] = f(np.transpose(_pk(inputs["s5_d"]), (1, 0, 2)))
    m["s5_bglu"] = f(np.transpose(_pk(inputs["s5_b_glu"]), (1, 0, 2)))
    m["s5_wglu"] = f(inputs["s5_w_glu"])
    return m


def shared_inputs(inputs):
    f = lambda a: np.ascontiguousarray(np.asarray(a, dtype=np.float32))
    m = {}
    m.update(s5_host_layouts(inputs))
    for l in range(DEPTH):
        m[f"ada_w{l}"] = f(inputs["ada_w"][l])
    m["ada_b"] = f(inputs["ada_b"]).reshape(DEPTH, 1, 6 * D)
    m["w_in"] = f(inputs["w_in"])
    m["pool_w"] = f(inputs["pool_w"])
    m["w_gate"] = f(inputs["w_gate"])
    m["w_branch"] = f(inputs["w_branch"])
    m["w_out"] = f(inputs["w_out"])
    m["w_query"] = f(inputs["peer_w_query"])
    m["keysT"] = f(np.transpose(np.asarray(inputs["peer_sub_keys"]), (0, 4, 1, 2, 3)).reshape(DEPTH, 128, 16, 128))
    for l in range(DEPTH):
        m[f"peer_uT{l}"] = f(np.asarray(inputs["peer_u"][l]).T)
        m[f"peer_v{l}"] = f(inputs["peer_v"][l])
    m.update(host_consts())
    return m


def kernel(**inputs):
    nc = build()
    sh = shared_inputs(inputs)
    in_maps = []
    for c in range(8):
        m = dict(sh)
        m.update(prep_inputs(inputs, c))
        in_maps.append(m)
    res = run_bass_kernel_spmd(nc, in_maps, core_ids=list(range(8)))
    return np.concatenate([np.asarray(r["out"]) for r in res.results], axis=0)
```

```python
import numpy as np
from contextlib import ExitStack
import concourse.bass as bass
import concourse.mybir as mybir
from concourse.bass_utils import run_bass_kernel_spmd

F32 = mybir.dt.float32
BF16 = mybir.dt.bfloat16
AF = mybir.ActivationFunctionType
ALU = mybir.AluOpType

D = 2048
T = 2048
NB = 2
DEPTH = 2
EPS = 1e-6


class Res:
    __slots__ = ("w", "rs")

    def __init__(self):
        self.w = {}
        self.rs = {}


def mkres(n):
    return [Res() for _ in range(n)]


class KB:
    def __init__(self, nc):
        self.nc = nc
        self.eng = {"pe": nc.tensor, "act": nc.scalar, "dve": nc.vector, "pool": nc.gpsimd, "sp": nc.sync}
        self.sems = {}
        self.cnt = {}
        for e in self.eng:
            self.sems[e] = nc.alloc_semaphore(name=f"c_{e}")
            self.cnt[e] = 0
        self.waited = {e: {} for e in self.eng}
        self.dq = {}
        for q, n in (("sp", 12), ("pool", 2), ("act", 4)):
            lst = []
            for i in range(n):
                key = f"d_{q}{i}"
                self.sems[key] = nc.alloc_semaphore(name=key)
                lst.append([key, 0])
            self.dq[q] = [lst, 0]
        self.ninst = 0

    def _wait(self, e, deps):
        wd = self.waited[e]
        for key, val in deps.items():
            if key == e and e == "pe":
                continue
            if wd.get(key, 0) >= val:
                continue
            self.eng[e].wait_ge(self.sems[key], val)
            wd[key] = val
            self.ninst += 1

    @staticmethod
    def _deps(reads, writes):
        deps = {}
        for r in reads:
            for k, v in r.w.items():
                if deps.get(k, 0) < v:
                    deps[k] = v
        for w in writes:
            for k, v in w.w.items():
                if deps.get(k, 0) < v:
                    deps[k] = v
            for k, v in w.rs.items():
                if deps.get(k, 0) < v:
                    deps[k] = v
        return deps

    @staticmethod
    def _mark(ev, reads, writes):
        k, v = ev
        for r in reads:
            if r.rs.get(k, 0) < v:
                r.rs[k] = v
        for w in writes:
            w.w = {k: v}
            w.rs = {}

    def op(self, e, fn, reads=(), writes=()):
        self._wait(e, self._deps(reads, writes))
        self.cnt[e] += 1
        ins = fn(self.eng[e])
        ins.then_inc(self.sems[e], 1)
        self.ninst += 1
        self._mark((e, self.cnt[e]), reads, writes)

    def dma(self, q, out, in_, reads=(), writes=(), **kw):
        deps = self._deps(reads, writes)
        lst, idx = self.dq[q]
        slot = lst[idx]
        self.dq[q][1] = (idx + 1) % len(lst)
        if slot[1] > 0:
            deps[slot[0]] = max(deps.get(slot[0], 0), slot[1])
        self._wait(q, deps)
        slot[1] += 16
        self.eng[q].dma_start(out=out, in_=in_, **kw).then_inc(self.sems[slot[0]], 16)
        self.ninst += 1
        self._mark((slot[0], slot[1]), reads, writes)

    def barrier(self):
        deps = {}
        for e in self.eng:
            if self.cnt[e] > 0:
                deps[e] = self.cnt[e]
        for q in self.dq:
            for key, val in self.dq[q][0]:
                if val > 0:
                    deps[key] = val
        for e in self.eng:
            d = dict(deps)
            d.pop(e, None) if e != "pe" else None
            self._wait(e, d)

    def wait_all(self, e, ress):
        deps = {}
        for r in ress:
            for k, v in r.w.items():
                if deps.get(k, 0) < v:
                    deps[k] = v
        self._wait(e, deps)


def _pk(v):
    v = np.asarray(v)
    n = v.shape[-1] // 128
    return np.ascontiguousarray(np.swapaxes(v.reshape(v.shape[:-1] + (n, 128)), -1, -2))


def host_consts():
    c = {}
    c["ident_bf"] = np.eye(128, dtype=np.float32)
    c["ident_f32"] = np.eye(128, dtype=np.float32)
    inv = np.zeros((128, 16), np.float32)
    inv[:, :] = 1.0 / (np.arange(16, dtype=np.float32) + 1.0)
    c["invc"] = inv
    si = np.arange(128)[:, None]
    ti = np.arange(128)[None, :]
    c["mtri"] = (si < ti).astype(np.float32)
    c["triu"] = (si > ti).astype(np.float32)
    c["mle"] = (si <= ti).astype(np.float32)
    c["rmask"] = (np.arange(128)[:, None] // 32 == np.arange(4)[None, :]).astype(np.float32)
    c["ones128"] = np.ones((128, 128), np.float32)
    c["zeros128"] = np.zeros((128, 128), np.float32)
    return c


class Ctx:
    pass


class scope:
    def __init__(self, g):
        self.g = g
        self.es = ExitStack()

    def __enter__(self):
        self.es.__enter__()
        return self.es

    def __exit__(self, *a):
        if a[0] is None:
            self.g.kb.barrier()
        return self.es.__exit__(*a)


_UNIQ = [0]


def uniq(name):
    _UNIQ[0] += 1
    return f"{name}_u{_UNIQ[0]}"


def build(debug=None, nlayers=DEPTH):
    nc = bass.Bass("TRN2", target_bir_lowering=False)
    kb = KB(nc)
    g = Ctx()
    g.nc, g.kb, g.debug = nc, kb, debug

    def din(name, shape, dt=F32):
        return nc.dram_tensor(name, list(shape), dt, kind="ExternalInput").ap()

    I = {}
    I["x"] = din("x", [NB, T, D])
    I["cT"] = din("cT", [128, 16, NB])
    adaw = [din(f"ada_w{l}", [D, 6 * D]) for l in range(DEPTH)]
    I["ada_w"] = adaw
    I["ada_b"] = din("ada_b", [DEPTH, 1, 6 * D])
    I["gmix"] = din("gmix", [128, DEPTH, 16])
    I["gffn"] = din("gffn", [128, DEPTH, 16])
    I["gfin"] = din("gfin", [128, 16])
    I["w_in"] = din("w_in", [DEPTH, D, 4608])
    I["pool_w"] = din("pool_w", [DEPTH, 4, 128, 128])
    I["w_gate"] = din("w_gate", [DEPTH, 4, D, D])
    I["w_branch"] = din("w_branch", [DEPTH, 4, 512, D])
    I["w_out"] = din("w_out", [DEPTH, D, D])
    I["w_query"] = din("w_query", [DEPTH, D, D])
    I["keysT"] = din("keysT", [DEPTH, 128, 16, 128])
    I["peer_uT"] = [din(f"peer_uT{l}", [D, 16384]) for l in range(DEPTH)]
    I["peer_v"] = [din(f"peer_v{l}", [16384, D]) for l in range(DEPTH)]
    I["pool_scale"] = din("pool_scale", [128, DEPTH, 4])
    I["hg_gain"] = din("hg_gain", [128, DEPTH, 4])
    for nm in ("s5_lre_s", "s5_lim_s", "s5_lst_s"):
        I[nm] = din(nm, [128, DEPTH, 16])
    for nm in ("s5_lre_b", "s5_lim_b", "s5_lst_b", "s5_bT_re", "s5_bT_im"):
        I[nm] = din(nm, [DEPTH, 128, 512])
    for nm in ("s5_cT_re", "s5_cT_im"):
        I[nm] = din(nm, [DEPTH, 128, 16, 128])
    I["s5_d"] = din("s5_d", [128, DEPTH, 4])
    I["s5_bglu"] = din("s5_bglu", [128, DEPTH, 4])
    I["s5_wglu"] = din("s5_wglu", [DEPTH, 512, 512])
    I["hg_lbl"] = din("hg_lbl", [128, DEPTH, 4])
    I["ident_bf"] = din("ident_bf", [128, 128])
    I["ident_f32"] = din("ident_f32", [128, 128])
    I["invc"] = din("invc", [128, 16])
    I["rmask"] = din("rmask", [128, 4])
    for nm in ("mtri", "triu", "mle", "ones128", "zeros128"):
        I[nm] = din(nm, [128, 128])
    g.I = I

    out = nc.dram_tensor("out", [NB, T, D], F32, kind="ExternalOutput").ap()
    g.out = out
    dbg_kind = "ExternalOutput" if debug else "Internal"
    g.ybs = nc.dram_tensor("ybs", [NB, D, T], BF16, kind=dbg_kind).ap()
    g.ybs_res = [mkres(16) for _ in range(NB)]
    g.hTs = nc.dram_tensor("hTs", [NB, D, T], BF16, kind=dbg_kind).ap()
    g.hTs_res = [mkres(16) for _ in range(NB)]
    g.pj = nc.dram_tensor("pj", [NB, 4608, T], BF16, kind=dbg_kind).ap()
    g.pj_res = [mkres(36) for _ in range(NB)]
    g.pjf = nc.dram_tensor("pjf", [NB, 512, T], F32, kind=dbg_kind).ap()
    g.pjf_res = [mkres(4) for _ in range(NB)]
    g.pv = nc.dram_tensor("pv", [NB, 2, T, 512], BF16, kind=dbg_kind).ap()
    g.pv_res = [[mkres(16) for _ in range(2)] for _ in range(NB)]
    g.gst = nc.dram_tensor("gst", [16384, NB * T], BF16, kind=dbg_kind).ap()
    g.gst_res = [mkres(16) for _ in range(NB)]
    g.gas = nc.dram_tensor("gas", [16384, NB * T], BF16, kind=dbg_kind).ap()
    g.gas_res = [Res() for _ in range(NB * T // 1024)]
    g.vbf = nc.dram_tensor("vbf", [16384, D], BF16, kind="Internal").ap()
    g.vbf_res = mkres(16)
    g.xres = nc.dram_tensor("xres", [NB, T, D], F32, kind=dbg_kind).ap()
    g.xres_res = [mkres(16) for _ in range(NB)]

    with ExitStack() as es:
        def sb(name, shape, dt):
            return es.enter_context(nc.sbuf_tensor(uniq(name), list(shape), dt))

        g.ps = [es.enter_context(nc.psum_tensor(f"ps{i}", [128, 512], F32)) for i in range(8)]
        g.ps_res = mkres(8)
        g.ident = sb("ident", [128, 128], BF16)
        g.identf = sb("identf", [128, 128], F32)
        g.invc = sb("invc", [128, 16], F32)
        g.cres = Res()
        kb.dma("pool", g.ident[:], I["ident_bf"], writes=[g.cres])
        kb.dma("sp", g.identf[:], I["ident_f32"], writes=[g.cres])
        kb.dma("sp", g.invc[:], I["invc"], writes=[g.cres])
        g.mtri = sb("mtri", [128, 128], F32)
        g.mtri_bf = sb("mtri_bf", [128, 128], BF16)
        g.triu = sb("triu", [128, 128], F32)
        g.mle = sb("mle", [128, 128], F32)
        g.ones128 = sb("ones128", [128, 128], F32)
        g.ones_bf = sb("ones_bf", [128, 128], BF16)
        g.zeros_bf = sb("zeros_bf", [128, 128], BF16)
        kb.dma("sp", g.mtri[:], I["mtri"], writes=[g.cres])
        kb.dma("sp", g.triu[:], I["triu"], writes=[g.cres])
        kb.dma("sp", g.mle[:], I["mle"], writes=[g.cres])
        kb.dma("sp", g.ones128[:], I["ones128"], writes=[g.cres])
        kb.dma("pool", g.mtri_bf[:], I["mtri"], writes=[g.cres])
        kb.dma("pool", g.ones_bf[:], I["ones128"], writes=[g.cres])
        kb.dma("pool", g.zeros_bf[:], I["zeros128"], writes=[g.cres])
        g.rmask = sb("rmask", [128, 4], F32)
        kb.dma("sp", g.rmask[:], I["rmask"], writes=[g.cres])
        g.epsT = sb("epsT", [128, 1], F32)
        g.onesT = sb("onesT", [128, 2], F32)
        kb.op("dve", lambda e: e.memset(g.epsT[:], EPS), writes=[g.cres])
        kb.op("dve", lambda e: e.memset(g.onesT[:], 1.0), writes=[g.cres])
        g.modT = sb("modT", [128, DEPTH, 96, NB], F32)
        g.mod_res = Res()
        g.A1 = sb("A1", [128, DEPTH, 16, NB], F32)
        g.A2 = sb("A2", [128, DEPTH, 16, NB], F32)
        g.gmix = sb("gmix", [128, DEPTH, 16], F32)
        g.gffn = sb("gffn", [128, DEPTH, 16], F32)
        g.gfin = sb("gfin", [128, 16], F32)
        kb.dma("sp", g.gmix[:], I["gmix"], writes=[g.cres])
        kb.dma("sp", g.gffn[:], I["gffn"], writes=[g.cres])
        kb.dma("sp", g.gfin[:], I["gfin"], writes=[g.cres])

        g.branches = [branch_s5, branch_hg, branch_sb, branch_pool]
        g.do_merge = True
        g.do_mixer = debug != "peer"
        g.do_peer = debug in (None, "peer", "all")
        if debug and debug.startswith("br:"):
            names = debug[3:].split(",")
            g.branches = [globals()["branch_" + n] for n in names]
            g.do_merge = False
        phase_mod(g)
        if debug == "mod":
            finish_debug(g, es)
            return nc
        for l in range(nlayers):
            if g.do_mixer:
                for b in range(NB):
                    with scope(g) as es2:
                        mixer_part(g, es2, l, b)
            if g.do_peer:
                phase_peer(g, l)
        if debug is None:
            with scope(g) as esf:
                phase_final(g, esf)
        finish(g)
    return nc


def dump(g, tag, ap, res, dt=None):
    if not g.debug:
        return
    if not hasattr(g, "dumps"):
        g.dumps = {}
    if tag in g.dumps:
        return
    shp = list(ap.shape)
    d = g.nc.dram_tensor("dbg_" + tag, shp, dt or ap.dtype, kind="ExternalOutput").ap()
    r = Res()
    g.kb.dma("sp", d, ap, reads=res, writes=[r])
    g.dumps[tag] = r


def finish(g):
    kb = g.kb
    if hasattr(g, "dumps"):
        kb.wait_all("sp", list(g.dumps.values()))
    allres = []
    for lst in (g.ybs_res, g.hTs_res, g.xres_res, g.pj_res, g.pjf_res, g.pv_res[0], g.pv_res[1], g.gst_res):
        for r in lst:
            allres += r
    allres += g.gas_res
    allres += g.vbf_res
    if hasattr(g, "out_res"):
        allres += g.out_res
    kb.wait_all("sp", allres)


def finish_debug(g, es):
    kb, nc = g.kb, g.nc
    dbg = nc.dram_tensor("dbg_mod", [128, DEPTH * 96 * NB], F32, kind="ExternalOutput").ap()
    r = Res()
    kb.dma("sp", dbg, g.modT[:].rearrange("p l c b -> p (l c b)"), reads=[g.mod_res], writes=[r])
    kb.wait_all("sp", [r])


def phase_mod(g):
    nc, kb, I = g.nc, g.kb, g.I
    with scope(g) as es:
        def sb(name, shape, dt):
            return es.enter_context(nc.sbuf_tensor(uniq(name), list(shape), dt))
        condT = sb("condT", [128, 16, NB], F32)
        r_cond = Res()
        kb.dma("sp", condT[:], I["cT"], writes=[r_cond])
        kb.op("act", lambda e: e.activation(out=condT[:], in_=condT[:], func=AF.Silu), reads=[r_cond], writes=[r_cond])
        wb = [sb(f"adaw{i}", [128, 16, 512], F32) for i in range(2)]
        wb_res = mkres(2)
        ab = sb("adab", [1, DEPTH * 6 * D], F32)
        r_ab = Res()
        kb.dma("sp", ab[:], I["ada_b"].rearrange("l o c -> o (l c)"), writes=[r_ab])
        nblk = DEPTH * 24
        def load(i):
            l, blk = divmod(i, 24)
            src = I["ada_w"][l][:, blk * 512:(blk + 1) * 512].rearrange("(k p) c -> p k c", p=128)
            kb.dma("sp", wb[i % 2][:], src, writes=[wb_res[i % 2]])
        load(0)
        for i in range(nblk):
            if i + 1 < nblk:
                load(i + 1)
            l, blk = divmod(i, 24)
            w = wb[i % 2]
            bank = i % 2
            ps = g.ps[bank]
            for j in range(4):
                ct = blk * 4 + j
                for k in range(16):
                    kb.op("pe", lambda e, k=k, j=j: e.matmul(ps[:, 2 * j:2 * j + 2], w[:, k, j * 128:(j + 1) * 128], condT[:, k, :],
                                                         start=(k == 0), stop=False),
                          reads=[wb_res[i % 2], r_cond], writes=[g.ps_res[bank]])
                c0 = l * 6 * D + ct * 128
                kb.op("pe", lambda e, j=j, c0=c0: e.matmul(ps[:, 2 * j:2 * j + 2], ab[0:1, c0:c0 + 128], g.onesT[0:1, :],
                                                        start=False, stop=True),
                      reads=[r_ab, g.cres], writes=[g.ps_res[bank]])
            kb.op("act", lambda e: e.activation(out=g.modT[:, l, blk * 4:blk * 4 + 4, :],
                                                in_=ps[:, 0:8].rearrange("p (j b) -> p j b", b=NB), func=AF.Identity),
                  reads=[g.ps_res[bank]], writes=[g.mod_res])
        for l in range(DEPTH):
            kb.op("dve", lambda e, l=l: e.scalar_tensor_tensor(out=g.A1[:, l], in0=g.modT[:, l, 16:32, :], scalar=1.0,
                                                              in1=g.gmix[:, l, :].unsqueeze(2).to_broadcast([128, 16, NB]),
                                                              op0=ALU.add, op1=ALU.mult),
                  reads=[g.mod_res, g.cres], writes=[g.mod_res])
            kb.op("dve", lambda e, l=l: e.scalar_tensor_tensor(out=g.A2[:, l], in0=g.modT[:, l, 64:80, :], scalar=1.0,
                                                              in1=g.gffn[:, l, :].unsqueeze(2).to_broadcast([128, 16, NB]),
                                                              op0=ALU.add, op1=ALU.mult),
                  reads=[g.mod_res, g.cres], writes=[g.mod_res])


def norm_to_hT(g, es, xsrc, xsrc_res, hT, hT_res, Asc, Bsh):
    nc, kb = g.nc, g.kb
    def sb(name, shape, dt):
        return es.enter_context(nc.sbuf_tensor(uniq(name), list(shape), dt))
    xt = [sb(f"nx{i}", [128, D], F32) for i in range(2)]
    xt_res = mkres(2)
    xn = [sb(f"nxn{i}", [128, D], BF16) for i in range(2)]
    xn_res = mkres(2)
    junk = sb("njunk", [128, D], BF16)
    junk_res = Res()
    ss = sb("nss", [128, 4], F32)
    ss_res = mkres(2)
    def load(tt):
        kb.dma("sp", xt[tt % 2][:], xsrc[tt * 128:(tt + 1) * 128, :], reads=[xsrc_res[tt]] if xsrc_res else [], writes=[xt_res[tt % 2]])
    load(0)
    for tt in range(16):
        if tt + 1 < 16:
            load(tt + 1)
        i = tt % 2
        kb.op("act", lambda e: e.activation(out=junk[:], in_=xt[i][:], func=AF.Square, accum_out=ss[:, i:i + 1]),
              reads=[xt_res[i]], writes=[junk_res, ss_res[i]])
        kb.op("act", lambda e: e.activation(out=ss[:, 2 + i:3 + i], in_=ss[:, i:i + 1], func=AF.Sqrt, scale=1.0 / D, bias=g.epsT[:]),
              reads=[ss_res[i], g.cres], writes=[ss_res[i]])
        kb.op("dve", lambda e: e.reciprocal(out=ss[:, 2 + i:3 + i], in_=ss[:, 2 + i:3 + i]), reads=[ss_res[i]], writes=[ss_res[i]])
        kb.op("dve", lambda e: e.tensor_scalar(out=xn[i][:], in0=xt[i][:], scalar1=ss[:, 2 + i:3 + i], scalar2=None, op0=ALU.mult),
              reads=[xt_res[i], ss_res[i]], writes=[xn_res[i]])
        for half in range(2):
            bank = 6 + half
            psb = g.ps[bank][:].bitcast(BF16)
            for j in range(8):
                k = half * 8 + j
                kb.op("pe", lambda e, j=j, k=k: e.transpose(psb[:, j * 128:(j + 1) * 128], xn[i][:, k * 128:(k + 1) * 128], g.ident[:]),
                      reads=[xn_res[i], g.cres], writes=[g.ps_res[bank]])
            for j in range(8):
                k = half * 8 + j
                kb.op("act", lambda e, j=j, k=k: e.activation(out=hT[:, k, tt * 128:(tt + 1) * 128], in_=psb[:, j * 128:(j + 1) * 128],
                                                               func=AF.Identity, scale=Asc(k), bias=Bsh(k)),
                      reads=[g.ps_res[bank], g.mod_res], writes=[hT_res])


def load_wblock(g, q, dst, dst_res, src2d):
    g.kb.dma(q, dst, src2d.rearrange("(k p) c -> p k c", p=128), writes=[dst_res])


def proj_fm(g, wblk, wblk_res, hT, hT_res, ncol_tiles, evac):
    kb = g.kb
    n = 0
    for j in range(ncol_tiles):
        for tc in range(4):
            bank = n % 4
            n += 1
            ps = g.ps[bank]
            for k in range(16):
                kb.op("pe", lambda e, k=k: e.matmul(ps[:], wblk[:, k, j * 128:(j + 1) * 128], hT[:, k, tc * 512:(tc + 1) * 512],
                                                  start=(k == 0), stop=(k == 15)),
                      reads=[wblk_res, hT_res], writes=[g.ps_res[bank]])
            evac(j, tc, ps, g.ps_res[bank])


def mixer_part(g, es, l, b):
    nc, kb, I = g.nc, g.kb, g.I
    with scope(g) as es1:
        phase_proj(g, es1, l, b)
    for br in g.branches:
        with scope(g) as es2:
            br(g, es2, l, b)
    if g.do_merge:
        with scope(g) as es3:
            phase_merge(g, es3, l, b)


def phase_proj(g, es, l, b):
    nc, kb, I = g.nc, g.kb, g.I
    def sb(name, shape, dt):
        return es.enter_context(nc.sbuf_tensor(uniq(name), list(shape), dt))
    hT = sb("hT", [128, 16, T], BF16)
    hT_res = Res()
    if l == 0:
        xsrc, xsrc_res = I["x"][b], None
    else:
        xsrc, xsrc_res = g.xres[b], g.xres_res[b]
    with scope(g) as es2:
        norm_to_hT(g, es2, xsrc, xsrc_res, hT, hT_res,
                   lambda k: g.A1[:, l, k, b:b + 1], lambda k: g.modT[:, l, k, b:b + 1])
    for k in range(16):
        kb.dma("sp", g.hTs[b, k * 128:(k + 1) * 128, :], hT[:, k, :], reads=[hT_res], writes=[g.hTs_res[b][k]])
    wblk = [sb(f"wblk{i}", [128, 16, 512], BF16) for i in range(2)]
    wblk_res = mkres(2)
    stg = [sb(f"pstg{i}", [128, T], BF16) for i in range(2)]
    stg_res = mkres(2)
    stgf = [sb(f"pstgf{i}", [128, T], F32) for i in range(2)]
    stgf_res = mkres(2)
    stgv = [sb(f"pstgv{i}", [128, 512], BF16) for i in range(2)]
    stgv_res = mkres(2)
    def wload(cb):
        load_wblock(g, "pool", wblk[cb % 2][:], wblk_res[cb % 2], I["w_in"][l, :, cb * 512:(cb + 1) * 512])
    wload(0)
    nst = 0
    for cb in range(9):
        if cb + 1 < 9:
            wload(cb + 1)
        w, w_res = wblk[cb % 2], wblk_res[cb % 2]
        if cb in (3, 7):
            which = 0 if cb == 3 else 1
            for tt in range(16):
                bank = tt % 4
                ps = g.ps[bank]
                for k in range(16):
                    kb.op("pe", lambda e, k=k: e.matmul(ps[:], hT[:, k, tt * 128:(tt + 1) * 128], w[:, k, :], start=(k == 0), stop=(k == 15)),
                          reads=[w_res, hT_res], writes=[g.ps_res[bank]])
                sv, sv_res = stgv[tt % 2], stgv_res[tt % 2]
                kb.op("act", lambda e: e.activation(out=sv[:], in_=ps[:], func=AF.Identity), reads=[g.ps_res[bank]], writes=[sv_res])
                kb.dma("sp", g.pv[b, which, tt * 128:(tt + 1) * 128, :], sv[:], reads=[sv_res], writes=[g.pv_res[b][which][tt]])
            continue
        for j in range(4):
            if cb == 2:
                st, st_res = stgf[j % 2], stgf_res[j % 2]
            else:
                st, st_res = stg[nst % 2], stg_res[nst % 2]
                nst += 1
            for tc in range(4):
                bank = (j * 4 + tc) % 4
                ps = g.ps[bank]
                for k in range(16):
                    kb.op("pe", lambda e, k=k: e.matmul(ps[:], w[:, k, j * 128:(j + 1) * 128], hT[:, k, tc * 512:(tc + 1) * 512],
                                                      start=(k == 0), stop=(k == 15)),
                          reads=[w_res, hT_res], writes=[g.ps_res[bank]])
                kb.op("act", lambda e: e.activation(out=st[:, tc * 512:(tc + 1) * 512], in_=ps[:], func=AF.Identity),
                      reads=[g.ps_res[bank]], writes=[st_res])
            if cb == 2:
                kb.dma("sp", g.pjf[b, j * 128:(j + 1) * 128, :], st[:], reads=[st_res], writes=[g.pjf_res[b][j]])
            else:
                ct = cb * 4 + j
                kb.dma("sp", g.pj[b, ct * 128:(ct + 1) * 128, :], st[:], reads=[st_res], writes=[g.pj_res[b][ct]])


def phase_final(g, es):
    nc, kb, I = g.nc, g.kb, g.I
    def sb(name, shape, dt):
        return es.enter_context(nc.sbuf_tensor(uniq(name), list(shape), dt))
    gbc = sb("fn_g", [128, D], F32)
    gbc_res = Res()
    build_rowbcast(g, sb, gbc, gbc_res, lambda k: g.gfin[:, k:k + 1])
    xt = [sb(f"fn_x{i}", [128, D], F32) for i in range(2)]
    xt_res = mkres(2)
    yt = [sb(f"fn_y{i}", [128, D], F32) for i in range(2)]
    yt_res = mkres(2)
    junk = sb("fn_junk", [128, D], BF16)
    junk_res = Res()
    ss = sb("fn_ss", [128, 4], F32)
    ss_res = mkres(2)
    g.out_res = mkres(NB * 16)
    n = 0
    for b in range(NB):
        for tt in range(16):
            i = n % 2
            n += 1
            rows = slice(tt * 128, (tt + 1) * 128)
            kb.dma("sp", xt[i][:], g.xres[b, rows, :], reads=[g.xres_res[b][tt]], writes=[xt_res[i]])
            kb.op("act", lambda e: e.activation(out=junk[:], in_=xt[i][:], func=AF.Square, accum_out=ss[:, i:i + 1]),
                  reads=[xt_res[i]], writes=[junk_res, ss_res[i]])
            kb.op("act", lambda e: e.activation(out=ss[:, 2 + i:3 + i], in_=ss[:, i:i + 1], func=AF.Sqrt, scale=1.0 / D, bias=g.epsT[:]),
                  reads=[ss_res[i], g.cres], writes=[ss_res[i]])
            kb.op("dve", lambda e: e.reciprocal(out=ss[:, 2 + i:3 + i], in_=ss[:, 2 + i:3 + i]), reads=[ss_res[i]], writes=[ss_res[i]])
            kb.op("dve", lambda e: e.scalar_tensor_tensor(out=yt[i][:], in0=xt[i][:], scalar=ss[:, 2 + i:3 + i], in1=gbc[:],
                                                          op0=ALU.mult, op1=ALU.mult),
                  reads=[xt_res[i], ss_res[i], gbc_res], writes=[yt_res[i]])
            kb.dma("sp", g.out[b, rows, :], yt[i][:], reads=[yt_res[i]], writes=[g.out_res[b * 16 + tt]])


def build_rowbcast(g, sb, dst, dst_res, colfn):
    kb = g.kb
    dg = [sb(f"rb_dg{i}", [128, 128], F32) for i in range(2)]
    dg_res = mkres(2)
    for k in range(16):
        i = k % 2
        kb.op("dve", lambda e: e.tensor_scalar(out=dg[i][:], in0=g.identf[:], scalar1=colfn(k), scalar2=None, op0=ALU.mult),
              reads=[g.cres, g.mod_res], writes=[dg_res[i]])
        kk = k % 4
        kb.op("pe", lambda e: e.matmul(g.ps[6][:, kk * 128:(kk + 1) * 128], g.ones128[:], dg[i][:], start=True, stop=True),
              reads=[g.cres, dg_res[i]], writes=[g.ps_res[6]])
        if kk == 3:
            k0 = k - 3
            kb.op("act", lambda e: e.activation(out=dst[:, k0 * 128:(k0 + 4) * 128], in_=g.ps[6][:], func=AF.Identity),
                  reads=[g.ps_res[6]], writes=[dst_res])


def phase_merge(g, es, l, b):
    nc, kb, I = g.nc, g.kb, g.I
    def sb(name, shape, dt):
        return es.enter_context(nc.sbuf_tensor(uniq(name), list(shape), dt))
    TCH = 1024
    NTC = TCH // 512
    g1bc = sb("mg_g1", [128, D], F32)
    g1_res = Res()
    build_rowbcast(g, sb, g1bc, g1_res, lambda k: g.modT[:, l, 32 + k, b:b + 1])
    hTc = sb("mg_hT", [128, 16, TCH], BF16)
    ybc = sb("mg_yb", [128, 16, TCH], BF16)
    mrg = sb("mg_mrg", [128, 16, TCH], BF16)
    acc = sb("mg_acc", [128, 4, TCH], F32)
    hTc_res, ybc_res = Res(), Res()
    mrg_res = mkres(16)
    acc_res = mkres(4)
    wg = [sb(f"mg_wg{i}", [128, 16, 512], BF16) for i in range(2)]
    wg_res = mkres(2)
    wbr = [sb(f"mg_wb{i}", [128, 4, 512], BF16) for i in range(2)]
    wbr_res = mkres(2)
    sgm = [sb(f"mg_sg{i}", [128, 512], F32) for i in range(2)]
    sgm_res = mkres(2)
    tmp = [sb(f"mg_tmp{i}", [128, 512], F32) for i in range(2)]
    tmp_res = mkres(2)
    xt = [sb(f"mg_xt{i}", [128, 512], F32) for i in range(2)]
    xt_res = mkres(2)
    xsrc = I["x"][b] if l == 0 else g.xres[b]
    for ch in range(T // TCH):
        tsl = slice(ch * TCH, (ch + 1) * TCH)
        kb.dma("sp", hTc[:], g.hTs[b, :, tsl].rearrange("(k p) t -> p k t", p=128), reads=g.hTs_res[b], writes=[hTc_res])
        kb.dma("sp", ybc[:], g.ybs[b, :, tsl].rearrange("(k p) t -> p k t", p=128), reads=g.ybs_res[b], writes=[ybc_res])
        nw = 0
        def wload(cb, n, i):
            kb.dma("pool", wg[i][:], I["w_gate"][l, n, :, cb * 512:(cb + 1) * 512].rearrange("(k p) c -> p k c", p=128), writes=[wg_res[i]])
            kb.dma("pool", wbr[i][:], I["w_branch"][l, n, :, cb * 512:(cb + 1) * 512].rearrange("(k p) c -> p k c", p=128), writes=[wbr_res[i]])
        seq = [(cb, n) for cb in range(4) for n in range(4)]
        wload(seq[0][0], seq[0][1], 0)
        cnt = 0
        for si, (cb, n) in enumerate(seq):
            wi = si % 2
            if si + 1 < len(seq):
                wload(seq[si + 1][0], seq[si + 1][1], (si + 1) % 2)
            for j in range(4):
                dt_ = cb * 4 + j
                for tcc in range(NTC):
                    cs = slice(tcc * 512, (tcc + 1) * 512)
                    i = cnt % 2
                    cnt += 1
                    psg, psb = g.ps[i], g.ps[2 + i]
                    for k in range(16):
                        kb.op("pe", lambda e, k=k: e.matmul(psg[:], wg[wi][:, k, j * 128:(j + 1) * 128], hTc[:, k, cs], start=(k == 0), stop=(k == 15)),
                              reads=[wg_res[wi], hTc_res], writes=[g.ps_res[i]])
                    for kk in range(4):
                        kb.op("pe", lambda e, kk=kk: e.matmul(psb[:], wbr[wi][:, kk, j * 128:(j + 1) * 128], ybc[:, 4 * n + kk, cs],
                                                            start=(kk == 0), stop=(kk == 3)),
                              reads=[wbr_res[wi], ybc_res], writes=[g.ps_res[2 + i]])
                    kb.op("act", lambda e: e.activation(out=sgm[i][:], in_=psg[:], func=AF.Sigmoid), reads=[g.ps_res[i]], writes=[sgm_res[i]])
                    if n == 0:
                        kb.op("dve", lambda e: e.tensor_tensor(out=acc[:, j, cs], in0=sgm[i][:], in1=psb[:], op=ALU.mult),
                              reads=[sgm_res[i], g.ps_res[2 + i]], writes=[acc_res[j]])
                    else:
                        kb.op("dve", lambda e: e.tensor_tensor(out=tmp[i][:], in0=sgm[i][:], in1=psb[:], op=ALU.mult),
                              reads=[sgm_res[i], g.ps_res[2 + i]], writes=[tmp_res[i]])
                        if n < 3:
                            kb.op("pool", lambda e: e.tensor_tensor(out=acc[:, j, cs], in0=acc[:, j, cs], in1=tmp[i][:], op=ALU.add),
                                  reads=[tmp_res[i], acc_res[j]], writes=[acc_res[j]])
                        else:
                            kb.op("pool", lambda e: e.tensor_tensor(out=mrg[:, dt_, cs], in0=acc[:, j, cs], in1=tmp[i][:], op=ALU.add),
                                  reads=[tmp_res[i], acc_res[j]], writes=[mrg_res[dt_]])
        if ch == 0 and b == 0:
            dump(g, "mg_mrg", mrg[:], mrg_res)
        def woload(dc, i):
            kb.dma("pool", wg[i][:], I["w_out"][l, :, dc * 512:(dc + 1) * 512].rearrange("(k p) c -> p k c", p=128), writes=[wg_res[i]])
        woload(0, 0)
        cnt = 0
        for dc in range(4):
            if dc + 1 < 4:
                woload(dc + 1, (dc + 1) % 2)
            wi = dc % 2
            dsl = slice(dc * 512, (dc + 1) * 512)
            for tt in range(TCH // 128):
                i = cnt % 2
                cnt += 1
                tg = ch * (TCH // 128) + tt
                rows = slice(tg * 128, (tg + 1) * 128)
                kb.dma("sp", xt[i][:], xsrc[rows, dsl], reads=([g.xres_res[b][tg]] if l > 0 else []), writes=[xt_res[i]])
                ps = g.ps[4 + i]
                for k in range(16):
                    kb.op("pe", lambda e, k=k: e.matmul(ps[:], mrg[:, k, tt * 128:(tt + 1) * 128], wg[wi][:, k, :], start=(k == 0), stop=(k == 15)),
                          reads=[mrg_res[k], wg_res[wi]], writes=[g.ps_res[4 + i]])
                kb.op("dve", lambda e: e.tensor_tensor(out=tmp[i][:], in0=ps[:], in1=g1bc[:, dsl], op=ALU.mult),
                      reads=[g.ps_res[4 + i], g1_res], writes=[tmp_res[i]])
                kb.op("dve", lambda e: e.tensor_tensor(out=xt[i][:], in0=xt[i][:], in1=tmp[i][:], op=ALU.add),
                      reads=[tmp_res[i], xt_res[i]], writes=[xt_res[i]])
                kb.dma("sp", g.xres[b, rows, dsl], xt[i][:], reads=[xt_res[i]], writes=[g.xres_res[b][tg]])


def phase_peer(g, l):
    for blk in range(16):
        g.kb.dma("pool", g.vbf[blk * 1024:(blk + 1) * 1024, :], g.I["peer_v"][l][blk * 1024:(blk + 1) * 1024, :], writes=[g.vbf_res[blk]])
    for b in range(NB):
        with scope(g) as es:
            peer_route(g, es, l, b)
    for tg in range(NB * T // 1024):
        with scope(g) as es:
            peer_x(g, es, l, tg)
    with scope(g) as es:
        peer_y(g, es, l)


def peer_route(g, es, l, b):
    nc, kb, I = g.nc, g.kb, g.I
    def sb(name, shape, dt):
        return es.enter_context(nc.sbuf_tensor(uniq(name), list(shape), dt))
    NEG = -1e30
    qT = sb("pr_qT", [128, 16, T], BF16)
    qT_res = mkres(16)
    xsrc, xsrc_res = g.xres[b], g.xres_res[b]
    if g.debug == "peer":
        xsrc, xsrc_res = I["x"][b], None
    with scope(g) as es1:
        def sb1(name, shape, dt):
            return es1.enter_context(nc.sbuf_tensor(uniq(name), list(shape), dt))
        hT = sb1("pr_hT", [128, 16, T], BF16)
        hT_res = Res()
        with scope(g) as es2:
            norm_to_hT(g, es2, xsrc, xsrc_res, hT, hT_res,
                       lambda k: g.A2[:, l, k, b:b + 1], lambda k: g.modT[:, l, 48 + k, b:b + 1])
        for k in range(16):
            kb.dma("sp", g.hTs[b, k * 128:(k + 1) * 128, :], hT[:, k, :], reads=[hT_res], writes=[g.hTs_res[b][k]])
        wblk = [sb1(f"pr_w{i}", [128, 16, 512], BF16) for i in range(2)]
        wblk_res = mkres(2)
        def wload(cb):
            load_wblock(g, "pool", wblk[cb % 2][:], wblk_res[cb % 2], I["w_query"][l, :, cb * 512:(cb + 1) * 512])
        wload(0)
        for cb in range(4):
            if cb + 1 < 4:
                wload(cb + 1)
            w, w_res = wblk[cb % 2], wblk_res[cb % 2]
            for j in range(4):
                ct = cb * 4 + j
                for tc in range(4):
                    bank = (j * 4 + tc) % 4
                    ps = g.ps[bank]
                    for k in range(16):
                        kb.op("pe", lambda e, k=k: e.matmul(ps[:], w[:, k, j * 128:(j + 1) * 128], hT[:, k, tc * 512:(tc + 1) * 512],
                                                          start=(k == 0), stop=(k == 15)),
                              reads=[w_res, hT_res], writes=[g.ps_res[bank]])
                    kb.op("act", lambda e: e.activation(out=qT[:, ct, tc * 512:(tc + 1) * 512], in_=ps[:], func=AF.Identity),
                          reads=[g.ps_res[bank]], writes=[qT_res[ct]])
    keysT = sb("pr_keys", [128, 16, 128], BF16)
    keys_res = Res()
    kb.dma("pool", keysT[:], I["keysT"][l], writes=[keys_res])
    sc = sb("pr_sc", [128, 16, 128], F32)
    sc2 = sb("pr_sc2", [128, 16, 128], F32)
    top = sb("pr_top", [128, 16, 16], F32)
    cand = sb("pr_cand", [128, 8, 256], F32)
    cand2 = sb("pr_cand2", [128, 8, 256], F32)
    best = sb("pr_best", [128, 8, 24], F32)
    sm = sb("pr_sm", [128, 8, 8], F32)
    ex = sb("pr_ex", [128, 8, 16], F32)
    r_sc, r_sc2, r_e, r_top, r_cand, r_cand2, r_best, r_sm, r_ex = mkres(9)
    NPB = 4
    P = [sb(f"pr_P{i}", [128, 8, 128], F32) for i in range(NPB)]
    P_res = mkres(NPB)
    Gh = [sb(f"pr_G{i}", [128, 1024], BF16) for i in range(NPB)]
    Gh_res = mkres(NPB)
    GT = [sb(f"pr_GT{i}", [128, 8, 128], BF16) for i in range(2)]
    GT_res = mkres(2)
    sc4 = sc[:].rearrange("p (h two) k -> p h two k", two=2)
    E = [sb(f"pr_E{i}", [128, 1024], BF16) for i in range(4)]
    E_res = mkres(4)
    top4 = top[:].rearrange("p (h two) k -> p h two k", two=2)
    for tt in range(16):
        tsl = slice(tt * 128, (tt + 1) * 128)
        for q4 in range(4):
            bank = q4 % 2
            ps = g.ps[bank]
            for jj in range(4):
                hp = q4 * 4 + jj
                kb.op("pe", lambda e, hp=hp, jj=jj: e.matmul(ps[:, jj * 128:(jj + 1) * 128], qT[:, hp, tsl], keysT[:, hp, :], start=True, stop=True),
                      reads=[qT_res[hp], keys_res], writes=[g.ps_res[bank]])
            kb.op("act", lambda e: e.activation(out=sc[:, q4 * 4:(q4 + 1) * 4, :], in_=ps[:].rearrange("p (a k) -> p a k", k=128), func=AF.Identity),
                  reads=[g.ps_res[bank]], writes=[r_sc])
        for hp in range(16):
            kb.op("dve", lambda e, hp=hp: e.max(out=top[:, hp, 0:8], in_=sc[:, hp, :]), reads=[r_sc], writes=[r_top])
            kb.op("dve", lambda e, hp=hp: e.match_replace(out=sc2[:, hp, :], in_to_replace=top[:, hp, 0:8], in_values=sc[:, hp, :], imm_value=NEG),
                  reads=[r_sc, r_top], writes=[r_sc2])
            kb.op("dve", lambda e, hp=hp: e.max(out=top[:, hp, 8:16], in_=sc2[:, hp, :]), reads=[r_sc2], writes=[r_top])
        kb.op("dve", lambda e: e.tensor_tensor(out=cand[:].rearrange("p h (i j) -> p h i j", j=16),
                                               in0=top4[:, :, 0, :].unsqueeze(3).to_broadcast([128, 8, 16, 16]),
                                               in1=top4[:, :, 1, :].unsqueeze(2).to_broadcast([128, 8, 16, 16]), op=ALU.add),
              reads=[r_top], writes=[r_cand])
        for h in range(8):
            kb.op("dve", lambda e, h=h: e.max(out=best[:, h, 0:8], in_=cand[:, h, :]), reads=[r_cand], writes=[r_best])
            kb.op("dve", lambda e, h=h: e.match_replace(out=cand2[:, h, :], in_to_replace=best[:, h, 0:8], in_values=cand[:, h, :], imm_value=NEG),
                  reads=[r_cand, r_best], writes=[r_cand2])
            kb.op("dve", lambda e, h=h: e.max(out=best[:, h, 8:16], in_=cand2[:, h, :]), reads=[r_cand2], writes=[r_best])
        kb.op("dve", lambda e: e.tensor_tensor(out=ex[:], in0=best[:, :, 0:16], in1=best[:, :, 0:1].to_broadcast([128, 8, 16]), op=ALU.subtract),
              reads=[r_best], writes=[r_ex])
        kb.op("act", lambda e: e.activation(out=ex[:], in_=ex[:], func=AF.Exp), reads=[r_ex], writes=[r_ex])
        kb.op("dve", lambda e: e.tensor_reduce(out=sm[:, :, 0], in_=ex[:], axis=mybir.AxisListType.X, op=ALU.add), reads=[r_ex], writes=[r_sm])
        kb.op("act", lambda e: e.activation(out=sm[:, :, 0], in_=sm[:, :, 0], func=AF.Ln), reads=[r_sm], writes=[r_sm])
        kb.op("dve", lambda e: e.scalar_tensor_tensor(out=sm[:, :, 2], in0=sm[:, :, 0], scalar=-1.0, in1=best[:, :, 0], op0=ALU.mult, op1=ALU.subtract),
              reads=[r_best, r_sm], writes=[r_sm])
        kb.op("dve", lambda e: e.tensor_copy(out=sm[:, :, 1], in_=best[:, :, 15]), reads=[r_best, r_sm], writes=[r_sm])
        if tt == 0 and b == 0:
            dump(g, "pr_sc", sc[:], [r_sc]); dump(g, "pr_best", best[:], [r_best]); dump(g, "pr_sm", sm[:], [r_sm])
        n = 0
        for ib in range(16):
            gi = ib % 2
            banks = (2 + 2 * gi, 3 + 2 * gi)
            for bk in banks:
                kb.op("pe", lambda e, bk=bk: e.matmul(g.ps[bk][:], g.zeros_bf[:], qT[:, 0, 0:512], start=True, stop=True),
                      reads=[g.cres, qT_res[0]], writes=[g.ps_res[bk]])
            for h in range(8):
                i = n % NPB
                n += 1
                kb.op("pool", lambda e: e.tensor_tensor(out=P[i][:], in0=sc4[:, h, 0, ib * 8:(ib + 1) * 8].unsqueeze(2).to_broadcast([128, 8, 128]),
                                                        in1=sc4[:, h, 1, :].unsqueeze(1).to_broadcast([128, 8, 128]), op=ALU.add),
                      reads=[r_sc], writes=[P_res[i]])
                P2 = P[i][:].rearrange("p a k -> p (a k)")
                kb.op("act", lambda e: e.activation(out=E[i][:], in_=P2, func=AF.Exp, bias=sm[:, h, 2:3]), reads=[P_res[i], r_sm], writes=[E_res[i]])
                kb.op("dve", lambda e: e.scalar_tensor_tensor(out=Gh[i][:], in0=P2, scalar=sm[:, h, 1:2], in1=E[i][:], op0=ALU.is_ge, op1=ALU.mult),
                      reads=[P_res[i], E_res[i], r_sm], writes=[Gh_res[i]])
                for ii in range(8):
                    bk = banks[ii // 4]
                    kb.op("pe", lambda e, ii=ii, bk=bk: e.matmul(g.ps[bk][:, (ii % 4) * 128:(ii % 4 + 1) * 128], Gh[i][:, ii * 128:(ii + 1) * 128],
                                                               g.ident[:], start=False, stop=True),
                          reads=[Gh_res[i], g.cres], writes=[g.ps_res[bk]])
            for half, bk in enumerate(banks):
                kb.op("act", lambda e, half=half, bk=bk: e.activation(out=GT[gi][:, half * 4:(half + 1) * 4, :],
                                                                      in_=g.ps[bk][:].rearrange("p (a t) -> p a t", t=128), func=AF.Identity),
                      reads=[g.ps_res[bk]], writes=[GT_res[gi]])
            dst = g.gst[ib * 1024:(ib + 1) * 1024, b * T + tt * 128:b * T + (tt + 1) * 128].rearrange("(a j) t -> j a t", j=128)
            kb.dma("sp", dst, GT[gi][:], reads=[GT_res[gi]], writes=[g.gst_res[b][tt]])


def peer_x(g, es, l, tg):
    nc, kb, I = g.nc, g.kb, g.I
    def sb(name, shape, dt):
        return es.enter_context(nc.sbuf_tensor(uniq(name), list(shape), dt))
    b, half = divmod(tg, 2)
    tsl = slice(half * 1024, (half + 1) * 1024)
    gsl = slice(tg * 1024, (tg + 1) * 1024)
    h2c = sb("px_h2", [128, 16, 1024], BF16)
    h2_res = Res()
    kb.dma("sp", h2c[:], g.hTs[b, :, tsl].rearrange("(k p) t -> p k t", p=128), reads=g.hTs_res[b], writes=[h2_res])
    ublk = [sb(f"px_u{i}", [128, 16, 512], BF16) for i in range(2)]
    ublk_res = mkres(2)
    gt = [sb(f"px_gt{i}", [128, 1024], BF16) for i in range(2)]
    gt_res = mkres(2)
    ga = [sb(f"px_ga{i}", [128, 512], F32) for i in range(2)]
    ga_res = mkres(2)
    go = [sb(f"px_go{i}", [128, 1024], BF16) for i in range(2)]
    go_res = mkres(2)
    gst_deps = g.gst_res[b][half * 8:(half + 1) * 8]
    def uload(eb):
        load_wblock(g, "pool", ublk[eb % 2][:], ublk_res[eb % 2], I["peer_uT"][l][:, eb * 512:(eb + 1) * 512])
    uload(0)
    n = 0
    for eb in range(32):
        if eb + 1 < 32:
            uload(eb + 1)
        u, u_res = ublk[eb % 2], ublk_res[eb % 2]
        for ec in range(4):
            e0 = (eb * 4 + ec) * 128
            i = (eb * 4 + ec) % 2
            kb.dma("sp", gt[i][:], g.gst[e0:e0 + 128, gsl], reads=gst_deps, writes=[gt_res[i]])
            for tcc in range(2):
                cs = slice(tcc * 512, (tcc + 1) * 512)
                j = n % 2
                n += 1
                ps = g.ps[j]
                for k in range(16):
                    kb.op("pe", lambda e, k=k: e.matmul(ps[:], u[:, k, ec * 128:(ec + 1) * 128], h2c[:, k, cs], start=(k == 0), stop=(k == 15)),
                          reads=[u_res, h2_res], writes=[g.ps_res[j]])
                kb.op("act", lambda e: e.activation(out=ga[j][:], in_=ps[:], func=AF.Gelu), reads=[g.ps_res[j]], writes=[ga_res[j]])
                kb.op("dve", lambda e: e.tensor_tensor(out=go[i][:, cs], in0=ga[j][:], in1=gt[i][:, cs], op=ALU.mult),
                      reads=[ga_res[j], gt_res[i]], writes=[go_res[i]])
            kb.dma("sp", g.gas[e0:e0 + 128, gsl], go[i][:], reads=[go_res[i]], writes=[g.gas_res[tg]])


def peer_y(g, es, l):
    nc, kb, I = g.nc, g.kb, g.I
    def sb(name, shape, dt):
        return es.enter_context(nc.sbuf_tensor(uniq(name), list(shape), dt))
    EC = 8
    g2bc = [sb(f"py_g2{b}", [128, D], F32) for b in range(NB)]
    g2_res = mkres(NB)
    with scope(g) as es1:
        def sb1(name, shape, dt):
            return es1.enter_context(nc.sbuf_tensor(uniq(name), list(shape), dt))
        for b in range(NB):
            build_rowbcast(g, sb1, g2bc[b], g2_res[b], lambda k, b=b: g.modT[:, l, 80 + k, b:b + 1])
    gab = [sb(f"py_ga{i}", [128, EC, 512], BF16) for i in range(2)]
    gab_res = mkres(2)
    vb = [sb(f"py_v{i}", [128, EC, 512], BF16) for i in range(2)]
    vb_res = mkres(2)
    xt = [sb(f"py_xt{i}", [128, 512], F32) for i in range(2)]
    xt_res = mkres(2)
    tmp = [sb(f"py_tmp{i}", [128, 512], F32) for i in range(2)]
    tmp_res = mkres(2)
    NBLK = 128 // EC
    for grp in range(NB * T // 512):
        b = grp // 4
        gsl = slice(grp * 512, (grp + 1) * 512)
        for dq in range(4):
            dsl = slice(dq * 512, (dq + 1) * 512)
            def load(blk):
                i = blk % 2
                e0 = blk * EC * 128
                kb.dma("sp", gab[i][:], g.gas[e0:e0 + EC * 128, gsl].rearrange("(c p) t -> p c t", p=128), reads=[g.gas_res[grp // 2]], writes=[gab_res[i]])
                kb.dma("sp", vb[i][:], g.vbf[e0:e0 + EC * 128, dsl].rearrange("(c p) d -> p c d", p=128), reads=[g.vbf_res[blk]], writes=[vb_res[i]])
            load(0)
            for blk in range(NBLK):
                if blk + 1 < NBLK:
                    load(blk + 1)
                i = blk % 2
                for c in range(EC):
                    first = (blk == 0 and c == 0)
                    last = (blk == NBLK - 1 and c == EC - 1)
                    for tt in range(4):
                        kb.op("pe", lambda e, tt=tt, c=c: e.matmul(g.ps[tt][:], gab[i][:, c, tt * 128:(tt + 1) * 128], vb[i][:, c, :], start=first, stop=last),
                              reads=[gab_res[i], vb_res[i]], writes=[g.ps_res[tt]])
            for tt in range(4):
                tg_ = (grp % 4) * 4 + tt
                rows = slice(tg_ * 128, (tg_ + 1) * 128)
                i = tt % 2
                kb.dma("sp", xt[i][:], g.xres[b, rows, dsl], reads=[g.xres_res[b][tg_]], writes=[xt_res[i]])
                kb.op("dve", lambda e: e.tensor_tensor(out=tmp[i][:], in0=g.ps[tt][:], in1=g2bc[b][:, dsl], op=ALU.mult),
                      reads=[g.ps_res[tt], g2_res[b]], writes=[tmp_res[i]])
                kb.op("dve", lambda e: e.tensor_tensor(out=xt[i][:], in0=xt[i][:], in1=tmp[i][:], op=ALU.add),
                      reads=[tmp_res[i], xt_res[i]], writes=[xt_res[i]])
                kb.dma("sp", g.xres[b, rows, dsl], xt[i][:], reads=[xt_res[i]], writes=[g.xres_res[b][tg_]])


def branch_pool(g, es, l, b):
    nc, kb, I = g.nc, g.kb, g.I
    def sb(name, shape, dt):
        return es.enter_context(nc.sbuf_tensor(uniq(name), list(shape), dt))
    pT = sb("pl_p", [128, 4, T], BF16)
    pT_res = mkres(4)
    pw = sb("pl_w", [128, 4, 128], BF16)
    pw_res = Res()
    psc = sb("pl_sc", [128, 4], F32)
    kb.dma("pool", pw[:], I["pool_w"][l].rearrange("g c d -> c g d"), writes=[pw_res])
    kb.dma("sp", psc[:], I["pool_scale"][:, l, :], writes=[pw_res])
    for gi in range(4):
        kb.dma("sp", pT[:, gi, :], g.pj[b, (32 + gi) * 128:(33 + gi) * 128, :], reads=[g.pj_res[b][32 + gi]], writes=[pT_res[gi]])
    dump(g, "pl_pT", pT[:], pT_res)
    dump(g, "pl_pw", pw[:], [pw_res])
    dump(g, "pl_psc", psc[:], [pw_res])
    sa = sb("pl_sa", [128, T], F32)
    sbb = sb("pl_sb", [128, T], F32)
    s_res = mkres(2)
    pooled = sb("pl_pooled", [128, T], BF16)
    pooled_res = Res()
    tmp = sb("pl_tmp", [128, 16], F32)
    tmp_res = Res()
    yo = [sb(f"pl_yo{i}", [128, T], BF16) for i in range(2)]
    yo_res = mkres(2)
    for gi in range(4):
        wlen = 2 ** (gi + 1)
        cur, cur_res = pT[:, gi, :], pT_res[gi]
        bufs = [(sa, s_res[0]), (sbb, s_res[1])]
        for lev in range(gi + 1):
            d = 2 ** lev
            dst, dst_res = bufs[lev % 2]
            kb.op("dve", lambda e, cur=cur, dst=dst, d=d: e.tensor_tensor(out=dst[:, d:], in0=cur[:, d:], in1=cur[:, :T - d], op=ALU.add),
                  reads=[cur_res], writes=[dst_res])
            kb.op("dve", lambda e, cur=cur, dst=dst, d=d: e.tensor_copy(out=dst[:, :d], in_=cur[:, :d]),
                  reads=[cur_res], writes=[dst_res])
            cur, cur_res = dst[:], dst_res
        kb.op("dve", lambda e, cur=cur: e.scalar_tensor_tensor(out=pooled[:], in0=cur, scalar=1.0 / wlen, in1=pT[:, gi, :],
                                                              op0=ALU.mult, op1=ALU.subtract),
              reads=[cur_res, pT_res[gi]], writes=[pooled_res])
        n = wlen - 1
        kb.op("dve", lambda e, cur=cur, n=n: e.tensor_tensor(out=tmp[:, :n], in0=cur[:, :n], in1=g.invc[:, :n], op=ALU.mult),
              reads=[cur_res, g.cres], writes=[tmp_res])
        kb.op("dve", lambda e, n=n: e.tensor_tensor(out=pooled[:, :n], in0=tmp[:, :n], in1=pT[:, gi, :n], op=ALU.subtract),
              reads=[tmp_res, pT_res[gi]], writes=[pooled_res])
        dump(g, "pl_pooled", pooled[:], [pooled_res])
        dump(g, "pl_cur", cur, [cur_res])
        y, y_res = yo[gi % 2], yo_res[gi % 2]
        for tc in range(4):
            bank = 4 + tc % 2
            ps = g.ps[bank]
            kb.op("pe", lambda e, tc=tc, ps=ps: e.matmul(ps[:], pw[:, gi, :], pooled[:, tc * 512:(tc + 1) * 512], start=True, stop=True),
                  reads=[pw_res, pooled_res], writes=[g.ps_res[bank]])
            kb.op("act", lambda e, tc=tc, ps=ps, y=y: e.activation(out=y[:, tc * 512:(tc + 1) * 512], in_=ps[:], func=AF.Identity,
                                                                   scale=psc[:, gi:gi + 1]),
                  reads=[g.ps_res[bank], pw_res], writes=[y_res])
        ct = 12 + gi
        kb.dma("sp", g.ybs[b, ct * 128:(ct + 1) * 128, :], y[:], reads=[y_res], writes=[g.ybs_res[b][ct]])


def branch_sb(g, es, l, b):
    nc, kb, I = g.nc, g.kb, g.I
    def sb(name, shape, dt):
        return es.enter_context(nc.sbuf_tensor(uniq(name), list(shape), dt))
    scale = 1.0 / float(np.sqrt(128.0))
    qT = sb("sb_q", [128, T], BF16)
    kT = sb("sb_k", [128, T], BF16)
    v = sb("sb_v", [128, 16, 128], BF16)
    in_res = mkres(3)
    C = sb("sb_C", [128, T], F32)
    C_res = Res()
    E = [sb(f"sb_E{i}", [128, 512], F32) for i in range(2)]
    L = [sb(f"sb_L{i}", [128, 512], F32) for i in range(2)]
    A = [sb(f"sb_A{i}", [128, 512], F32) for i in range(2)]
    W = [sb(f"sb_W{i}", [128, 512], BF16) for i in range(2)]
    E_res, L_res, A_res, W_res = mkres(2), mkres(2), mkres(2), mkres(2)
    yo = sb("sb_yo", [128, T], BF16)
    yo_res = Res()
    for h in range(4):
        kb.dma("sp", qT[:], g.pj[b, (20 + h) * 128:(21 + h) * 128, :], reads=[g.pj_res[b][20 + h]], writes=[in_res[0]])
        kb.dma("sp", kT[:], g.pj[b, (24 + h) * 128:(25 + h) * 128, :], reads=[g.pj_res[b][24 + h]], writes=[in_res[1]])
        kb.dma("sp", v[:], g.pv[b, 1, :, h * 128:(h + 1) * 128].rearrange("(tt s) e -> s tt e", s=128),
               reads=g.pv_res[b][1], writes=[in_res[2]])
        kb.op("pool", lambda e: e.memset(C[:], 0.0), writes=[C_res])
        for c in range(4):
            kb.op("pe", lambda e, c=c: e.matmul(g.ps[c][:], g.zeros_bf[:], qT[:, 0:512], start=True, stop=True),
                  reads=[g.cres, in_res[0]], writes=[g.ps_res[c]])
        n = 0
        for kbk in range(15, -1, -1):
            for c in range(kbk // 4, 4):
                diag = (c == kbk // 4)
                col0 = (kbk % 4) * 128 if diag else 0
                w = 512 - col0
                t0 = c * 512 + col0
                i = n % 2
                n += 1
                zb = 4 + i
                psz, pst, pso = g.ps[zb], g.ps[6], g.ps[7]
                kb.op("pe", lambda e: e.matmul(psz[:, :w], kT[:, kbk * 128:(kbk + 1) * 128], qT[:, t0:t0 + w], start=True, stop=True),
                      reads=[in_res[0], in_res[1]], writes=[g.ps_res[zb]])
                kb.op("act", lambda e: e.activation(out=E[i][:, :w], in_=psz[:, :w], func=AF.Exp, scale=scale),
                      reads=[g.ps_res[zb]], writes=[E_res[i]])
                kb.op("act", lambda e: e.activation(out=L[i][:, :w], in_=E[i][:, :w], func=AF.Ln, bias=g.onesT[:, 0:1]),
                      reads=[E_res[i], g.cres], writes=[L_res[i]])
                kb.op("dve", lambda e: e.scalar_tensor_tensor(out=A[i][:, :w], in0=psz[:, :w], scalar=scale, in1=L[i][:, :w],
                                                              op0=ALU.mult, op1=ALU.subtract),
                      reads=[g.ps_res[zb], L_res[i]], writes=[A_res[i]])
                if diag:
                    kb.op("dve", lambda e: e.tensor_tensor(out=L[i][:, 0:128], in0=L[i][:, 0:128], in1=g.mtri[:], op=ALU.mult),
                          reads=[L_res[i], g.cres, A_res[i]], writes=[L_res[i]])
                kb.op("pe", lambda e: e.matmul(pst[:, :w], g.triu[:], L[i][:, :w], start=True, stop=True),
                      reads=[g.cres, L_res[i]], writes=[g.ps_res[6]])
                kb.op("pe", lambda e: e.matmul(pso[:, :w], g.ones128[:], L[i][:, :w], start=True, stop=True),
                      reads=[g.cres, L_res[i]], writes=[g.ps_res[7]])
                kb.op("dve", lambda e: e.tensor_tensor(out=A[i][:, :w], in0=A[i][:, :w], in1=pst[:, :w], op=ALU.subtract),
                      reads=[A_res[i], g.ps_res[6]], writes=[A_res[i]])
                kb.op("dve", lambda e: e.tensor_tensor(out=A[i][:, :w], in0=A[i][:, :w], in1=C[:, t0:t0 + w], op=ALU.subtract),
                      reads=[A_res[i], C_res], writes=[A_res[i]])
                kb.op("act", lambda e: e.activation(out=W[i][:, :w], in_=A[i][:, :w], func=AF.Exp),
                      reads=[A_res[i]], writes=[W_res[i]])
                if diag:
                    kb.op("dve", lambda e: e.tensor_tensor(out=W[i][:, 0:128], in0=W[i][:, 0:128], in1=g.mtri_bf[:], op=ALU.mult),
                          reads=[W_res[i], g.cres], writes=[W_res[i]])
                kb.op("dve", lambda e: e.tensor_tensor(out=C[:, t0:t0 + w], in0=C[:, t0:t0 + w], in1=pso[:, :w], op=ALU.add),
                      reads=[C_res, g.ps_res[7]], writes=[C_res])
                kb.op("pe", lambda e: e.matmul(g.ps[c][:, col0:512], v[:, kbk, :], W[i][:, :w], start=False, stop=True),
                      reads=[in_res[2], W_res[i]], writes=[g.ps_res[c]])
        for c in range(4):
            kb.op("act", lambda e, c=c: e.activation(out=yo[:, c * 512:(c + 1) * 512], in_=g.ps[c][:], func=AF.Identity),
                  reads=[g.ps_res[c]], writes=[yo_res])
        ct = 8 + h
        kb.dma("sp", g.ybs[b, ct * 128:(ct + 1) * 128, :], yo[:], reads=[yo_res], writes=[g.ybs_res[b][ct]])


def branch_hg(g, es, l, b):
    nc, kb, I = g.nc, g.kb, g.I
    def sb(name, shape, dt):
        return es.enter_context(nc.sbuf_tensor(uniq(name), list(shape), dt))
    CH, NCH = 32, T // 32
    MID, LAST, CPB = CH // 2 - 1, CH - 1, 512 // CH
    prm = sb("hg_prm", [128, 4, 4], F32)
    prm_res = Res()
    lbl = sb("hg_lbl", [128, DEPTH, 4], F32)
    kb.dma("sp", prm[:, 0, :], I["hg_gain"][:, l, :], writes=[prm_res])
    kb.dma("sp", lbl[:], I["hg_lbl"], writes=[prm_res])
    if l == 0:
        kb.op("dve", lambda e: e.memset(prm[:, 1, :], 0.0), writes=[prm_res])
    else:
        kb.op("dve", lambda e: e.tensor_tensor(out=prm[:, 3, :], in0=lbl[:, 1, :], in1=lbl[:, 0, :], op=ALU.subtract),
              reads=[prm_res], writes=[prm_res])
        kb.op("act", lambda e: e.activation(out=prm[:, 1, :], in_=prm[:, 3, :], func=AF.Sigmoid), reads=[prm_res], writes=[prm_res])
    kb.op("dve", lambda e: e.tensor_scalar(out=prm[:, 2, :], in0=prm[:, 1, :], scalar1=-1.0, scalar2=1.0, op0=ALU.mult, op1=ALU.add),
          reads=[prm_res], writes=[prm_res])
    qr = sb("hg_qr", [128, T], BF16)
    fr = sb("hg_fr", [128, T], F32)
    gr = sb("hg_gr", [128, T], BF16)
    v = sb("hg_v", [CH, NCH, 128], BF16)
    in_res = mkres(4)
    fg = sb("hg_fg", [128, T], F32)
    kk = sb("hg_kk", [128, T], F32)
    bb = sb("hg_bb", [128, T], F32)
    d1 = sb("hg_d1", [128, T], F32)
    e1 = sb("hg_e1", [128, T], F32)
    e2 = sb("hg_e2", [128, T], F32)
    qs = sb("hg_qs", [128, T], F32)
    qe = sb("hg_qe", [128, T], BF16)
    ke = sb("hg_ke", [128, T], BF16)
    qb = sb("hg_qb", [128, T], BF16)
    kd = sb("hg_kd", [128, T], BF16)
    sm = sb("hg_sm", [128, 4, NCH], F32)
    r_fg, r_kk, r_bb, r_d1, r_e1, r_e2, r_qs, r_qe, r_ke, r_qb, r_kd, r_sm = mkres(12)
    PT = [sb(f"hg_PT{i}", [CH, CH], BF16) for i in range(2)]
    PT_res = mkres(2)
    kdT = [sb(f"hg_kdT{i}", [CH, 128], BF16) for i in range(2)]
    kdT_res = mkres(2)
    S = sb("hg_S", [128, 128], F32)
    Sb = sb("hg_Sb", [128, 128], BF16)
    S_res, Sb_res = Res(), Res()
    sq = sb("hg_sq", [128, 512], BF16)
    rt = sb("hg_rt", [128, 512], F32)
    on = sb("hg_on", [128, 512], F32)
    sgl = sb("hg_sgl", [128, 512], F32)
    r_sq, r_rt, r_on, r_sgl = mkres(4)
    yo = sb("hg_yo", [128, T], BF16)
    yo_res = Res()
    def v3(t):
        return t[:].rearrange("p (c s) -> p c s", s=CH)
    for h in range(4):
        kb.dma("sp", qr[:], g.pj[b, (4 + h) * 128:(5 + h) * 128, :], reads=[g.pj_res[b][4 + h]], writes=[in_res[0]])
        kb.dma("sp", fr[:], g.pjf[b, h * 128:(h + 1) * 128, :], reads=[g.pjf_res[b][h]], writes=[in_res[1]])
        kb.dma("sp", gr[:], g.pj[b, (16 + h) * 128:(17 + h) * 128, :], reads=[g.pj_res[b][16 + h]], writes=[in_res[2]])
        kb.dma("sp", v[:], g.pv[b, 0, :, h * 128:(h + 1) * 128].rearrange("(c s) e -> s c e", s=CH),
               reads=g.pv_res[b][0], writes=[in_res[3]])
        kb.op("act", lambda e: e.activation(out=fg[:], in_=fr[:], func=AF.Sigmoid), reads=[in_res[1]], writes=[r_fg])
        kb.op("dve", lambda e: e.tensor_scalar(out=fg[:], in0=fg[:], scalar1=prm[:, 2, h:h + 1], scalar2=prm[:, 1, h:h + 1],
                                               op0=ALU.mult, op1=ALU.add), reads=[r_fg, prm_res], writes=[r_fg])
        kb.op("dve", lambda e: e.tensor_scalar(out=kk[:], in0=fg[:], scalar1=-1.0, scalar2=1.0, op0=ALU.mult, op1=ALU.add),
              reads=[r_fg], writes=[r_kk])
        kb.op("dve", lambda e: e.tensor_scalar(out=fg[:], in0=fg[:], scalar1=1e-6, scalar2=None, op0=ALU.max),
              reads=[r_fg, r_kk], writes=[r_fg])
        kb.op("act", lambda e: e.activation(out=fg[:], in_=fg[:], func=AF.Ln), reads=[r_fg], writes=[r_fg])
        for c in range(NCH):
            kb.op("dve", lambda e, c=c: e.tensor_tensor_scan(out=bb[:, c * CH:(c + 1) * CH], data0=g.ones128[:, 0:CH],
                                                            data1=fg[:, c * CH:(c + 1) * CH], initial=0.0, op0=ALU.mult, op1=ALU.add),
                  reads=[r_fg, g.cres], writes=[r_bb])
        bb3 = v3(bb)
        kb.op("dve", lambda e: e.tensor_tensor(out=v3(d1), in0=bb3, in1=bb3[:, :, MID:MID + 1].to_broadcast([128, NCH, CH]), op=ALU.subtract),
              reads=[r_bb], writes=[r_d1])
        kb.op("act", lambda e: e.activation(out=e1[:], in_=d1[:], func=AF.Exp), reads=[r_d1], writes=[r_e1])
        kb.op("act", lambda e: e.activation(out=e2[:], in_=d1[:], func=AF.Exp, scale=-1.0), reads=[r_d1], writes=[r_e2])
        kb.op("act", lambda e: e.activation(out=qs[:], in_=qr[:], func=AF.Silu), reads=[in_res[0]], writes=[r_qs])
        kb.op("dve", lambda e: e.tensor_tensor(out=qe[:], in0=qs[:], in1=e1[:], op=ALU.mult), reads=[r_qs, r_e1], writes=[r_qe])
        kb.op("dve", lambda e: e.tensor_tensor(out=ke[:], in0=kk[:], in1=e2[:], op=ALU.mult), reads=[r_kk, r_e2], writes=[r_ke])
        kb.op("act", lambda e: e.activation(out=sm[:, 0, :].unsqueeze(2), in_=bb3[:, :, MID:MID + 1], func=AF.Exp), reads=[r_bb], writes=[r_sm])
        kb.op("dve", lambda e: e.tensor_tensor(out=sm[:, 1, :].unsqueeze(2), in0=bb3[:, :, LAST:LAST + 1], in1=bb3[:, :, MID:MID + 1], op=ALU.subtract),
              reads=[r_bb, r_sm], writes=[r_sm])
        kb.op("act", lambda e: e.activation(out=sm[:, 1, :], in_=sm[:, 1, :], func=AF.Exp), reads=[r_sm], writes=[r_sm])
        kb.op("act", lambda e: e.activation(out=sm[:, 2, :].unsqueeze(2), in_=bb3[:, :, LAST:LAST + 1], func=AF.Exp), reads=[r_bb, r_sm], writes=[r_sm])
        kb.op("dve", lambda e: e.tensor_tensor(out=v3(qb), in0=v3(qe), in1=sm[:, 0, :].unsqueeze(2).to_broadcast([128, NCH, CH]), op=ALU.mult),
              reads=[r_qe, r_sm], writes=[r_qb])
        kb.op("dve", lambda e: e.tensor_tensor(out=v3(kd), in0=v3(ke), in1=sm[:, 1, :].unsqueeze(2).to_broadcast([128, NCH, CH]), op=ALU.mult),
              reads=[r_ke, r_sm], writes=[r_kd])
        dump(g, "hg_logf", fg[:], [r_fg]); dump(g, "hg_bb", bb[:], [r_bb]); dump(g, "hg_qe", qe[:], [r_qe]); dump(g, "hg_ke", ke[:], [r_ke])
        dump(g, "hg_qb", qb[:], [r_qb]); dump(g, "hg_kd", kd[:], [r_kd]); dump(g, "hg_sm", sm[:], [r_sm]); dump(g, "hg_v", v[:], [in_res[3]])
        for c in range(NCH):
            i = c % 2
            ob = (c // CPB) % 2
            col = (c % CPB) * CH
            pso = g.ps[ob]
            kb.op("pe", lambda e: e.matmul(g.ps[2][0:CH, 0:CH], ke[:, c * CH:(c + 1) * CH], qe[:, c * CH:(c + 1) * CH], start=True, stop=True),
                  reads=[r_ke, r_qe], writes=[g.ps_res[2]])
            kb.op("dve", lambda e: e.scalar_tensor_tensor(out=PT[i][:], in0=g.ps[2][0:CH, 0:CH], scalar=1e30, in1=g.mle[0:CH, 0:CH],
                                                          op0=ALU.min, op1=ALU.mult),
                  reads=[g.ps_res[2], g.cres], writes=[PT_res[i]])
            kb.op("pe", lambda e: e.matmul(pso[:, col:col + CH], v[:, c, :], PT[i][:], start=True, stop=(c == 0)),
                  reads=[in_res[3], PT_res[i]], writes=[g.ps_res[ob]])
            if c > 0:
                kb.op("pe", lambda e: e.matmul(pso[:, col:col + CH], Sb[:], qb[:, c * CH:(c + 1) * CH], start=False, stop=True),
                      reads=[Sb_res, r_qb], writes=[g.ps_res[ob]])
            if c < NCH - 1:
                pst = g.ps[3][:].bitcast(BF16)
                kb.op("pe", lambda e: e.transpose(pst[0:CH, 0:128], kd[:, c * CH:(c + 1) * CH], g.ident[:]),
                      reads=[r_kd, g.cres], writes=[g.ps_res[3]])
                kb.op("act", lambda e: e.activation(out=kdT[i][:], in_=pst[0:CH, 0:128], func=AF.Identity),
                      reads=[g.ps_res[3]], writes=[kdT_res[i]])
                kb.op("pe", lambda e: e.matmul(g.ps[4][:, 0:128], kdT[i][:], v[:, c, :], start=True, stop=True),
                      reads=[kdT_res[i], in_res[3]], writes=[g.ps_res[4]])
                if c == 0:
                    kb.op("dve", lambda e: e.tensor_copy(out=S[:], in_=g.ps[4][:, 0:128]), reads=[g.ps_res[4]], writes=[S_res])
                else:
                    kb.op("dve", lambda e: e.scalar_tensor_tensor(out=S[:], in0=S[:], scalar=sm[:, 2, c:c + 1], in1=g.ps[4][:, 0:128],
                                                                  op0=ALU.mult, op1=ALU.add),
                          reads=[S_res, r_sm, g.ps_res[4]], writes=[S_res])
                kb.op("act", lambda e: e.activation(out=Sb[:], in_=S[:], func=AF.Identity), reads=[S_res], writes=[Sb_res])
            if c % CPB == CPB - 1:
                blk = c // CPB
                cs = slice(blk * 512, (blk + 1) * 512)
                kb.op("act", lambda e: e.activation(out=sq[:], in_=pso[:], func=AF.Square), reads=[g.ps_res[ob]], writes=[r_sq])
                kb.op("pe", lambda e: e.matmul(g.ps[5][:], g.ones_bf[:], sq[:], start=True, stop=True),
                      reads=[g.cres, r_sq], writes=[g.ps_res[5]])
                kb.op("act", lambda e: e.activation(out=rt[:], in_=g.ps[5][:], func=AF.Sqrt, scale=1.0 / 128.0, bias=g.epsT[:]),
                      reads=[g.ps_res[5], g.cres], writes=[r_rt])
                kb.op("dve", lambda e: e.reciprocal(out=rt[:], in_=rt[:]), reads=[r_rt], writes=[r_rt])
                kb.op("dve", lambda e: e.tensor_tensor(out=on[:], in0=pso[:], in1=rt[:], op=ALU.mult),
                      reads=[g.ps_res[ob], r_rt], writes=[r_on])
                kb.op("act", lambda e: e.activation(out=sgl[:], in_=gr[:, cs], func=AF.Silu), reads=[in_res[2]], writes=[r_sgl])
                kb.op("dve", lambda e: e.scalar_tensor_tensor(out=yo[:, cs], in0=on[:], scalar=prm[:, 0, h:h + 1], in1=sgl[:],
                                                              op0=ALU.mult, op1=ALU.mult),
                      reads=[r_on, r_sgl, prm_res], writes=[yo_res])
        dump(g, "hg_S", S[:], [S_res]); dump(g, "hg_on", on[:], [r_on]); dump(g, "hg_rt", rt[:], [r_rt])
        ct = 4 + h
        kb.dma("sp", g.ybs[b, ct * 128:(ct + 1) * 128, :], yo[:], reads=[yo_res], writes=[g.ybs_res[b][ct]])


def s5_discretize(g, sb, lre, lim, lst, n, res, tag):
    kb = g.kb
    TWO_PI = float(2.0 * np.pi)
    t = {k: sb(f"s5{tag}_{k}", [128, n], F32) for k in ("lr", "step", "mag", "ang", "kf", "sh", "sq", "ch", "are", "aim")}
    ki = sb(f"s5{tag}_ki", [128, n], mybir.dt.int32)
    R = [res]
    def dve(fn):
        kb.op("dve", fn, reads=R, writes=R)
    def act(fn):
        kb.op("act", fn, reads=R, writes=R)
    dve(lambda e: e.tensor_scalar(out=t["lr"][:], in0=lre[:], scalar1=-1e-4, scalar2=None, op0=ALU.min))
    act(lambda e: e.activation(out=t["step"][:], in_=lst[:], func=AF.Exp))
    dve(lambda e: e.tensor_tensor(out=t["mag"][:], in0=t["lr"][:], in1=t["step"][:], op=ALU.mult))
    act(lambda e: e.activation(out=t["mag"][:], in_=t["mag"][:], func=AF.Exp))
    dve(lambda e: e.tensor_tensor(out=t["ang"][:], in0=lim[:], in1=t["step"][:], op=ALU.mult))
    dve(lambda e: e.tensor_scalar(out=t["kf"][:], in0=t["ang"][:], scalar1=1.0 / TWO_PI, scalar2=None, op0=ALU.mult))
    dve(lambda e: e.tensor_copy(out=ki[:], in_=t["kf"][:]))
    dve(lambda e: e.tensor_copy(out=t["kf"][:], in_=ki[:]))
    dve(lambda e: e.scalar_tensor_tensor(out=t["ang"][:], in0=t["kf"][:], scalar=-TWO_PI, in1=t["ang"][:], op0=ALU.mult, op1=ALU.add))
    act(lambda e: e.activation(out=t["sh"][:], in_=t["ang"][:], func=AF.Sin, scale=0.5))
    act(lambda e: e.activation(out=t["sq"][:], in_=t["ang"][:], func=AF.Sin, scale=0.25))
    dve(lambda e: e.tensor_tensor(out=t["ch"][:], in0=t["sq"][:], in1=t["sq"][:], op=ALU.mult))
    dve(lambda e: e.tensor_scalar(out=t["ch"][:], in0=t["ch"][:], scalar1=-2.0, scalar2=1.0, op0=ALU.mult, op1=ALU.add))
    dve(lambda e: e.tensor_tensor(out=t["aim"][:], in0=t["sh"][:], in1=t["ch"][:], op=ALU.mult))
    dve(lambda e: e.scalar_tensor_tensor(out=t["aim"][:], in0=t["aim"][:], scalar=2.0, in1=t["mag"][:], op0=ALU.mult, op1=ALU.mult))
    dve(lambda e: e.tensor_tensor(out=t["are"][:], in0=t["sh"][:], in1=t["sh"][:], op=ALU.mult))
    dve(lambda e: e.tensor_scalar(out=t["are"][:], in0=t["are"][:], scalar1=-2.0, scalar2=1.0, op0=ALU.mult, op1=ALU.add))
    dve(lambda e: e.tensor_tensor(out=t["are"][:], in0=t["are"][:], in1=t["mag"][:], op=ALU.mult))
    return t


def branch_s5(g, es, l, b):
    nc, kb, I = g.nc, g.kb, g.I
    def sb(name, shape, dt):
        return es.enter_context(nc.sbuf_tensor(uniq(name), list(shape), dt))
    NK = 11
    pres = Res()
    R = [pres, g.cres]
    pw = sb("s5pw", [128, 3, NK, 16], F32)
    bbr = sb("s5bbr", [128, 4, 128], F32)
    bbi = sb("s5bbi", [128, 4, 128], F32)
    bbpr = sb("s5bbpr", [128, 16, 128], BF16)
    bbpi = sb("s5bbpi", [128, 16, 128], BF16)
    with scope(g) as esp:
        def sbp(name, shape, dt):
            return esp.enter_context(nc.sbuf_tensor(uniq(name), list(shape), dt))
        raw_s = [sbp(f"s5rs{i}", [128, 16], F32) for i in range(3)]
        raw_b = [sbp(f"s5rb{i}", [128, 512], F32) for i in range(3)]
        bTr = sbp("s5bTr", [128, 512], F32)
        bTi = sbp("s5bTi", [128, 512], F32)
        for i, nm in enumerate(("s5_lre_s", "s5_lim_s", "s5_lst_s")):
            kb.dma("sp", raw_s[i][:], I[nm][:, l, :], writes=R)
        for i, nm in enumerate(("s5_lre_b", "s5_lim_b", "s5_lst_b")):
            kb.dma("sp", raw_b[i][:], I[nm][l], writes=R)
        kb.dma("sp", bTr[:], I["s5_bT_re"][l], writes=R)
        kb.dma("sp", bTi[:], I["s5_bT_im"][l], writes=R)
        ds = s5_discretize(g, sbp, raw_s[0], raw_s[1], raw_s[2], 16, pres, "s")
        db = s5_discretize(g, sbp, raw_b[0], raw_b[1], raw_b[2], 512, pres, "b")
        def dve(fn):
            kb.op("dve", fn, reads=R, writes=R)
        dve(lambda e: e.tensor_copy(out=pw[:, 0, 0, :], in_=ds["are"][:]))
        dve(lambda e: e.tensor_copy(out=pw[:, 1, 0, :], in_=ds["aim"][:]))
        tmp16 = sbp("s5tmp16", [128, 16], F32)
        for k in range(1, NK):
            dve(lambda e, k=k: e.tensor_tensor(out=tmp16[:], in0=pw[:, 1, k - 1, :], in1=pw[:, 1, k - 1, :], op=ALU.mult))
            dve(lambda e, k=k: e.tensor_tensor(out=pw[:, 0, k, :], in0=pw[:, 0, k - 1, :], in1=pw[:, 0, k - 1, :], op=ALU.mult))
            dve(lambda e, k=k: e.tensor_tensor(out=pw[:, 0, k, :], in0=pw[:, 0, k, :], in1=tmp16[:], op=ALU.subtract))
            dve(lambda e, k=k: e.scalar_tensor_tensor(out=pw[:, 1, k, :], in0=pw[:, 0, k - 1, :], scalar=2.0, in1=pw[:, 1, k - 1, :],
                                                      op0=ALU.mult, op1=ALU.mult))
        dve(lambda e: e.tensor_scalar(out=pw[:, 2, :, :], in0=pw[:, 1, :, :], scalar1=-1.0, scalar2=None, op0=ALU.mult))
        den = sbp("s5den", [128, 512], F32)
        nr = sbp("s5nr", [128, 512], F32)
        fr = sbp("s5fr", [128, 512], F32)
        fi = sbp("s5fi", [128, 512], F32)
        t1 = sbp("s5t1", [128, 512], F32)
        lr, li, are, aim = db["lr"], raw_b[1], db["are"], db["aim"]
        dve(lambda e: e.tensor_tensor(out=den[:], in0=lr[:], in1=lr[:], op=ALU.mult))
        dve(lambda e: e.tensor_tensor(out=t1[:], in0=li[:], in1=li[:], op=ALU.mult))
        dve(lambda e: e.tensor_tensor(out=den[:], in0=den[:], in1=t1[:], op=ALU.add))
        dve(lambda e: e.reciprocal(out=den[:], in_=den[:]))
        dve(lambda e: e.tensor_scalar(out=nr[:], in0=are[:], scalar1=-1.0, scalar2=None, op0=ALU.add))
        dve(lambda e: e.tensor_tensor(out=fr[:], in0=nr[:], in1=lr[:], op=ALU.mult))
        dve(lambda e: e.tensor_tensor(out=t1[:], in0=aim[:], in1=li[:], op=ALU.mult))
        dve(lambda e: e.tensor_tensor(out=fr[:], in0=fr[:], in1=t1[:], op=ALU.add))
        dve(lambda e: e.tensor_tensor(out=fr[:], in0=fr[:], in1=den[:], op=ALU.mult))
        dve(lambda e: e.tensor_tensor(out=fi[:], in0=aim[:], in1=lr[:], op=ALU.mult))
        dve(lambda e: e.tensor_tensor(out=t1[:], in0=nr[:], in1=li[:], op=ALU.mult))
        dve(lambda e: e.tensor_tensor(out=fi[:], in0=fi[:], in1=t1[:], op=ALU.subtract))
        dve(lambda e: e.tensor_tensor(out=fi[:], in0=fi[:], in1=den[:], op=ALU.mult))
        bbr2 = bbr[:].rearrange("p a b -> p (a b)")
        bbi2 = bbi[:].rearrange("p a b -> p (a b)")
        dve(lambda e: e.tensor_tensor(out=t1[:], in0=fr[:], in1=bTr[:], op=ALU.mult))
        dve(lambda e: e.tensor_tensor(out=nr[:], in0=fi[:], in1=bTi[:], op=ALU.mult))
        dve(lambda e: e.tensor_tensor(out=bbr2, in0=t1[:], in1=nr[:], op=ALU.subtract))
        dve(lambda e: e.tensor_tensor(out=t1[:], in0=fr[:], in1=bTi[:], op=ALU.mult))
        dve(lambda e: e.tensor_tensor(out=nr[:], in0=fi[:], in1=bTr[:], op=ALU.mult))
        dve(lambda e: e.tensor_tensor(out=bbi2, in0=t1[:], in1=nr[:], op=ALU.add))
        for st in range(16):
            dve(lambda e, st=st: e.tensor_scalar(out=bbpr[:, st, :], in0=bbr[:, st // 4, :], scalar1=g.rmask[:, st % 4:st % 4 + 1],
                                                 scalar2=None, op0=ALU.mult))
            dve(lambda e, st=st: e.tensor_scalar(out=bbpi[:, st, :], in0=bbi[:, st // 4, :], scalar1=g.rmask[:, st % 4:st % 4 + 1],
                                                 scalar2=None, op0=ALU.mult))
        dump(g, "s5_pw", pw[:], R)
        dump(g, "s5_bbr", bbr[:], R)
    cTr = sb("s5cTr", [128, 16, 128], BF16)
    cTi = sb("s5cTi", [128, 16, 128], BF16)
    wgl = sb("s5wgl", [128, 4, 512], BF16)
    dsk = sb("s5dsk", [128, 4], F32)
    bgl = sb("s5bgl", [128, 4], F32)
    kb.dma("pool", cTr[:], I["s5_cT_re"][l], writes=R)
    kb.dma("pool", cTi[:], I["s5_cT_im"][l], writes=R)
    kb.dma("pool", wgl[:], I["s5_wglu"][l].rearrange("(k p) c -> p k c", p=128), writes=R)
    kb.dma("sp", dsk[:], I["s5_d"][:, l, :], writes=R)
    kb.dma("sp", bgl[:], I["s5_bglu"][:, l, :], writes=R)
    kb.op("act", lambda e: e.activation(out=cTi[:], in_=cTi[:], func=AF.Identity, scale=-1.0), reads=R, writes=R)
    uT = sb("s5uT", [128, 4, T], BF16)
    uT_res = Res()
    kb.dma("sp", uT[:], g.pj[b, 0:512, :].rearrange("(k p) t -> p k t", p=128), reads=g.pj_res[b][0:4], writes=[uT_res])
    X = [[sb(f"s5X{i}{c}", [128, T], F32) for c in range(2)] for i in range(2)]
    X_res = [mkres(2) for _ in range(2)]
    xsb = [[sb(f"s5xb{i}{c}", [128, T], BF16) for c in range(2)] for i in range(4)]
    xsb_res = [mkres(2) for _ in range(4)]
    yg = sb("s5yg", [128, 4, T], BF16)
    yg_res = mkres(4)
    ytmp = [sb(f"s5yt{i}", [128, 512], F32) for i in range(2)]
    ytmp_res = mkres(2)
    for st in range(16):
        ut, po = st // 4, 32 * (st % 4)
        for tc in range(4):
            cs = slice(tc * 512, (tc + 1) * 512)
            for c, bbt in enumerate((bbpr, bbpi)):
                bank = 2 * c + tc % 2
                ps = g.ps[bank]
                kb.op("pe", lambda e: e.matmul(ps[:], bbt[:, st, :], uT[:, ut, cs], start=True, stop=True),
                      reads=[pres, uT_res], writes=[g.ps_res[bank]])
                kb.op("act", lambda e: e.activation(out=X[0][c][:, cs], in_=ps[:], func=AF.Identity),
                      reads=[g.ps_res[bank]], writes=[X_res[0][c]])
        cur = 0
        for k in range(NK):
            d = 2 ** k
            s0, s1 = X[cur], X[1 - cur]
            r0, r1 = X_res[cur], X_res[1 - cur]
            ar, ai, nai = pw[:, 0, k, st:st + 1], pw[:, 1, k, st:st + 1], pw[:, 2, k, st:st + 1]
            kb.op("dve", lambda e: e.scalar_tensor_tensor(out=s1[0][:, d:], in0=s0[0][:, :T - d], scalar=ar, in1=s0[0][:, d:],
                                                          op0=ALU.mult, op1=ALU.add), reads=[r0[0], pres], writes=[r1[0]])
            kb.op("dve", lambda e: e.scalar_tensor_tensor(out=s1[0][:, d:], in0=s0[1][:, :T - d], scalar=nai, in1=s1[0][:, d:],
                                                          op0=ALU.mult, op1=ALU.add), reads=[r0[1], pres, r1[0]], writes=[r1[0]])
            kb.op("dve", lambda e: e.scalar_tensor_tensor(out=s1[1][:, d:], in0=s0[1][:, :T - d], scalar=ar, in1=s0[1][:, d:],
                                                          op0=ALU.mult, op1=ALU.add), reads=[r0[1], pres], writes=[r1[1]])
            kb.op("dve", lambda e: e.scalar_tensor_tensor(out=s1[1][:, d:], in0=s0[0][:, :T - d], scalar=ai, in1=s1[1][:, d:],
                                                          op0=ALU.mult, op1=ALU.add), reads=[r0[0], pres, r1[1]], writes=[r1[1]])
            for c in range(2):
                kb.op("act", lambda e, c=c: e.activation(out=s1[c][:, :d], in_=s0[c][:, :d], func=AF.Identity),
                      reads=[r0[c]], writes=[r1[c]])
            cur = 1 - cur
        for c in range(2):
            kb.op("act", lambda e, c=c: e.activation(out=xsb[st % 4][c][:], in_=X[cur][c][:], func=AF.Identity),
                  reads=[X_res[cur][c]], writes=[xsb_res[st % 4][c]])
        if st == 0:
            dump(g, "s5_xs0", X[cur][0][:], [X_res[cur][0]])
        if st % 4 == 3:
            ct = st // 4
            for tc in range(4):
                cs = slice(tc * 512, (tc + 1) * 512)
                bank = 4 + tc % 2
                ps = g.ps[bank]
                n = 0
                for j in range(4):
                    for c, cT in enumerate((cTr, cTi)):
                        kb.op("pe", lambda e: e.matmul(ps[:], cT[:, 4 * ct + j, :], xsb[j][c][:, cs], start=(n == 0), stop=(n == 7)),
                              reads=[pres, xsb_res[j][c]], writes=[g.ps_res[bank]])
                        n += 1
                yt, yt_res = ytmp[tc % 2], ytmp_res[tc % 2]
                kb.op("dve", lambda e: e.scalar_tensor_tensor(out=yt[:], in0=uT[:, ct, cs], scalar=dsk[:, ct:ct + 1], in1=ps[:],
                                                              op0=ALU.mult, op1=ALU.add),
                      reads=[uT_res, pres, g.ps_res[bank]], writes=[yt_res])
                kb.op("act", lambda e: e.activation(out=yg[:, ct, cs], in_=yt[:], func=AF.Gelu), reads=[yt_res], writes=[yg_res[ct]])
    sg = [sb(f"s5sg{i}", [128, 512], BF16) for i in range(2)]
    sg_res = mkres(2)
    yo = [sb(f"s5yo{i}", [128, T], BF16) for i in range(2)]
    yo_res = mkres(2)
    n = 0
    for co in range(4):
        for tc in range(4):
            cs = slice(tc * 512, (tc + 1) * 512)
            bank = 6 + n % 2
            i = n % 2
            n += 1
            ps = g.ps[bank]
            for ci in range(4):
                kb.op("pe", lambda e: e.matmul(ps[:], wgl[:, ci, co * 128:(co + 1) * 128], yg[:, ci, cs], start=(ci == 0), stop=(ci == 3)),
                      reads=[pres, yg_res[ci]], writes=[g.ps_res[bank]])
            kb.op("act", lambda e: e.activation(out=sg[i][:], in_=ps[:], func=AF.Sigmoid, bias=bgl[:, co:co + 1]),
                  reads=[g.ps_res[bank], pres], writes=[sg_res[i]])
            kb.op("dve", lambda e: e.tensor_tensor(out=yo[co % 2][:, cs], in0=yg[:, co, cs], in1=sg[i][:], op=ALU.mult),
                  reads=[yg_res[co], sg_res[i]], writes=[yo_res[co % 2]])
        kb.dma("sp", g.ybs[b, co * 128:(co + 1) * 128, :], yo[co % 2][:], reads=[yo_res[co % 2]], writes=[g.ybs_res[b][co]])


def prep_inputs(inputs, core):
    f = lambda a: np.ascontiguousarray(np.asarray(a, dtype=np.float32))
    b0 = core * NB
    m = {}
    m["x"] = f(inputs["x"][b0:b0 + NB])
    m["cT"] = f(np.transpose(_pk(inputs["c"][b0:b0 + NB]), (1, 2, 0)))
    m["gmix"] = f(np.transpose(_pk(inputs["norm_mix_gain"]), (1, 0, 2)))
    m["gffn"] = f(np.transpose(_pk(inputs["norm_ffn_gain"]), (1, 0, 2)))
    m["gfin"] = f(_pk(inputs["final_gain"]))
    m["pool_scale"] = f(np.transpose(_pk(inputs["pool_scale"]), (1, 0, 2)))
    m["hg_gain"] = f(np.transpose(_pk(inputs["hg_norm_gain"]), (1, 0, 2)))
    m["hg_lbl"] = f(np.transpose(_pk(inputs["hg_lb_logits"]), (1, 0, 2)))
    return m


def s5_host_layouts(inputs):
    f = lambda a: np.ascontiguousarray(np.asarray(a, dtype=np.float32))
    L = DEPTH
    m = {}
    lre, lim, lst = f(inputs["s5_lambda_re"]), f(inputs["s5_lambda_im"]), f(inputs["s5_log_step"])
    lstf = np.broadcast_to(lst[:, :, None], (L, 32, 64))
    def s_lay(a):
        return f(a.reshape(L, 16, 2, 64).transpose(2, 3, 0, 1).reshape(128, L, 16))
    def b_lay(a):
        t = a.reshape(L, 4, 4, 2, 64).transpose(0, 2, 1, 3, 4)
        t = np.broadcast_to(t[:, :, None], (L, 4, 32, 4, 2, 64))
        return f(t.reshape(L, 128, 512))
    m["s5_lre_s"], m["s5_lim_s"], m["s5_lst_s"] = s_lay(lre), s_lay(lim), s_lay(lstf)
    m["s5_lre_b"], m["s5_lim_b"], m["s5_lst_b"] = b_lay(lre), b_lay(lim), b_lay(lstf)
    for nm, key in (("s5_bT_re", "s5_b_re"), ("s5_bT_im", "s5_b_im")):
        bb = f(inputs[key]).reshape(L, 4, 4, 2, 64, 16)
        t = bb.transpose(0, 2, 3, 5, 1, 4)
        arr = np.zeros((L, 4, 2, 16, 4, 2, 64), np.float32)
        for gp in range(2):
            arr[:, :, gp, :, :, gp, :] = t[:, :, gp]
        m[nm] = f(arr.reshape(L, 128, 512))
    for nm, key in (("s5_cT_re", "s5_c_re"), ("s5_cT_im", "s5_c_im")):
        cc = f(inputs[key]).reshape(L, 16, 2, 16, 64)
        arr = np.zeros((L, 2, 64, 16, 4, 2, 16), np.float32)
        for st in range(16):
            for gp in range(2):
                arr[:, gp, :, st, st % 4, gp, :] = cc[:, st, gp].transpose(0, 2, 1)
        m[nm] = f(arr.reshape(L, 128, 16, 128))
    m["s5_d"] = f(np.transpose(_pk(inputs["s5_d"]), (1, 0, 2)))
    m["s5_bglu"] = f(np.transpose(_pk(inputs["s5_b_glu"]), (1, 0, 2)))
    m["s5_wglu"] = f(inputs["s5_w_glu"])
    return m


def shared_inputs(inputs):
    f = lambda a: np.ascontiguousarray(np.asarray(a, dtype=np.float32))
    m = {}
    m.update(s5_host_layouts(inputs))
    for l in range(DEPTH):
        m[f"ada_w{l}"] = f(inputs["ada_w"][l])
    m["ada_b"] = f(inputs["ada_b"]).reshape(DEPTH, 1, 6 * D)
    m["w_in"] = f(inputs["w_in"])
    m["pool_w"] = f(inputs["pool_w"])
    m["w_gate"] = f(inputs["w_gate"])
    m["w_branch"] = f(inputs["w_branch"])
    m["w_out"] = f(inputs["w_out"])
    m["w_query"] = f(inputs["peer_w_query"])
    m["keysT"] = f(np.transpose(np.asarray(inputs["peer_sub_keys"]), (0, 4, 1, 2, 3)).reshape(DEPTH, 128, 16, 128))
    for l in range(DEPTH):
        m[f"peer_uT{l}"] = f(np.asarray(inputs["peer_u"][l]).T)
        m[f"peer_v{l}"] = f(inputs["peer_v"][l])
    m.update(host_consts())
    return m


def kernel(**inputs):
    nc = build()
    sh = shared_inputs(inputs)
    in_maps = []
    for c in range(8):
        m = dict(sh)
        m.update(prep_inputs(inputs, c))
        in_maps.append(m)
    res = run_bass_kernel_spmd(nc, in_maps, core_ids=list(range(8)))
    return np.concatenate([np.asarray(r["out"]) for r in res.results], axis=0)
```

```python
import numpy as np
from contextlib import ExitStack
import concourse.bass as bass
import concourse.mybir as mybir
from concourse.bass_utils import run_bass_kernel_spmd

F32 = mybir.dt.float32
BF16 = mybir.dt.bfloat16
AF = mybir.ActivationFunctionType
ALU = mybir.AluOpType

D = 2048
T = 2048
NB = 2
DEPTH = 2
EPS = 1e-6


class Res:
    __slots__ = ("w", "rs")

    def __init__(self):
        self.w = {}
        self.rs = {}


def mkres(n):
    return [Res() for _ in range(n)]


class KB:
    def __init__(self, nc):
        self.nc = nc
        self.eng = {"pe": nc.tensor, "act": nc.scalar, "dve": nc.vector, "pool": nc.gpsimd, "sp": nc.sync}
        self.sems = {}
        self.cnt = {}
        for e in self.eng:
            self.sems[e] = nc.alloc_semaphore(name=f"c_{e}")
            self.cnt[e] = 0
        self.waited = {e: {} for e in self.eng}
        self.dq = {}
        for q, n in (("sp", 12), ("pool", 2), ("act", 4)):
            lst = []
            for i in range(n):
                key = f"d_{q}{i}"
                self.sems[key] = nc.alloc_semaphore(name=key)
                lst.append([key, 0])
            self.dq[q] = [lst, 0]
        self.ninst = 0

    def _wait(self, e, deps):
        wd = self.waited[e]
        for key, val in deps.items():
            if key == e and e == "pe":
                continue
            if wd.get(key, 0) >= val:
                continue
            self.eng[e].wait_ge(self.sems[key], val)
            wd[key] = val
            self.ninst += 1

    @staticmethod
    def _deps(reads, writes):
        deps = {}
        for r in reads:
            for k, v in r.w.items():
                if deps.get(k, 0) < v:
                    deps[k] = v
        for w in writes:
            for k, v in w.w.items():
                if deps.get(k, 0) < v:
                    deps[k] = v
            for k, v in w.rs.items():
                if deps.get(k, 0) < v:
                    deps[k] = v
        return deps

    @staticmethod
    def _mark(ev, reads, writes):
        k, v = ev
        for r in reads:
            if r.rs.get(k, 0) < v:
                r.rs[k] = v
        for w in writes:
            w.w = {k: v}
            w.rs = {}

    def op(self, e, fn, reads=(), writes=()):
        self._wait(e, self._deps(reads, writes))
        self.cnt[e] += 1
        ins = fn(self.eng[e])
        ins.then_inc(self.sems[e], 1)
        self.ninst += 1
        self._mark((e, self.cnt[e]), reads, writes)

    def dma(self, q, out, in_, reads=(), writes=(), **kw):
        deps = self._deps(reads, writes)
        lst, idx = self.dq[q]
        slot = lst[idx]
        self.dq[q][1] = (idx + 1) % len(lst)
        if slot[1] > 0:
            deps[slot[0]] = max(deps.get(slot[0], 0), slot[1])
        self._wait(q, deps)
        slot[1] += 16
        self.eng[q].dma_start(out=out, in_=in_, **kw).then_inc(self.sems[slot[0]], 16)
        self.ninst += 1
        self._mark((slot[0], slot[1]), reads, writes)

    def barrier(self):
        deps = {}
        for e in self.eng:
            if self.cnt[e] > 0:
                deps[e] = self.cnt[e]
        for q in self.dq:
            for key, val in self.dq[q][0]:
                if val > 0:
                    deps[key] = val
        for e in self.eng:
            d = dict(deps)
            d.pop(e, None) if e != "pe" else None
            self._wait(e, d)

    def wait_all(self, e, ress):
        deps = {}
        for r in ress:
            for k, v in r.w.items():
                if deps.get(k, 0) < v:
                    deps[k] = v
        self._wait(e, deps)


def _pk(v):
    v = np.asarray(v)
    n = v.shape[-1] // 128
    return np.ascontiguousarray(np.swapaxes(v.reshape(v.shape[:-1] + (n, 128)), -1, -2))


def host_consts():
    c = {}
    c["ident_bf"] = np.eye(128, dtype=np.float32)
    c["ident_f32"] = np.eye(128, dtype=np.float32)
    inv = np.zeros((128, 16), np.float32)
    inv[:, :] = 1.0 / (np.arange(16, dtype=np.float32) + 1.0)
    c["invc"] = inv
    si = np.arange(128)[:, None]
    ti = np.arange(128)[None, :]
    c["mtri"] = (si < ti).astype(np.float32)
    c["triu"] = (si > ti).astype(np.float32)
    c["mle"] = (si <= ti).astype(np.float32)
    c["rmask"] = (np.arange(128)[:, None] // 32 == np.arange(4)[None, :]).astype(np.float32)
    c["ones128"] = np.ones((128, 128), np.float32)
    c["zeros128"] = np.zeros((128, 128), np.float32)
    return c


class Ctx:
    pass


class scope:
    def __init__(self, g):
        self.g = g
        self.es = ExitStack()

    def __enter__(self):
        self.es.__enter__()
        return self.es

    def __exit__(self, *a):
        if a[0] is None:
            self.g.kb.barrier()
        return self.es.__exit__(*a)


_UNIQ = [0]


def uniq(name):
    _UNIQ[0] += 1
    return f"{name}_u{_UNIQ[0]}"


def build(debug=None, nlayers=DEPTH):
    nc = bass.Bass("TRN2", target_bir_lowering=False)
    kb = KB(nc)
    g = Ctx()
    g.nc, g.kb, g.debug = nc, kb, debug

    def din(name, shape, dt=F32):
        return nc.dram_tensor(name, list(shape), dt, kind="ExternalInput").ap()

    I = {}
    I["x"] = din("x", [NB, T, D])
    I["cT"] = din("cT", [128, 16, NB])
    adaw = [din(f"ada_w{l}", [D, 6 * D]) for l in range(DEPTH)]
    I["ada_w"] = adaw
    I["ada_b"] = din("ada_b", [DEPTH, 1, 6 * D])
    I["gmix"] = din("gmix", [128, DEPTH, 16])
    I["gffn"] = din("gffn", [128, DEPTH, 16])
    I["gfin"] = din("gfin", [128, 16])
    I["w_in"] = din("w_in", [DEPTH, D, 4608])
    I["pool_w"] = din("pool_w", [DEPTH, 4, 128, 128])
    I["w_gate"] = din("w_gate", [DEPTH, 4, D, D])
    I["w_branch"] = din("w_branch", [DEPTH, 4, 512, D])
    I["w_out"] = din("w_out", [DEPTH, D, D])
    I["w_query"] = din("w_query", [DEPTH, D, D])
    I["keysT"] = din("keysT", [DEPTH, 128, 16, 128])
    I["peer_uT"] = [din(f"peer_uT{l}", [D, 16384]) for l in range(DEPTH)]
    I["peer_v"] = [din(f"peer_v{l}", [16384, D]) for l in range(DEPTH)]
    I["pool_scale"] = din("pool_scale", [128, DEPTH, 4])
    I["hg_gain"] = din("hg_gain", [128, DEPTH, 4])
    for nm in ("s5_lre_s", "s5_lim_s", "s5_lst_s"):
        I[nm] = din(nm, [128, DEPTH, 16])
    for nm in ("s5_lre_b", "s5_lim_b", "s5_lst_b", "s5_bT_re", "s5_bT_im"):
        I[nm] = din(nm, [DEPTH, 128, 512])
    for nm in ("s5_cT_re", "s5_cT_im"):
        I[nm] = din(nm, [DEPTH, 128, 16, 128])
    I["s5_d"] = din("s5_d", [128, DEPTH, 4])
    I["s5_bglu"] = din("s5_bglu", [128, DEPTH, 4])
    I["s5_wglu"] = din("s5_wglu", [DEPTH, 512, 512])
    I["hg_lbl"] = din("hg_lbl", [128, DEPTH, 4])
    I["ident_bf"] = din("ident_bf", [128, 128])
    I["ident_f32"] = din("ident_f32", [128, 128])
    I["invc"] = din("invc", [128, 16])
    I["rmask"] = din("rmask", [128, 4])
    for nm in ("mtri", "triu", "mle", "ones128", "zeros128"):
        I[nm] = din(nm, [128, 128])
    g.I = I

    out = nc.dram_tensor("out", [NB, T, D], F32, kind="ExternalOutput").ap()
    g.out = out
    dbg_kind = "ExternalOutput" if debug else "Internal"
    g.ybs = nc.dram_tensor("ybs", [NB, D, T], BF16, kind=dbg_kind).ap()
    g.ybs_res = [mkres(16) for _ in range(NB)]
    g.hTs = nc.dram_tensor("hTs", [NB, D, T], BF16, kind=dbg_kind).ap()
    g.hTs_res = [mkres(16) for _ in range(NB)]
    g.pj = nc.dram_tensor("pj", [NB, 4608, T], BF16, kind=dbg_kind).ap()
    g.pj_res = [mkres(36) for _ in range(NB)]
    g.pjf = nc.dram_tensor("pjf", [NB, 512, T], F32, kind=dbg_kind).ap()
    g.pjf_res = [mkres(4) for _ in range(NB)]
    g.pv = nc.dram_tensor("pv", [NB, 2, T, 512], BF16, kind=dbg_kind).ap()
    g.pv_res = [[mkres(16) for _ in range(2)] for _ in range(NB)]
    g.gst = nc.dram_tensor("gst", [16384, NB * T], BF16, kind=dbg_kind).ap()
    g.gst_res = [mkres(16) for _ in range(NB)]
    g.gas = nc.dram_tensor("gas", [16384, NB * T], BF16, kind=dbg_kind).ap()
    g.gas_res = [Res() for _ in range(NB * T // 1024)]
    g.vbf = nc.dram_tensor("vbf", [16384, D], BF16, kind="Internal").ap()
    g.vbf_res = mkres(16)
    g.xres = nc.dram_tensor("xres", [NB, T, D], F32, kind=dbg_kind).ap()
    g.xres_res = [mkres(16) for _ in range(NB)]

    with ExitStack() as es:
        def sb(name, shape, dt):
            return es.enter_context(nc.sbuf_tensor(uniq(name), list(shape), dt))

        g.ps = [es.enter_context(nc.psum_tensor(f"ps{i}", [128, 512], F32)) for i in range(8)]
        g.ps_res = mkres(8)
        g.ident = sb("ident", [128, 128], BF16)
        g.identf = sb("identf", [128, 128], F32)
        g.invc = sb("invc", [128, 16], F32)
        g.cres = Res()
        kb.dma("pool", g.ident[:], I["ident_bf"], writes=[g.cres])
        kb.dma("sp", g.identf[:], I["ident_f32"], writes=[g.cres])
        kb.dma("sp", g.invc[:], I["invc"], writes=[g.cres])
        g.mtri = sb("mtri", [128, 128], F32)
        g.mtri_bf = sb("mtri_bf", [128, 128], BF16)
        g.triu = sb("triu", [128, 128], F32)
        g.mle = sb("mle", [128, 128], F32)
        g.ones128 = sb("ones128", [128, 128], F32)
        g.ones_bf = sb("ones_bf", [128, 128], BF16)
        g.zeros_bf = sb("zeros_bf", [128, 128], BF16)
        kb.dma("sp", g.mtri[:], I["mtri"], writes=[g.cres])
        kb.dma("sp", g.triu[:], I["triu"], writes=[g.cres])
        kb.dma("sp", g.mle[:], I["mle"], writes=[g.cres])
        kb.dma("sp", g.ones128[:], I["ones128"], writes=[g.cres])
        kb.dma("pool", g.mtri_bf[:], I["mtri"], writes=[g.cres])
        kb.dma("pool", g.ones_bf[:], I["ones128"], writes=[g.cres])
        kb.dma("pool", g.zeros_bf[:], I["zeros128"], writes=[g.cres])
        g.rmask = sb("rmask", [128, 4], F32)
        kb.dma("sp", g.rmask[:], I["rmask"], writes=[g.cres])
        g.epsT = sb("epsT", [128, 1], F32)
        g.onesT = sb("onesT", [128, 2], F32)
        kb.op("dve", lambda e: e.memset(g.epsT[:], EPS), writes=[g.cres])
        kb.op("dve", lambda e: e.memset(g.onesT[:], 1.0), writes=[g.cres])
        g.modT = sb("modT", [128, DEPTH, 96, NB], F32)
        g.mod_res = Res()
        g.A1 = sb("A1", [128, DEPTH, 16, NB], F32)
        g.A2 = sb("A2", [128, DEPTH, 16, NB], F32)
        g.gmix = sb("gmix", [128, DEPTH, 16], F32)
        g.gffn = sb("gffn", [128, DEPTH, 16], F32)
        g.gfin = sb("gfin", [128, 16], F32)
        kb.dma("sp", g.gmix[:], I["gmix"], writes=[g.cres])
        kb.dma("sp", g.gffn[:], I["gffn"], writes=[g.cres])
        kb.dma("sp", g.gfin[:], I["gfin"], writes=[g.cres])

        g.branches = [branch_s5, branch_hg, branch_sb, branch_pool]
        g.do_merge = True
        g.do_mixer = debug != "peer"
        g.do_peer = debug in (None, "peer", "all")
        if debug and debug.startswith("br:"):
            names = debug[3:].split(",")
            g.branches = [globals()["branch_" + n] for n in names]
            g.do_merge = False
        phase_mod(g)
        if debug == "mod":
            finish_debug(g, es)
            return nc
        for l in range(nlayers):
            if g.do_mixer:
                for b in range(NB):
                    with scope(g) as es2:
                        mixer_part(g, es2, l, b)
            if g.do_peer:
                phase_peer(g, l)
        if debug is None:
            with scope(g) as esf:
                phase_final(g, esf)
        finish(g)
    return nc


def dump(g, tag, ap, res, dt=None):
    if not g.debug:
        return
    if not hasattr(g, "dumps"):
        g.dumps = {}
    if tag in g.dumps:
        return
    shp = list(ap.shape)
    d = g.nc.dram_tensor("dbg_" + tag, shp, dt or ap.dtype, kind="ExternalOutput").ap()
    r = Res()
    g.kb.dma("sp", d, ap, reads=res, writes=[r])
    g.dumps[tag] = r


def finish(g):
    kb = g.kb
    if hasattr(g, "dumps"):
        kb.wait_all("sp", list(g.dumps.values()))
    allres = []
    for lst in (g.ybs_res, g.hTs_res, g.xres_res, g.pj_res, g.pjf_res, g.pv_res[0], g.pv_res[1], g.gst_res):
        for r in lst:
            allres += r
    allres += g.gas_res
    allres += g.vbf_res
    if hasattr(g, "out_res"):
        allres += g.out_res
    kb.wait_all("sp", allres)


def finish_debug(g, es):
    kb, nc = g.kb, g.nc
    dbg = nc.dram_tensor("dbg_mod", [128, DEPTH * 96 * NB], F32, kind="ExternalOutput").ap()
    r = Res()
    kb.dma("sp", dbg, g.modT[:].rearrange("p l c b -> p (l c b)"), reads=[g.mod_res], writes=[r])
    kb.wait_all("sp", [r])


def phase_mod(g):
    nc, kb, I = g.nc, g.kb, g.I
    with scope(g) as es:
        def sb(name, shape, dt):
            return es.enter_context(nc.sbuf_tensor(uniq(name), list(shape), dt))
        condT = sb("condT", [128, 16, NB], F32)
        r_cond = Res()
        kb.dma("sp", condT[:], I["cT"], writes=[r_cond])
        kb.op("act", lambda e: e.activation(out=condT[:], in_=condT[:], func=AF.Silu), reads=[r_cond], writes=[r_cond])
        wb = [sb(f"adaw{i}", [128, 16, 512], F32) for i in range(2)]
        wb_res = mkres(2)
        ab = sb("adab", [1, DEPTH * 6 * D], F32)
        r_ab = Res()
        kb.dma("sp", ab[:], I["ada_b"].rearrange("l o c -> o (l c)"), writes=[r_ab])
        nblk = DEPTH * 24
        def load(i):
            l, blk = divmod(i, 24)
            src = I["ada_w"][l][:, blk * 512:(blk + 1) * 512].rearrange("(k p) c -> p k c", p=128)
            kb.dma("sp", wb[i % 2][:], src, writes=[wb_res[i % 2]])
        load(0)
        for i in range(nblk):
            if i + 1 < nblk:
                load(i + 1)
            l, blk = divmod(i, 24)
            w = wb[i % 2]
            bank = i % 2
            ps = g.ps[bank]
            for j in range(4):
                ct = blk * 4 + j
                for k in range(16):
                    kb.op("pe", lambda e, k=k, j=j: e.matmul(ps[:, 2 * j:2 * j + 2], w[:, k, j * 128:(j + 1) * 128], condT[:, k, :],
                                                         start=(k == 0), stop=False),
                          reads=[wb_res[i % 2], r_cond], writes=[g.ps_res[bank]])
                c0 = l * 6 * D + ct * 128
                kb.op("pe", lambda e, j=j, c0=c0: e.matmul(ps[:, 2 * j:2 * j + 2], ab[0:1, c0:c0 + 128], g.onesT[0:1, :],
                                                        start=False, stop=True),
                      reads=[r_ab, g.cres], writes=[g.ps_res[bank]])
            kb.op("act", lambda e: e.activation(out=g.modT[:, l, blk * 4:blk * 4 + 4, :],
                                                in_=ps[:, 0:8].rearrange("p (j b) -> p j b", b=NB), func=AF.Identity),
                  reads=[g.ps_res[bank]], writes=[g.mod_res])
        for l in range(DEPTH):
            kb.op("dve", lambda e, l=l: e.scalar_tensor_tensor(out=g.A1[:, l], in0=g.modT[:, l, 16:32, :], scalar=1.0,
                                                              in1=g.gmix[:, l, :].unsqueeze(2).to_broadcast([128, 16, NB]),
                                                              op0=ALU.add, op1=ALU.mult),
                  reads=[g.mod_res, g.cres], writes=[g.mod_res])
            kb.op("dve", lambda e, l=l: e.scalar_tensor_tensor(out=g.A2[:, l], in0=g.modT[:, l, 64:80, :], scalar=1.0,
                                                              in1=g.gffn[:, l, :].unsqueeze(2).to_broadcast([128, 16, NB]),
                                                              op0=ALU.add, op1=ALU.mult),
                  reads=[g.mod_res, g.cres], writes=[g.mod_res])


def norm_to_hT(g, es, xsrc, xsrc_res, hT, hT_res, Asc, Bsh):
    nc, kb = g.nc, g.kb
    def sb(name, shape, dt):
        return es.enter_context(nc.sbuf_tensor(uniq(name), list(shape), dt))
    xt = [sb(f"nx{i}", [128, D], F32) for i in range(2)]
    xt_res = mkres(2)
    xn = [sb(f"nxn{i}", [128, D], BF16) for i in range(2)]
    xn_res = mkres(2)
    junk = sb("njunk", [128, D], BF16)
    junk_res = Res()
    ss = sb("nss", [128, 4], F32)
    ss_res = mkres(2)
    def load(tt):
        kb.dma("sp", xt[tt % 2][:], xsrc[tt * 128:(tt + 1) * 128, :], reads=[xsrc_res[tt]] if xsrc_res else [], writes=[xt_res[tt % 2]])
    load(0)
    for tt in range(16):
        if tt + 1 < 16:
            load(tt + 1)
        i = tt % 2
        kb.op("act", lambda e: e.activation(out=junk[:], in_=xt[i][:], func=AF.Square, accum_out=ss[:, i:i + 1]),
              reads=[xt_res[i]], writes=[junk_res, ss_res[i]])
        kb.op("act", lambda e: e.activation(out=ss[:, 2 + i:3 + i], in_=ss[:, i:i + 1], func=AF.Sqrt, scale=1.0 / D, bias=g.epsT[:]),
              reads=[ss_res[i], g.cres], writes=[ss_res[i]])
        kb.op("dve", lambda e: e.reciprocal(out=ss[:, 2 + i:3 + i], in_=ss[:, 2 + i:3 + i]), reads=[ss_res[i]], writes=[ss_res[i]])
        kb.op("dve", lambda e: e.tensor_scalar(out=xn[i][:], in0=xt[i][:], scalar1=ss[:, 2 + i:3 + i], scalar2=None, op0=ALU.mult),
              reads=[xt_res[i], ss_res[i]], writes=[xn_res[i]])
        for half in range(2):
            bank = 6 + half
            psb = g.ps[bank][:].bitcast(BF16)
            for j in range(8):
                k = half * 8 + j
                kb.op("pe", lambda e, j=j, k=k: e.transpose(psb[:, j * 128:(j + 1) * 128], xn[i][:, k * 128:(k + 1) * 128], g.ident[:]),
                      reads=[xn_res[i], g.cres], writes=[g.ps_res[bank]])
            for j in range(8):
                k = half * 8 + j
                kb.op("act", lambda e, j=j, k=k: e.activation(out=hT[:, k, tt * 128:(tt + 1) * 128], in_=psb[:, j * 128:(j + 1) * 128],
                                                               func=AF.Identity, scale=Asc(k), bias=Bsh(k)),
                      reads=[g.ps_res[bank], g.mod_res], writes=[hT_res])


def load_wblock(g, q, dst, dst_res, src2d):
    g.kb.dma(q, dst, src2d.rearrange("(k p) c -> p k c", p=128), writes=[dst_res])


def proj_fm(g, wblk, wblk_res, hT, hT_res, ncol_tiles, evac):
    kb = g.kb
    n = 0
    for j in range(ncol_tiles):
        for tc in range(4):
            bank = n % 4
            n += 1
            ps = g.ps[bank]
            for k in range(16):
                kb.op("pe", lambda e, k=k: e.matmul(ps[:], wblk[:, k, j * 128:(j + 1) * 128], hT[:, k, tc * 512:(tc + 1) * 512],
                                                  start=(k == 0), stop=(k == 15)),
                      reads=[wblk_res, hT_res], writes=[g.ps_res[bank]])
            evac(j, tc, ps, g.ps_res[bank])


def mixer_part(g, es, l, b):
    nc, kb, I = g.nc, g.kb, g.I
    with scope(g) as es1:
        phase_proj(g, es1, l, b)
    for br in g.branches:
        with scope(g) as es2:
            br(g, es2, l, b)
    if g.do_merge:
        with scope(g) as es3:
            phase_merge(g, es3, l, b)


def phase_proj(g, es, l, b):
    nc, kb, I = g.nc, g.kb, g.I
    def sb(name, shape, dt):
        return es.enter_context(nc.sbuf_tensor(uniq(name), list(shape), dt))
    hT = sb("hT", [128, 16, T], BF16)
    hT_res = Res()
    if l == 0:
        xsrc, xsrc_res = I["x"][b], None
    else:
        xsrc, xsrc_res = g.xres[b], g.xres_res[b]
    with scope(g) as es2:
        norm_to_hT(g, es2, xsrc, xsrc_res, hT, hT_res,
                   lambda k: g.A1[:, l, k, b:b + 1], lambda k: g.modT[:, l, k, b:b + 1])
    for k in range(16):
        kb.dma("sp", g.hTs[b, k * 128:(k + 1) * 128, :], hT[:, k, :], reads=[hT_res], writes=[g.hTs_res[b][k]])
    wblk = [sb(f"wblk{i}", [128, 16, 512], BF16) for i in range(2)]
    wblk_res = mkres(2)
    stg = [sb(f"pstg{i}", [128, T], BF16) for i in range(2)]
    stg_res = mkres(2)
    stgf = [sb(f"pstgf{i}", [128, T], F32) for i in range(2)]
    stgf_res = mkres(2)
    stgv = [sb(f"pstgv{i}", [128, 512], BF16) for i in range(2)]
    stgv_res = mkres(2)
    def wload(cb):
        load_wblock(g, "pool", wblk[cb % 2][:], wblk_res[cb % 2], I["w_in"][l, :, cb * 512:(cb + 1) * 512])
    wload(0)
    nst = 0
    for cb in range(9):
        if cb + 1 < 9:
            wload(cb + 1)
        w, w_res = wblk[cb % 2], wblk_res[cb % 2]
        if cb in (3, 7):
            which = 0 if cb == 3 else 1
            for tt in range(16):
                bank = tt % 4
                ps = g.ps[bank]
                for k in range(16):
                    kb.op("pe", lambda e, k=k: e.matmul(ps[:], hT[:, k, tt * 128:(tt + 1) * 128], w[:, k, :], start=(k == 0), stop=(k == 15)),
                          reads=[w_res, hT_res], writes=[g.ps_res[bank]])
                sv, sv_res = stgv[tt % 2], stgv_res[tt % 2]
                kb.op("act", lambda e: e.activation(out=sv[:], in_=ps[:], func=AF.Identity), reads=[g.ps_res[bank]], writes=[sv_res])
                kb.dma("sp", g.pv[b, which, tt * 128:(tt + 1) * 128, :], sv[:], reads=[sv_res], writes=[g.pv_res[b][which][tt]])
            continue
        for j in range(4):
            if cb == 2:
                st, st_res = stgf[j % 2], stgf_res[j % 2]
            else:
                st, st_res = stg[nst % 2], stg_res[nst % 2]
                nst += 1
            for tc in range(4):
                bank = (j * 4 + tc) % 4
                ps = g.ps[bank]
                for k in range(16):
                    kb.op("pe", lambda e, k=k: e.matmul(ps[:], w[:, k, j * 128:(j + 1) * 128], hT[:, k, tc * 512:(tc + 1) * 512],
                                                      start=(k == 0), stop=(k == 15)),
                          reads=[w_res, hT_res], writes=[g.ps_res[bank]])
                kb.op("act", lambda e: e.activation(out=st[:, tc * 512:(tc + 1) * 512], in_=ps[:], func=AF.Identity),
                      reads=[g.ps_res[bank]], writes=[st_res])
            if cb == 2:
                kb.dma("sp", g.pjf[b, j * 128:(j + 1) * 128, :], st[:], reads=[st_res], writes=[g.pjf_res[b][j]])
            else:
                ct = cb * 4 + j
                kb.dma("sp", g.pj[b, ct * 128:(ct + 1) * 128, :], st[:], reads=[st_res], writes=[g.pj_res[b][ct]])


def phase_final(g, es):
    nc, kb, I = g.nc, g.kb, g.I
    def sb(name, shape, dt):
        return es.enter_context(nc.sbuf_tensor(uniq(name), list(shape), dt))
    gbc = sb("fn_g", [128, D], F32)
    gbc_res = Res()
    build_rowbcast(g, sb, gbc, gbc_res, lambda k: g.gfin[:, k:k + 1])
    xt = [sb(f"fn_x{i}", [128, D], F32) for i in range(2)]
    xt_res = mkres(2)
    yt = [sb(f"fn_y{i}", [128, D], F32) for i in range(2)]
    yt_res = mkres(2)
    junk = sb("fn_junk", [128, D], BF16)
    junk_res = Res()
    ss = sb("fn_ss", [128, 4], F32)
    ss_res = mkres(2)
    g.out_res = mkres(NB * 16)
    n = 0
    for b in range(NB):
        for tt in range(16):
            i = n % 2
            n += 1
            rows = slice(tt * 128, (tt + 1) * 128)
            kb.dma("sp", xt[i][:], g.xres[b, rows, :], reads=[g.xres_res[b][tt]], writes=[xt_res[i]])
            kb.op("act", lambda e: e.activation(out=junk[:], in_=xt[i][:], func=AF.Square, accum_out=ss[:, i:i + 1]),
                  reads=[xt_res[i]], writes=[junk_res, ss_res[i]])
            kb.op("act", lambda e: e.activation(out=ss[:, 2 + i:3 + i], in_=ss[:, i:i + 1], func=AF.Sqrt, scale=1.0 / D, bias=g.epsT[:]),
                  reads=[ss_res[i], g.cres], writes=[ss_res[i]])
            kb.op("dve", lambda e: e.reciprocal(out=ss[:, 2 + i:3 + i], in_=ss[:, 2 + i:3 + i]), reads=[ss_res[i]], writes=[ss_res[i]])
            kb.op("dve", lambda e: e.scalar_tensor_tensor(out=yt[i][:], in0=xt[i][:], scalar=ss[:, 2 + i:3 + i], in1=gbc[:],
                                                          op0=ALU.mult, op1=ALU.mult),
                  reads=[xt_res[i], ss_res[i], gbc_res], writes=[yt_res[i]])
            kb.dma("sp", g.out[b, rows, :], yt[i][:], reads=[yt_res[i]], writes=[g.out_res[b * 16 + tt]])


def build_rowbcast(g, sb, dst, dst_res, colfn):
    kb = g.kb
    dg = [sb(f"rb_dg{i}", [128, 128], F32) for i in range(2)]
    dg_res = mkres(2)
    for k in range(16):
        i = k % 2
        kb.op("dve", lambda e: e.tensor_scalar(out=dg[i][:], in0=g.identf[:], scalar1=colfn(k), scalar2=None, op0=ALU.mult),
              reads=[g.cres, g.mod_res], writes=[dg_res[i]])
        kk = k % 4
        kb.op("pe", lambda e: e.matmul(g.ps[6][:, kk * 128:(kk + 1) * 128], g.ones128[:], dg[i][:], start=True, stop=True),
              reads=[g.cres, dg_res[i]], writes=[g.ps_res[6]])
        if kk == 3:
            k0 = k - 3
            kb.op("act", lambda e: e.activation(out=dst[:, k0 * 128:(k0 + 4) * 128], in_=g.ps[6][:], func=AF.Identity),
                  reads=[g.ps_res[6]], writes=[dst_res])


def phase_merge(g, es, l, b):
    nc, kb, I = g.nc, g.kb, g.I
    def sb(name, shape, dt):
        return es.enter_context(nc.sbuf_tensor(uniq(name), list(shape), dt))
    TCH = 1024
    NTC = TCH // 512
    g1bc = sb("mg_g1", [128, D], F32)
    g1_res = Res()
    build_rowbcast(g, sb, g1bc, g1_res, lambda k: g.modT[:, l, 32 + k, b:b + 1])
    hTc = sb("mg_hT", [128, 16, TCH], BF16)
    ybc = sb("mg_yb", [128, 16, TCH], BF16)
    mrg = sb("mg_mrg", [128, 16, TCH], BF16)
    acc = sb("mg_acc", [128, 4, TCH], F32)
    hTc_res, ybc_res = Res(), Res()
    mrg_res = mkres(16)
    acc_res = mkres(4)
    wg = [sb(f"mg_wg{i}", [128, 16, 512], BF16) for i in range(2)]
    wg_res = mkres(2)
    wbr = [sb(f"mg_wb{i}", [128, 4, 512], BF16) for i in range(2)]
    wbr_res = mkres(2)
    sgm = [sb(f"mg_sg{i}", [128, 512], F32) for i in range(2)]
    sgm_res = mkres(2)
    tmp = [sb(f"mg_tmp{i}", [128, 512], F32) for i in range(2)]
    tmp_res = mkres(2)
    xt = [sb(f"mg_xt{i}", [128, 512], F32) for i in range(2)]
    xt_res = mkres(2)
    xsrc = I["x"][b] if l == 0 else g.xres[b]
    for ch in range(T // TCH):
        tsl = slice(ch * TCH, (ch + 1) * TCH)
        kb.dma("sp", hTc[:], g.hTs[b, :, tsl].rearrange("(k p) t -> p k t", p=128), reads=g.hTs_res[b], writes=[hTc_res])
        kb.dma("sp", ybc[:], g.ybs[b, :, tsl].rearrange("(k p) t -> p k t", p=128), reads=g.ybs_res[b], writes=[ybc_res])
        nw = 0
        def wload(cb, n, i):
            kb.dma("pool", wg[i][:], I["w_gate"][l, n, :, cb * 512:(cb + 1) * 512].rearrange("(k p) c -> p k c", p=128), writes=[wg_res[i]])
            kb.dma("pool", wbr[i][:], I["w_branch"][l, n, :, cb * 512:(cb + 1) * 512].rearrange("(k p) c -> p k c", p=128), writes=[wbr_res[i]])
        seq = [(cb, n) for cb in range(4) for n in range(4)]
        wload(seq[0][0], seq[0][1], 0)
        cnt = 0
        for si, (cb, n) in enumerate(seq):
            wi = si % 2
            if si + 1 < len(seq):
                wload(seq[si + 1][0], seq[si + 1][1], (si + 1) % 2)
            for j in range(4):
                dt_ = cb * 4 + j
                for tcc in range(NTC):
                    cs = slice(tcc * 512, (tcc + 1) * 512)
                    i = cnt % 2
                    cnt += 1
                    psg, psb = g.ps[i], g.ps[2 + i]
                    for k in range(16):
                        kb.op("pe", lambda e, k=k: e.matmul(psg[:], wg[wi][:, k, j * 128:(j + 1) * 128], hTc[:, k, cs], start=(k == 0), stop=(k == 15)),
                              reads=[wg_res[wi], hTc_res], writes=[g.ps_res[i]])
                    for kk in range(4):
                        kb.op("pe", lambda e, kk=kk: e.matmul(psb[:], wbr[wi][:, kk, j * 128:(j + 1) * 128], ybc[:, 4 * n + kk, cs],
                                                            start=(kk == 0), stop=(kk == 3)),
                              reads=[wbr_res[wi], ybc_res], writes=[g.ps_res[2 + i]])
                    kb.op("act", lambda e: e.activation(out=sgm[i][:], in_=psg[:], func=AF.Sigmoid), reads=[g.ps_res[i]], writes=[sgm_res[i]])
                    if n == 0:
                        kb.op("dve", lambda e: e.tensor_tensor(out=acc[:, j, cs], in0=sgm[i][:], in1=psb[:], op=ALU.mult),
                              reads=[sgm_res[i], g.ps_res[2 + i]], writes=[acc_res[j]])
                    else:
                        kb.op("dve", lambda e: e.tensor_tensor(out=tmp[i][:], in0=sgm[i][:], in1=psb[:], op=ALU.mult),
                              reads=[sgm_res[i], g.ps_res[2 + i]], writes=[tmp_res[i]])
                        if n < 3:
                            kb.op("pool", lambda e: e.tensor_tensor(out=acc[:, j, cs], in0=acc[:, j, cs], in1=tmp[i][:], op=ALU.add),
                                  reads=[tmp_res[i], acc_res[j]], writes=[acc_res[j]])
                        else:
                            kb.op("pool", lambda e: e.tensor_tensor(out=mrg[:, dt_, cs], in0=acc[:, j, cs], in1=tmp[i][:], op=ALU.add),
                                  reads=[tmp_res[i], acc_res[j]], writes=[mrg_res[dt_]])
        if ch == 0 and b == 0:
            dump(g, "mg_mrg", mrg[:], mrg_res)
        def woload(dc, i):
            kb.dma("pool", wg[i][:], I["w_out"][l, :, dc * 512:(dc + 1) * 512].rearrange("(k p) c -> p k c", p=128), writes=[wg_res[i]])
        woload(0, 0)
        cnt = 0
        for dc in range(4):
            if dc + 1 < 4:
                woload(dc + 1, (dc + 1) % 2)
            wi = dc % 2
            dsl = slice(dc * 512, (dc + 1) * 512)
            for tt in range(TCH // 128):
                i = cnt % 2
                cnt += 1
                tg = ch * (TCH // 128) + tt
                rows = slice(tg * 128, (tg + 1) * 128)
                kb.dma("sp", xt[i][:], xsrc[rows, dsl], reads=([g.xres_res[b][tg]] if l > 0 else []), writes=[xt_res[i]])
                ps = g.ps[4 + i]
                for k in range(16):
                    kb.op("pe", lambda e, k=k: e.matmul(ps[:], mrg[:, k, tt * 128:(tt + 1) * 128], wg[wi][:, k, :], start=(k == 0), stop=(k == 15)),
                          reads=[mrg_res[k], wg_res[wi]], writes=[g.ps_res[4 + i]])
                kb.op("dve", lambda e: e.tensor_tensor(out=tmp[i][:], in0=ps[:], in1=g1bc[:, dsl], op=ALU.mult),
                      reads=[g.ps_res[4 + i], g1_res], writes=[tmp_res[i]])
                kb.op("dve", lambda e: e.tensor_tensor(out=xt[i][:], in0=xt[i][:], in1=tmp[i][:], op=ALU.add),
                      reads=[tmp_res[i], xt_res[i]], writes=[xt_res[i]])
                kb.dma("sp", g.xres[b, rows, dsl], xt[i][:], reads=[xt_res[i]], writes=[g.xres_res[b][tg]])


def phase_peer(g, l):
    for blk in range(16):
        g.kb.dma("pool", g.vbf[blk * 1024:(blk + 1) * 1024, :], g.I["peer_v"][l][blk * 1024:(blk + 1) * 1024, :], writes=[g.vbf_res[blk]])
    for b in range(NB):
        with scope(g) as es:
            peer_route(g, es, l, b)
    for tg in range(NB * T // 1024):
        with scope(g) as es:
            peer_x(g, es, l, tg)
    with scope(g) as es:
        peer_y(g, es, l)


def peer_route(g, es, l, b):
    nc, kb, I = g.nc, g.kb, g.I
    def sb(name, shape, dt):
        return es.enter_context(nc.sbuf_tensor(uniq(name), list(shape), dt))
    NEG = -1e30
    qT = sb("pr_qT", [128, 16, T], BF16)
    qT_res = mkres(16)
    xsrc, xsrc_res = g.xres[b], g.xres_res[b]
    if g.debug == "peer":
        xsrc, xsrc_res = I["x"][b], None
    with scope(g) as es1:
        def sb1(name, shape, dt):
            return es1.enter_context(nc.sbuf_tensor(uniq(name), list(shape), dt))
        hT = sb1("pr_hT", [128, 16, T], BF16)
        hT_res = Res()
        with scope(g) as es2:
            norm_to_hT(g, es2, xsrc, xsrc_res, hT, hT_res,
                       lambda k: g.A2[:, l, k, b:b + 1], lambda k: g.modT[:, l, 48 + k, b:b + 1])
        for k in range(16):
            kb.dma("sp", g.hTs[b, k * 128:(k + 1) * 128, :], hT[:, k, :], reads=[hT_res], writes=[g.hTs_res[b][k]])
        wblk = [sb1(f"pr_w{i}", [128, 16, 512], BF16) for i in range(2)]
        wblk_res = mkres(2)
        def wload(cb):
            load_wblock(g, "pool", wblk[cb % 2][:], wblk_res[cb % 2], I["w_query"][l, :, cb * 512:(cb + 1) * 512])
        wload(0)
        for cb in range(4):
            if cb + 1 < 4:
                wload(cb + 1)
            w, w_res = wblk[cb % 2], wblk_res[cb % 2]
            for j in range(4):
                ct = cb * 4 + j
                for tc in range(4):
                    bank = (j * 4 + tc) % 4
                    ps = g.ps[bank]
                    for k in range(16):
                        kb.op("pe", lambda e, k=k: e.matmul(ps[:], w[:, k, j * 128:(j + 1) * 128], hT[:, k, tc * 512:(tc + 1) * 512],
                                                          start=(k == 0), stop=(k == 15)),
                              reads=[w_res, hT_res], writes=[g.ps_res[bank]])
                    kb.op("act", lambda e: e.activation(out=qT[:, ct, tc * 512:(tc + 1) * 512], in_=ps[:], func=AF.Identity),
                          reads=[g.ps_res[bank]], writes=[qT_res[ct]])
    keysT = sb("pr_keys", [128, 16, 128], BF16)
    keys_res = Res()
    kb.dma("pool", keysT[:], I["keysT"][l], writes=[keys_res])
    sc = sb("pr_sc", [128, 16, 128], F32)
    sc2 = sb("pr_sc2", [128, 16, 128], F32)
    top = sb("pr_top", [128, 16, 16], F32)
    cand = sb("pr_cand", [128, 8, 256], F32)
    cand2 = sb("pr_cand2", [128, 8, 256], F32)
    best = sb("pr_best", [128, 8, 24], F32)
    sm = sb("pr_sm", [128, 8, 8], F32)
    ex = sb("pr_ex", [128, 8, 16], F32)
    r_sc, r_sc2, r_e, r_top, r_cand, r_cand2, r_best, r_sm, r_ex = mkres(9)
    NPB = 4
    P = [sb(f"pr_P{i}", [128, 8, 128], F32) for i in range(NPB)]
    P_res = mkres(NPB)
    Gh = [sb(f"pr_G{i}", [128, 1024], BF16) for i in range(NPB)]
    Gh_res = mkres(NPB)
    GT = [sb(f"pr_GT{i}", [128, 8, 128], BF16) for i in range(2)]
    GT_res = mkres(2)
    sc4 = sc[:].rearrange("p (h two) k -> p h two k", two=2)
    E = [sb(f"pr_E{i}", [128, 1024], BF16) for i in range(4)]
    E_res = mkres(4)
    top4 = top[:].rearrange("p (h two) k -> p h two k", two=2)
    for tt in range(16):
        tsl = slice(tt * 128, (tt + 1) * 128)
        for q4 in range(4):
            bank = q4 % 2
            ps = g.ps[bank]
            for jj in range(4):
                hp = q4 * 4 + jj
                kb.op("pe", lambda e, hp=hp, jj=jj: e.matmul(ps[:, jj * 128:(jj + 1) * 128], qT[:, hp, tsl], keysT[:, hp, :], start=True, stop=True),
                      reads=[qT_res[hp], keys_res], writes=[g.ps_res[bank]])
            kb.op("act", lambda e: e.activation(out=sc[:, q4 * 4:(q4 + 1) * 4, :], in_=ps[:].rearrange("p (a k) -> p a k", k=128), func=AF.Identity),
                  reads=[g.ps_res[bank]], writes=[r_sc])
        for hp in range(16):
            kb.op("dve", lambda e, hp=hp: e.max(out=top[:, hp, 0:8], in_=sc[:, hp, :]), reads=[r_sc], writes=[r_top])
            kb.op("dve", lambda e, hp=hp: e.match_replace(out=sc2[:, hp, :], in_to_replace=top[:, hp, 0:8], in_values=sc[:, hp, :], imm_value=NEG),
                  reads=[r_sc, r_top], writes=[r_sc2])
            kb.op("dve", lambda e, hp=hp: e.max(out=top[:, hp, 8:16], in_=sc2[:, hp, :]), reads=[r_sc2], writes=[r_top])
        kb.op("dve", lambda e: e.tensor_tensor(out=cand[:].rearrange("p h (i j) -> p h i j", j=16),
                                               in0=top4[:, :, 0, :].unsqueeze(3).to_broadcast([128, 8, 16, 16]),
                                               in1=top4[:, :, 1, :].unsqueeze(2).to_broadcast([128, 8, 16, 16]), op=ALU.add),
              reads=[r_top], writes=[r_cand])
        for h in range(8):
            kb.op("dve", lambda e, h=h: e.max(out=best[:, h, 0:8], in_=cand[:, h, :]), reads=[r_cand], writes=[r_best])
            kb.op("dve", lambda e, h=h: e.match_replace(out=cand2[:, h, :], in_to_replace=best[:, h, 0:8], in_values=cand[:, h, :], imm_value=NEG),
                  reads=[r_cand, r_best], writes=[r_cand2])
            kb.op("dve", lambda e, h=h: e.max(out=best[:, h, 8:16], in_=cand2[:, h, :]), reads=[r_cand2], writes=[r_best])
        kb.op("dve", lambda e: e.tensor_tensor(out=ex[:], in0=best[:, :, 0:16], in1=best[:, :, 0:1].to_broadcast([128, 8, 16]), op=ALU.subtract),
              reads=[r_best], writes=[r_ex])
        kb.op("act", lambda e: e.activation(out=ex[:], in_=ex[:], func=AF.Exp), reads=[r_ex], writes=[r_ex])
        kb.op("dve", lambda e: e.tensor_reduce(out=sm[:, :, 0], in_=ex[:], axis=mybir.AxisListType.X, op=ALU.add), reads=[r_ex], writes=[r_sm])
        kb.op("act", lambda e: e.activation(out=sm[:, :, 0], in_=sm[:, :, 0], func=AF.Ln), reads=[r_sm], writes=[r_sm])
        kb.op("dve", lambda e: e.scalar_tensor_tensor(out=sm[:, :, 2], in0=sm[:, :, 0], scalar=-1.0, in1=best[:, :, 0], op0=ALU.mult, op1=ALU.subtract),
              reads=[r_best, r_sm], writes=[r_sm])
        kb.op("dve", lambda e: e.tensor_copy(out=sm[:, :, 1], in_=best[:, :, 15]), reads=[r_best, r_sm], writes=[r_sm])
        if tt == 0 and b == 0:
            dump(g, "pr_sc", sc[:], [r_sc]); dump(g, "pr_best", best[:], [r_best]); dump(g, "pr_sm", sm[:], [r_sm])
        n = 0
        pending = []
        for ib in range(16):
            gi = ib % 2
            banks = (2 + 2 * gi, 3 + 2 * gi)
            for bk in banks:
                kb.op("pe", lambda e, bk=bk: e.matmul(g.ps[bk][:], g.zeros_bf[:], qT[:, 0, 0:512], start=True, stop=True),
                      reads=[g.cres, qT_res[0]], writes=[g.ps_res[bk]])
            for h in range(8):
                i = n % NPB
                n += 1
                kb.op("pool", lambda e: e.tensor_tensor(out=P[i][:], in0=sc4[:, h, 0, ib * 8:(ib + 1) * 8].unsqueeze(2).to_broadcast([128, 8, 128]),
                                                        in1=sc4[:, h, 1, :].unsqueeze(1).to_broadcast([128, 8, 128]), op=ALU.add),
                      reads=[r_sc], writes=[P_res[i]])
                P2 = P[i][:].rearrange("p a k -> p (a k)")
                kb.op("act", lambda e: e.activation(out=E[i][:], in_=P2, func=AF.Exp, bias=sm[:, h, 2:3]), reads=[P_res[i], r_sm], writes=[E_res[i]])
                kb.op("dve", lambda e: e.scalar_tensor_tensor(out=Gh[i][:], in0=P2, scalar=sm[:, h, 1:2], in1=E[i][:], op0=ALU.is_ge, op1=ALU.mult),
                      reads=[P_res[i], E_res[i], r_sm], writes=[Gh_res[i]])
                for ii in range(8):
                    bk = banks[ii // 4]
                    kb.op("pe", lambda e, ii=ii, bk=bk: e.matmul(g.ps[bk][:, (ii % 4) * 128:(ii % 4 + 1) * 128], Gh[i][:, ii * 128:(ii + 1) * 128],
                                                               g.ident[:], start=False, stop=True),
                          reads=[Gh_res[i], g.cres], writes=[g.ps_res[bk]])
                if h == 5 and pending:
                    pending.pop(0)()
            def evac(ib=ib, gi=gi, banks=banks):
                for half, bk in enumerate(banks):
                    kb.op("act", lambda e, half=half, bk=bk: e.activation(out=GT[gi][:, half * 4:(half + 1) * 4, :],
                                                                          in_=g.ps[bk][:].rearrange("p (a t) -> p a t", t=128), func=AF.Identity),
                          reads=[g.ps_res[bk]], writes=[GT_res[gi]])
                dst = g.gst[ib * 1024:(ib + 1) * 1024, b * T + tt * 128:b * T + (tt + 1) * 128].rearrange("(a j) t -> j a t", j=128)
                kb.dma("sp", dst, GT[gi][:], reads=[GT_res[gi]], writes=[g.gst_res[b][tt]])
            pending.append(evac)
        while pending:
            pending.pop(0)()


def peer_x(g, es, l, tg):
    nc, kb, I = g.nc, g.kb, g.I
    def sb(name, shape, dt):
        return es.enter_context(nc.sbuf_tensor(uniq(name), list(shape), dt))
    b, half = divmod(tg, 2)
    tsl = slice(half * 1024, (half + 1) * 1024)
    gsl = slice(tg * 1024, (tg + 1) * 1024)
    h2c = sb("px_h2", [128, 16, 1024], BF16)
    h2_res = Res()
    kb.dma("sp", h2c[:], g.hTs[b, :, tsl].rearrange("(k p) t -> p k t", p=128), reads=g.hTs_res[b], writes=[h2_res])
    ublk = [sb(f"px_u{i}", [128, 16, 512], BF16) for i in range(2)]
    ublk_res = mkres(2)
    gt = [sb(f"px_gt{i}", [128, 1024], BF16) for i in range(2)]
    gt_res = mkres(2)
    ga = [sb(f"px_ga{i}", [128, 512], F32) for i in range(2)]
    ga_res = mkres(2)
    go = [sb(f"px_go{i}", [128, 1024], BF16) for i in range(2)]
    go_res = mkres(2)
    gst_deps = g.gst_res[b][half * 8:(half + 1) * 8]
    def uload(eb):
        load_wblock(g, "pool", ublk[eb % 2][:], ublk_res[eb % 2], I["peer_uT"][l][:, eb * 512:(eb + 1) * 512])
    uload(0)
    n = 0
    for eb in range(32):
        if eb + 1 < 32:
            uload(eb + 1)
        u, u_res = ublk[eb % 2], ublk_res[eb % 2]
        for ec in range(4):
            e0 = (eb * 4 + ec) * 128
            i = (eb * 4 + ec) % 2
            kb.dma("sp", gt[i][:], g.gst[e0:e0 + 128, gsl], reads=gst_deps, writes=[gt_res[i]])
            for tcc in range(2):
                cs = slice(tcc * 512, (tcc + 1) * 512)
                j = n % 2
                n += 1
                ps = g.ps[j]
                for k in range(16):
                    kb.op("pe", lambda e, k=k: e.matmul(ps[:], u[:, k, ec * 128:(ec + 1) * 128], h2c[:, k, cs], start=(k == 0), stop=(k == 15)),
                          reads=[u_res, h2_res], writes=[g.ps_res[j]])
                kb.op("act", lambda e: e.activation(out=ga[j][:], in_=ps[:], func=AF.Gelu), reads=[g.ps_res[j]], writes=[ga_res[j]])
                kb.op("dve", lambda e: e.tensor_tensor(out=go[i][:, cs], in0=ga[j][:], in1=gt[i][:, cs], op=ALU.mult),
                      reads=[ga_res[j], gt_res[i]], writes=[go_res[i]])
            kb.dma("sp", g.gas[e0:e0 + 128, gsl], go[i][:], reads=[go_res[i]], writes=[g.gas_res[tg]])


def peer_y(g, es, l):
    nc, kb, I = g.nc, g.kb, g.I
    def sb(name, shape, dt):
        return es.enter_context(nc.sbuf_tensor(uniq(name), list(shape), dt))
    EC = 8
    g2bc = [sb(f"py_g2{b}", [128, D], F32) for b in range(NB)]
    g2_res = mkres(NB)
    with scope(g) as es1:
        def sb1(name, shape, dt):
            return es1.enter_context(nc.sbuf_tensor(uniq(name), list(shape), dt))
        for b in range(NB):
            build_rowbcast(g, sb1, g2bc[b], g2_res[b], lambda k, b=b: g.modT[:, l, 80 + k, b:b + 1])
    gab = [sb(f"py_ga{i}", [128, EC, 512], BF16) for i in range(2)]
    gab_res = mkres(2)
    vb = [sb(f"py_v{i}", [128, EC, 512], BF16) for i in range(2)]
    vb_res = mkres(2)
    xt = [sb(f"py_xt{i}", [128, 512], F32) for i in range(2)]
    xt_res = mkres(2)
    tmp = [sb(f"py_tmp{i}", [128, 512], F32) for i in range(2)]
    tmp_res = mkres(2)
    NBLK = 128 // EC
    for grp in range(NB * T // 512):
        b = grp // 4
        gsl = slice(grp * 512, (grp + 1) * 512)
        for dq in range(4):
            dsl = slice(dq * 512, (dq + 1) * 512)
            def load(blk):
                i = blk % 2
                e0 = blk * EC * 128
                kb.dma("sp", gab[i][:], g.gas[e0:e0 + EC * 128, gsl].rearrange("(c p) t -> p c t", p=128), reads=[g.gas_res[grp // 2]], writes=[gab_res[i]])
                kb.dma("sp", vb[i][:], g.vbf[e0:e0 + EC * 128, dsl].rearrange("(c p) d -> p c d", p=128), reads=[g.vbf_res[blk]], writes=[vb_res[i]])
            load(0)
            for blk in range(NBLK):
                if blk + 1 < NBLK:
                    load(blk + 1)
                i = blk % 2
                for c in range(EC):
                    first = (blk == 0 and c == 0)
                    last = (blk == NBLK - 1 and c == EC - 1)
                    for tt in range(4):
                        kb.op("pe", lambda e, tt=tt, c=c: e.matmul(g.ps[tt][:], gab[i][:, c, tt * 128:(tt + 1) * 128], vb[i][:, c, :], start=first, stop=last),
                              reads=[gab_res[i], vb_res[i]], writes=[g.ps_res[tt]])
            for tt in range(4):
                tg_ = (grp % 4) * 4 + tt
                rows = slice(tg_ * 128, (tg_ + 1) * 128)
                i = tt % 2
                kb.dma("sp", xt[i][:], g.xres[b, rows, dsl], reads=[g.xres_res[b][tg_]], writes=[xt_res[i]])
                kb.op("dve", lambda e: e.tensor_tensor(out=tmp[i][:], in0=g.ps[tt][:], in1=g2bc[b][:, dsl], op=ALU.mult),
                      reads=[g.ps_res[tt], g2_res[b]], writes=[tmp_res[i]])
                kb.op("dve", lambda e: e.tensor_tensor(out=xt[i][:], in0=xt[i][:], in1=tmp[i][:], op=ALU.add),
                      reads=[tmp_res[i], xt_res[i]], writes=[xt_res[i]])
                kb.dma("sp", g.xres[b, rows, dsl], xt[i][:], reads=[xt_res[i]], writes=[g.xres_res[b][tg_]])


def branch_pool(g, es, l, b):
    nc, kb, I = g.nc, g.kb, g.I
    def sb(name, shape, dt):
        return es.enter_context(nc.sbuf_tensor(uniq(name), list(shape), dt))
    pT = sb("pl_p", [128, 4, T], BF16)
    pT_res = mkres(4)
    pw = sb("pl_w", [128, 4, 128], BF16)
    pw_res = Res()
    psc = sb("pl_sc", [128, 4], F32)
    kb.dma("pool", pw[:], I["pool_w"][l].rearrange("g c d -> c g d"), writes=[pw_res])
    kb.dma("sp", psc[:], I["pool_scale"][:, l, :], writes=[pw_res])
    for gi in range(4):
        kb.dma("sp", pT[:, gi, :], g.pj[b, (32 + gi) * 128:(33 + gi) * 128, :], reads=[g.pj_res[b][32 + gi]], writes=[pT_res[gi]])
    dump(g, "pl_pT", pT[:], pT_res)
    dump(g, "pl_pw", pw[:], [pw_res])
    dump(g, "pl_psc", psc[:], [pw_res])
    sa = sb("pl_sa", [128, T], F32)
    sbb = sb("pl_sb", [128, T], F32)
    s_res = mkres(2)
    pooled = sb("pl_pooled", [128, T], BF16)
    pooled_res = Res()
    tmp = sb("pl_tmp", [128, 16], F32)
    tmp_res = Res()
    yo = [sb(f"pl_yo{i}", [128, T], BF16) for i in range(2)]
    yo_res = mkres(2)
    for gi in range(4):
        wlen = 2 ** (gi + 1)
        cur, cur_res = pT[:, gi, :], pT_res[gi]
        bufs = [(sa, s_res[0]), (sbb, s_res[1])]
        for lev in range(gi + 1):
            d = 2 ** lev
            dst, dst_res = bufs[lev % 2]
            kb.op("dve", lambda e, cur=cur, dst=dst, d=d: e.tensor_tensor(out=dst[:, d:], in0=cur[:, d:], in1=cur[:, :T - d], op=ALU.add),
                  reads=[cur_res], writes=[dst_res])
            kb.op("dve", lambda e, cur=cur, dst=dst, d=d: e.tensor_copy(out=dst[:, :d], in_=cur[:, :d]),
                  reads=[cur_res], writes=[dst_res])
            cur, cur_res = dst[:], dst_res
        kb.op("dve", lambda e, cur=cur: e.scalar_tensor_tensor(out=pooled[:], in0=cur, scalar=1.0 / wlen, in1=pT[:, gi, :],
                                                              op0=ALU.mult, op1=ALU.subtract),
              reads=[cur_res, pT_res[gi]], writes=[pooled_res])
        n = wlen - 1
        kb.op("dve", lambda e, cur=cur, n=n: e.tensor_tensor(out=tmp[:, :n], in0=cur[:, :n], in1=g.invc[:, :n], op=ALU.mult),
              reads=[cur_res, g.cres], writes=[tmp_res])
        kb.op("dve", lambda e, n=n: e.tensor_tensor(out=pooled[:, :n], in0=tmp[:, :n], in1=pT[:, gi, :n], op=ALU.subtract),
              reads=[tmp_res, pT_res[gi]], writes=[pooled_res])
        dump(g, "pl_pooled", pooled[:], [pooled_res])
        dump(g, "pl_cur", cur, [cur_res])
        y, y_res = yo[gi % 2], yo_res[gi % 2]
        for tc in range(4):
            bank = 4 + tc % 2
            ps = g.ps[bank]
            kb.op("pe", lambda e, tc=tc, ps=ps: e.matmul(ps[:], pw[:, gi, :], pooled[:, tc * 512:(tc + 1) * 512], start=True, stop=True),
                  reads=[pw_res, pooled_res], writes=[g.ps_res[bank]])
            kb.op("act", lambda e, tc=tc, ps=ps, y=y: e.activation(out=y[:, tc * 512:(tc + 1) * 512], in_=ps[:], func=AF.Identity,
                                                                   scale=psc[:, gi:gi + 1]),
                  reads=[g.ps_res[bank], pw_res], writes=[y_res])
        ct = 12 + gi
        kb.dma("sp", g.ybs[b, ct * 128:(ct + 1) * 128, :], y[:], reads=[y_res], writes=[g.ybs_res[b][ct]])


def branch_sb(g, es, l, b):
    nc, kb, I = g.nc, g.kb, g.I
    def sb(name, shape, dt):
        return es.enter_context(nc.sbuf_tensor(uniq(name), list(shape), dt))
    scale = 1.0 / float(np.sqrt(128.0))
    qT = sb("sb_q", [128, T], BF16)
    kT = sb("sb_k", [128, T], BF16)
    v = sb("sb_v", [128, 16, 128], BF16)
    in_res = mkres(3)
    C = sb("sb_C", [128, T], F32)
    C_res = Res()
    E = [sb(f"sb_E{i}", [128, 512], F32) for i in range(2)]
    L = [sb(f"sb_L{i}", [128, 512], F32) for i in range(2)]
    A = [sb(f"sb_A{i}", [128, 512], F32) for i in range(2)]
    W = [sb(f"sb_W{i}", [128, 512], BF16) for i in range(2)]
    E_res, L_res, A_res, W_res = mkres(2), mkres(2), mkres(2), mkres(2)
    yo = sb("sb_yo", [128, T], BF16)
    yo_res = Res()
    for h in range(4):
        kb.dma("sp", qT[:], g.pj[b, (20 + h) * 128:(21 + h) * 128, :], reads=[g.pj_res[b][20 + h]], writes=[in_res[0]])
        kb.dma("sp", kT[:], g.pj[b, (24 + h) * 128:(25 + h) * 128, :], reads=[g.pj_res[b][24 + h]], writes=[in_res[1]])
        kb.dma("sp", v[:], g.pv[b, 1, :, h * 128:(h + 1) * 128].rearrange("(tt s) e -> s tt e", s=128),
               reads=g.pv_res[b][1], writes=[in_res[2]])
        kb.op("pool", lambda e: e.memset(C[:], 0.0), writes=[C_res])
        for c in range(4):
            kb.op("pe", lambda e, c=c: e.matmul(g.ps[c][:], g.zeros_bf[:], qT[:, 0:512], start=True, stop=True),
                  reads=[g.cres, in_res[0]], writes=[g.ps_res[c]])
        n = 0
        for kbk in range(15, -1, -1):
            for c in range(kbk // 4, 4):
                diag = (c == kbk // 4)
                col0 = (kbk % 4) * 128 if diag else 0
                w = 512 - col0
                t0 = c * 512 + col0
                i = n % 2
                n += 1
                zb = 4 + i
                psz, pst, pso = g.ps[zb], g.ps[6], g.ps[7]
                kb.op("pe", lambda e: e.matmul(psz[:, :w], kT[:, kbk * 128:(kbk + 1) * 128], qT[:, t0:t0 + w], start=True, stop=True),
                      reads=[in_res[0], in_res[1]], writes=[g.ps_res[zb]])
                kb.op("act", lambda e: e.activation(out=E[i][:, :w], in_=psz[:, :w], func=AF.Exp, scale=scale),
                      reads=[g.ps_res[zb]], writes=[E_res[i]])
                kb.op("act", lambda e: e.activation(out=L[i][:, :w], in_=E[i][:, :w], func=AF.Ln, bias=g.onesT[:, 0:1]),
                      reads=[E_res[i], g.cres], writes=[L_res[i]])
                kb.op("dve", lambda e: e.scalar_tensor_tensor(out=A[i][:, :w], in0=psz[:, :w], scalar=scale, in1=L[i][:, :w],
                                                              op0=ALU.mult, op1=ALU.subtract),
                      reads=[g.ps_res[zb], L_res[i]], writes=[A_res[i]])
                if diag:
                    kb.op("dve", lambda e: e.tensor_tensor(out=L[i][:, 0:128], in0=L[i][:, 0:128], in1=g.mtri[:], op=ALU.mult),
                          reads=[L_res[i], g.cres, A_res[i]], writes=[L_res[i]])
                kb.op("pe", lambda e: e.matmul(pst[:, :w], g.triu[:], L[i][:, :w], start=True, stop=True),
                      reads=[g.cres, L_res[i]], writes=[g.ps_res[6]])
                kb.op("pe", lambda e: e.matmul(pso[:, :w], g.ones128[:], L[i][:, :w], start=True, stop=True),
                      reads=[g.cres, L_res[i]], writes=[g.ps_res[7]])
                kb.op("dve", lambda e: e.tensor_tensor(out=A[i][:, :w], in0=A[i][:, :w], in1=pst[:, :w], op=ALU.subtract),
                      reads=[A_res[i], g.ps_res[6]], writes=[A_res[i]])
                kb.op("dve", lambda e: e.tensor_tensor(out=A[i][:, :w], in0=A[i][:, :w], in1=C[:, t0:t0 + w], op=ALU.subtract),
                      reads=[A_res[i], C_res], writes=[A_res[i]])
                kb.op("act", lambda e: e.activation(out=W[i][:, :w], in_=A[i][:, :w], func=AF.Exp),
                      reads=[A_res[i]], writes=[W_res[i]])
                if diag:
                    kb.op("dve", lambda e: e.tensor_tensor(out=W[i][:, 0:128], in0=W[i][:, 0:128], in1=g.mtri_bf[:], op=ALU.mult),
                          reads=[W_res[i], g.cres], writes=[W_res[i]])
                kb.op("dve", lambda e: e.tensor_tensor(out=C[:, t0:t0 + w], in0=C[:, t0:t0 + w], in1=pso[:, :w], op=ALU.add),
                      reads=[C_res, g.ps_res[7]], writes=[C_res])
                kb.op("pe", lambda e: e.matmul(g.ps[c][:, col0:512], v[:, kbk, :], W[i][:, :w], start=False, stop=True),
                      reads=[in_res[2], W_res[i]], writes=[g.ps_res[c]])
        for c in range(4):
            kb.op("act", lambda e, c=c: e.activation(out=yo[:, c * 512:(c + 1) * 512], in_=g.ps[c][:], func=AF.Identity),
                  reads=[g.ps_res[c]], writes=[yo_res])
        ct = 8 + h
        kb.dma("sp", g.ybs[b, ct * 128:(ct + 1) * 128, :], yo[:], reads=[yo_res], writes=[g.ybs_res[b][ct]])


def branch_hg(g, es, l, b):
    nc, kb, I = g.nc, g.kb, g.I
    def sb(name, shape, dt):
        return es.enter_context(nc.sbuf_tensor(uniq(name), list(shape), dt))
    CH, NCH = 32, T // 32
    MID, LAST, CPB = CH // 2 - 1, CH - 1, 512 // CH
    prm = sb("hg_prm", [128, 4, 4], F32)
    prm_res = Res()
    lbl = sb("hg_lbl", [128, DEPTH, 4], F32)
    kb.dma("sp", prm[:, 0, :], I["hg_gain"][:, l, :], writes=[prm_res])
    kb.dma("sp", lbl[:], I["hg_lbl"], writes=[prm_res])
    if l == 0:
        kb.op("dve", lambda e: e.memset(prm[:, 1, :], 0.0), writes=[prm_res])
    else:
        kb.op("dve", lambda e: e.tensor_tensor(out=prm[:, 3, :], in0=lbl[:, 1, :], in1=lbl[:, 0, :], op=ALU.subtract),
              reads=[prm_res], writes=[prm_res])
        kb.op("act", lambda e: e.activation(out=prm[:, 1, :], in_=prm[:, 3, :], func=AF.Sigmoid), reads=[prm_res], writes=[prm_res])
    kb.op("dve", lambda e: e.tensor_scalar(out=prm[:, 2, :], in0=prm[:, 1, :], scalar1=-1.0, scalar2=1.0, op0=ALU.mult, op1=ALU.add),
          reads=[prm_res], writes=[prm_res])
    qr = sb("hg_qr", [128, T], BF16)
    fr = sb("hg_fr", [128, T], F32)
    gr = sb("hg_gr", [128, T], BF16)
    v = sb("hg_v", [CH, NCH, 128], BF16)
    in_res = mkres(4)
    fg = sb("hg_fg", [128, T], F32)
    kk = sb("hg_kk", [128, T], F32)
    bb = sb("hg_bb", [128, T], F32)
    d1 = sb("hg_d1", [128, T], F32)
    e1 = sb("hg_e1", [128, T], F32)
    e2 = sb("hg_e2", [128, T], F32)
    qs = sb("hg_qs", [128, T], F32)
    qe = sb("hg_qe", [128, T], BF16)
    ke = sb("hg_ke", [128, T], BF16)
    qb = sb("hg_qb", [128, T], BF16)
    kd = sb("hg_kd", [128, T], BF16)
    sm = sb("hg_sm", [128, 4, NCH], F32)
    r_fg, r_kk, r_bb, r_d1, r_e1, r_e2, r_qs, r_qe, r_ke, r_qb, r_kd, r_sm = mkres(12)
    PT = [sb(f"hg_PT{i}", [CH, CH], BF16) for i in range(2)]
    PT_res = mkres(2)
    kdT = [sb(f"hg_kdT{i}", [CH, 128], BF16) for i in range(2)]
    kdT_res = mkres(2)
    S = sb("hg_S", [128, 128], F32)
    Sb = sb("hg_Sb", [128, 128], BF16)
    S_res, Sb_res = Res(), Res()
    sq = sb("hg_sq", [128, 512], BF16)
    rt = sb("hg_rt", [128, 512], F32)
    on = sb("hg_on", [128, 512], F32)
    sgl = sb("hg_sgl", [128, 512], F32)
    r_sq, r_rt, r_on, r_sgl = mkres(4)
    yo = sb("hg_yo", [128, T], BF16)
    yo_res = Res()
    def v3(t):
        return t[:].rearrange("p (c s) -> p c s", s=CH)
    for h in range(4):
        kb.dma("sp", qr[:], g.pj[b, (4 + h) * 128:(5 + h) * 128, :], reads=[g.pj_res[b][4 + h]], writes=[in_res[0]])
        kb.dma("sp", fr[:], g.pjf[b, h * 128:(h + 1) * 128, :], reads=[g.pjf_res[b][h]], writes=[in_res[1]])
        kb.dma("sp", gr[:], g.pj[b, (16 + h) * 128:(17 + h) * 128, :], reads=[g.pj_res[b][16 + h]], writes=[in_res[2]])
        kb.dma("sp", v[:], g.pv[b, 0, :, h * 128:(h + 1) * 128].rearrange("(c s) e -> s c e", s=CH),
               reads=g.pv_res[b][0], writes=[in_res[3]])
        kb.op("act", lambda e: e.activation(out=fg[:], in_=fr[:], func=AF.Sigmoid), reads=[in_res[1]], writes=[r_fg])
        kb.op("dve", lambda e: e.tensor_scalar(out=fg[:], in0=fg[:], scalar1=prm[:, 2, h:h + 1], scalar2=prm[:, 1, h:h + 1],
                                               op0=ALU.mult, op1=ALU.add), reads=[r_fg, prm_res], writes=[r_fg])
        kb.op("dve", lambda e: e.tensor_scalar(out=kk[:], in0=fg[:], scalar1=-1.0, scalar2=1.0, op0=ALU.mult, op1=ALU.add),
              reads=[r_fg], writes=[r_kk])
        kb.op("dve", lambda e: e.tensor_scalar(out=fg[:], in0=fg[:], scalar1=1e-6, scalar2=None, op0=ALU.max),
              reads=[r_fg, r_kk], writes=[r_fg])
        kb.op("act", lambda e: e.activation(out=fg[:], in_=fg[:], func=AF.Ln), reads=[r_fg], writes=[r_fg])
        for c in range(NCH):
            kb.op("dve", lambda e, c=c: e.tensor_tensor_scan(out=bb[:, c * CH:(c + 1) * CH], data0=g.ones128[:, 0:CH],
                                                            data1=fg[:, c * CH:(c + 1) * CH], initial=0.0, op0=ALU.mult, op1=ALU.add),
                  reads=[r_fg, g.cres], writes=[r_bb])
        bb3 = v3(bb)
        kb.op("dve", lambda e: e.tensor_tensor(out=v3(d1), in0=bb3, in1=bb3[:, :, MID:MID + 1].to_broadcast([128, NCH, CH]), op=ALU.subtract),
              reads=[r_bb], writes=[r_d1])
        kb.op("act", lambda e: e.activation(out=e1[:], in_=d1[:], func=AF.Exp), reads=[r_d1], writes=[r_e1])
        kb.op("act", lambda e: e.activation(out=e2[:], in_=d1[:], func=AF.Exp, scale=-1.0), reads=[r_d1], writes=[r_e2])
        kb.op("act", lambda e: e.activation(out=qs[:], in_=qr[:], func=AF.Silu), reads=[in_res[0]], writes=[r_qs])
        kb.op("dve", lambda e: e.tensor_tensor(out=qe[:], in0=qs[:], in1=e1[:], op=ALU.mult), reads=[r_qs, r_e1], writes=[r_qe])
        kb.op("dve", lambda e: e.tensor_tensor(out=ke[:], in0=kk[:], in1=e2[:], op=ALU.mult), reads=[r_kk, r_e2], writes=[r_ke])
        kb.op("act", lambda e: e.activation(out=sm[:, 0, :].unsqueeze(2), in_=bb3[:, :, MID:MID + 1], func=AF.Exp), reads=[r_bb], writes=[r_sm])
        kb.op("dve", lambda e: e.tensor_tensor(out=sm[:, 1, :].unsqueeze(2), in0=bb3[:, :, LAST:LAST + 1], in1=bb3[:, :, MID:MID + 1], op=ALU.subtract),
              reads=[r_bb, r_sm], writes=[r_sm])
        kb.op("act", lambda e: e.activation(out=sm[:, 1, :], in_=sm[:, 1, :], func=AF.Exp), reads=[r_sm], writes=[r_sm])
        kb.op("act", lambda e: e.activation(out=sm[:, 2, :].unsqueeze(2), in_=bb3[:, :, LAST:LAST + 1], func=AF.Exp), reads=[r_bb, r_sm], writes=[r_sm])
        kb.op("dve", lambda e: e.tensor_tensor(out=v3(qb), in0=v3(qe), in1=sm[:, 0, :].unsqueeze(2).to_broadcast([128, NCH, CH]), op=ALU.mult),
              reads=[r_qe, r_sm], writes=[r_qb])
        kb.op("dve", lambda e: e.tensor_tensor(out=v3(kd), in0=v3(ke), in1=sm[:, 1, :].unsqueeze(2).to_broadcast([128, NCH, CH]), op=ALU.mult),
              reads=[r_ke, r_sm], writes=[r_kd])
        dump(g, "hg_logf", fg[:], [r_fg]); dump(g, "hg_bb", bb[:], [r_bb]); dump(g, "hg_qe", qe[:], [r_qe]); dump(g, "hg_ke", ke[:], [r_ke])
        dump(g, "hg_qb", qb[:], [r_qb]); dump(g, "hg_kd", kd[:], [r_kd]); dump(g, "hg_sm", sm[:], [r_sm]); dump(g, "hg_v", v[:], [in_res[3]])
        for c in range(NCH):
            i = c % 2
            ob = (c // CPB) % 2
            col = (c % CPB) * CH
            pso = g.ps[ob]
            kb.op("pe", lambda e: e.matmul(g.ps[2][0:CH, 0:CH], ke[:, c * CH:(c + 1) * CH], qe[:, c * CH:(c + 1) * CH], start=True, stop=True),
                  reads=[r_ke, r_qe], writes=[g.ps_res[2]])
            kb.op("dve", lambda e: e.scalar_tensor_tensor(out=PT[i][:], in0=g.ps[2][0:CH, 0:CH], scalar=1e30, in1=g.mle[0:CH, 0:CH],
                                                          op0=ALU.min, op1=ALU.mult),
                  reads=[g.ps_res[2], g.cres], writes=[PT_res[i]])
            kb.op("pe", lambda e: e.matmul(pso[:, col:col + CH], v[:, c, :], PT[i][:], start=True, stop=(c == 0)),
                  reads=[in_res[3], PT_res[i]], writes=[g.ps_res[ob]])
            if c > 0:
                kb.op("pe", lambda e: e.matmul(pso[:, col:col + CH], Sb[:], qb[:, c * CH:(c + 1) * CH], start=False, stop=True),
                      reads=[Sb_res, r_qb], writes=[g.ps_res[ob]])
            if c < NCH - 1:
                pst = g.ps[3][:].bitcast(BF16)
                kb.op("pe", lambda e: e.transpose(pst[0:CH, 0:128], kd[:, c * CH:(c + 1) * CH], g.ident[:]),
                      reads=[r_kd, g.cres], writes=[g.ps_res[3]])
                kb.op("act", lambda e: e.activation(out=kdT[i][:], in_=pst[0:CH, 0:128], func=AF.Identity),
                      reads=[g.ps_res[3]], writes=[kdT_res[i]])
                kb.op("pe", lambda e: e.matmul(g.ps[4][:, 0:128], kdT[i][:], v[:, c, :], start=True, stop=True),
                      reads=[kdT_res[i], in_res[3]], writes=[g.ps_res[4]])
                if c == 0:
                    kb.op("dve", lambda e: e.tensor_copy(out=S[:], in_=g.ps[4][:, 0:128]), reads=[g.ps_res[4]], writes=[S_res])
                else:
                    kb.op("dve", lambda e: e.scalar_tensor_tensor(out=S[:], in0=S[:], scalar=sm[:, 2, c:c + 1], in1=g.ps[4][:, 0:128],
                                                                  op0=ALU.mult, op1=ALU.add),
                          reads=[S_res, r_sm, g.ps_res[4]], writes=[S_res])
                kb.op("act", lambda e: e.activation(out=Sb[:], in_=S[:], func=AF.Identity), reads=[S_res], writes=[Sb_res])
            if c % CPB == CPB - 1:
                blk = c // CPB
                cs = slice(blk * 512, (blk + 1) * 512)
                kb.op("act", lambda e: e.activation(out=sq[:], in_=pso[:], func=AF.Square), reads=[g.ps_res[ob]], writes=[r_sq])
                kb.op("pe", lambda e: e.matmul(g.ps[5][:], g.ones_bf[:], sq[:], start=True, stop=True),
                      reads=[g.cres, r_sq], writes=[g.ps_res[5]])
                kb.op("act", lambda e: e.activation(out=rt[:], in_=g.ps[5][:], func=AF.Sqrt, scale=1.0 / 128.0, bias=g.epsT[:]),
                      reads=[g.ps_res[5], g.cres], writes=[r_rt])
                kb.op("dve", lambda e: e.reciprocal(out=rt[:], in_=rt[:]), reads=[r_rt], writes=[r_rt])
                kb.op("dve", lambda e: e.tensor_tensor(out=on[:], in0=pso[:], in1=rt[:], op=ALU.mult),
                      reads=[g.ps_res[ob], r_rt], writes=[r_on])
                kb.op("act", lambda e: e.activation(out=sgl[:], in_=gr[:, cs], func=AF.Silu), reads=[in_res[2]], writes=[r_sgl])
                kb.op("dve", lambda e: e.scalar_tensor_tensor(out=yo[:, cs], in0=on[:], scalar=prm[:, 0, h:h + 1], in1=sgl[:],
                                                              op0=ALU.mult, op1=ALU.mult),
                      reads=[r_on, r_sgl, prm_res], writes=[yo_res])
        dump(g, "hg_S", S[:], [S_res]); dump(g, "hg_on", on[:], [r_on]); dump(g, "hg_rt", rt[:], [r_rt])
        ct = 4 + h
        kb.dma("sp", g.ybs[b, ct * 128:(ct + 1) * 128, :], yo[:], reads=[yo_res], writes=[g.ybs_res[b][ct]])


def s5_discretize(g, sb, lre, lim, lst, n, res, tag):
    kb = g.kb
    TWO_PI = float(2.0 * np.pi)
    t = {k: sb(f"s5{tag}_{k}", [128, n], F32) for k in ("lr", "step", "mag", "ang", "kf", "sh", "sq", "ch", "are", "aim")}
    ki = sb(f"s5{tag}_ki", [128, n], mybir.dt.int32)
    R = [res]
    def dve(fn):
        kb.op("dve", fn, reads=R, writes=R)
    def act(fn):
        kb.op("act", fn, reads=R, writes=R)
    dve(lambda e: e.tensor_scalar(out=t["lr"][:], in0=lre[:], scalar1=-1e-4, scalar2=None, op0=ALU.min))
    act(lambda e: e.activation(out=t["step"][:], in_=lst[:], func=AF.Exp))
    dve(lambda e: e.tensor_tensor(out=t["mag"][:], in0=t["lr"][:], in1=t["step"][:], op=ALU.mult))
    act(lambda e: e.activation(out=t["mag"][:], in_=t["mag"][:], func=AF.Exp))
    dve(lambda e: e.tensor_tensor(out=t["ang"][:], in0=lim[:], in1=t["step"][:], op=ALU.mult))
    dve(lambda e: e.tensor_scalar(out=t["kf"][:], in0=t["ang"][:], scalar1=1.0 / TWO_PI, scalar2=None, op0=ALU.mult))
    dve(lambda e: e.tensor_copy(out=ki[:], in_=t["kf"][:]))
    dve(lambda e: e.tensor_copy(out=t["kf"][:], in_=ki[:]))
    dve(lambda e: e.scalar_tensor_tensor(out=t["ang"][:], in0=t["kf"][:], scalar=-TWO_PI, in1=t["ang"][:], op0=ALU.mult, op1=ALU.add))
    act(lambda e: e.activation(out=t["sh"][:], in_=t["ang"][:], func=AF.Sin, scale=0.5))
    act(lambda e: e.activation(out=t["sq"][:], in_=t["ang"][:], func=AF.Sin, scale=0.25))
    dve(lambda e: e.tensor_tensor(out=t["ch"][:], in0=t["sq"][:], in1=t["sq"][:], op=ALU.mult))
    dve(lambda e: e.tensor_scalar(out=t["ch"][:], in0=t["ch"][:], scalar1=-2.0, scalar2=1.0, op0=ALU.mult, op1=ALU.add))
    dve(lambda e: e.tensor_tensor(out=t["aim"][:], in0=t["sh"][:], in1=t["ch"][:], op=ALU.mult))
    dve(lambda e: e.scalar_tensor_tensor(out=t["aim"][:], in0=t["aim"][:], scalar=2.0, in1=t["mag"][:], op0=ALU.mult, op1=ALU.mult))
    dve(lambda e: e.tensor_tensor(out=t["are"][:], in0=t["sh"][:], in1=t["sh"][:], op=ALU.mult))
    dve(lambda e: e.tensor_scalar(out=t["are"][:], in0=t["are"][:], scalar1=-2.0, scalar2=1.0, op0=ALU.mult, op1=ALU.add))
    dve(lambda e: e.tensor_tensor(out=t["are"][:], in0=t["are"][:], in1=t["mag"][:], op=ALU.mult))
    return t


def branch_s5(g, es, l, b):
    nc, kb, I = g.nc, g.kb, g.I
    def sb(name, shape, dt):
        return es.enter_context(nc.sbuf_tensor(uniq(name), list(shape), dt))
    NK = 11
    pres = Res()
    R = [pres, g.cres]
    pw = sb("s5pw", [128, 3, NK, 16], F32)
    bbr = sb("s5bbr", [128, 4, 128], F32)
    bbi = sb("s5bbi", [128, 4, 128], F32)
    bbpr = sb("s5bbpr", [128, 16, 128], BF16)
    bbpi = sb("s5bbpi", [128, 16, 128], BF16)
    with scope(g) as esp:
        def sbp(name, shape, dt):
            return esp.enter_context(nc.sbuf_tensor(uniq(name), list(shape), dt))
        raw_s = [sbp(f"s5rs{i}", [128, 16], F32) for i in range(3)]
        raw_b = [sbp(f"s5rb{i}", [128, 512], F32) for i in range(3)]
        bTr = sbp("s5bTr", [128, 512], F32)
        bTi = sbp("s5bTi", [128, 512], F32)
        for i, nm in enumerate(("s5_lre_s", "s5_lim_s", "s5_lst_s")):
            kb.dma("sp", raw_s[i][:], I[nm][:, l, :], writes=R)
        for i, nm in enumerate(("s5_lre_b", "s5_lim_b", "s5_lst_b")):
            kb.dma("sp", raw_b[i][:], I[nm][l], writes=R)
        kb.dma("sp", bTr[:], I["s5_bT_re"][l], writes=R)
        kb.dma("sp", bTi[:], I["s5_bT_im"][l], writes=R)
        ds = s5_discretize(g, sbp, raw_s[0], raw_s[1], raw_s[2], 16, pres, "s")
        db = s5_discretize(g, sbp, raw_b[0], raw_b[1], raw_b[2], 512, pres, "b")
        def dve(fn):
            kb.op("dve", fn, reads=R, writes=R)
        dve(lambda e: e.tensor_copy(out=pw[:, 0, 0, :], in_=ds["are"][:]))
        dve(lambda e: e.tensor_copy(out=pw[:, 1, 0, :], in_=ds["aim"][:]))
        tmp16 = sbp("s5tmp16", [128, 16], F32)
        for k in range(1, NK):
            dve(lambda e, k=k: e.tensor_tensor(out=tmp16[:], in0=pw[:, 1, k - 1, :], in1=pw[:, 1, k - 1, :], op=ALU.mult))
            dve(lambda e, k=k: e.tensor_tensor(out=pw[:, 0, k, :], in0=pw[:, 0, k - 1, :], in1=pw[:, 0, k - 1, :], op=ALU.mult))
            dve(lambda e, k=k: e.tensor_tensor(out=pw[:, 0, k, :], in0=pw[:, 0, k, :], in1=tmp16[:], op=ALU.subtract))
            dve(lambda e, k=k: e.scalar_tensor_tensor(out=pw[:, 1, k, :], in0=pw[:, 0, k - 1, :], scalar=2.0, in1=pw[:, 1, k - 1, :],
                                                      op0=ALU.mult, op1=ALU.mult))
        dve(lambda e: e.tensor_scalar(out=pw[:, 2, :, :], in0=pw[:, 1, :, :], scalar1=-1.0, scalar2=None, op0=ALU.mult))
        den = sbp("s5den", [128, 512], F32)
        nr = sbp("s5nr", [128, 512], F32)
        fr = sbp("s5fr", [128, 512], F32)
        fi = sbp("s5fi", [128, 512], F32)
        t1 = sbp("s5t1", [128, 512], F32)
        lr, li, are, aim = db["lr"], raw_b[1], db["are"], db["aim"]
        dve(lambda e: e.tensor_tensor(out=den[:], in0=lr[:], in1=lr[:], op=ALU.mult))
        dve(lambda e: e.tensor_tensor(out=t1[:], in0=li[:], in1=li[:], op=ALU.mult))
        dve(lambda e: e.tensor_tensor(out=den[:], in0=den[:], in1=t1[:], op=ALU.add))
        dve(lambda e: e.reciprocal(out=den[:], in_=den[:]))
        dve(lambda e: e.tensor_scalar(out=nr[:], in0=are[:], scalar1=-1.0, scalar2=None, op0=ALU.add))
        dve(lambda e: e.tensor_tensor(out=fr[:], in0=nr[:], in1=lr[:], op=ALU.mult))
        dve(lambda e: e.tensor_tensor(out=t1[:], in0=aim[:], in1=li[:], op=ALU.mult))
        dve(lambda e: e.tensor_tensor(out=fr[:], in0=fr[:], in1=t1[:], op=ALU.add))
        dve(lambda e: e.tensor_tensor(out=fr[:], in0=fr[:], in1=den[:], op=ALU.mult))
        dve(lambda e: e.tensor_tensor(out=fi[:], in0=aim[:], in1=lr[:], op=ALU.mult))
        dve(lambda e: e.tensor_tensor(out=t1[:], in0=nr[:], in1=li[:], op=ALU.mult))
        dve(lambda e: e.tensor_tensor(out=fi[:], in0=fi[:], in1=t1[:], op=ALU.subtract))
        dve(lambda e: e.tensor_tensor(out=fi[:], in0=fi[:], in1=den[:], op=ALU.mult))
        bbr2 = bbr[:].rearrange("p a b -> p (a b)")
        bbi2 = bbi[:].rearrange("p a b -> p (a b)")
        dve(lambda e: e.tensor_tensor(out=t1[:], in0=fr[:], in1=bTr[:], op=ALU.mult))
        dve(lambda e: e.tensor_tensor(out=nr[:], in0=fi[:], in1=bTi[:], op=ALU.mult))
        dve(lambda e: e.tensor_tensor(out=bbr2, in0=t1[:], in1=nr[:], op=ALU.subtract))
        dve(lambda e: e.tensor_tensor(out=t1[:], in0=fr[:], in1=bTi[:], op=ALU.mult))
        dve(lambda e: e.tensor_tensor(out=nr[:], in0=fi[:], in1=bTr[:], op=ALU.mult))
        dve(lambda e: e.tensor_tensor(out=bbi2, in0=t1[:], in1=nr[:], op=ALU.add))
        for st in range(16):
            dve(lambda e, st=st: e.tensor_scalar(out=bbpr[:, st, :], in0=bbr[:, st // 4, :], scalar1=g.rmask[:, st % 4:st % 4 + 1],
                                                 scalar2=None, op0=ALU.mult))
            dve(lambda e, st=st: e.tensor_scalar(out=bbpi[:, st, :], in0=bbi[:, st // 4, :], scalar1=g.rmask[:, st % 4:st % 4 + 1],
                                                 scalar2=None, op0=ALU.mult))
        dump(g, "s5_pw", pw[:], R)
        dump(g, "s5_bbr", bbr[:], R)
    cTr = sb("s5cTr", [128, 16, 128], BF16)
    cTi = sb("s5cTi", [128, 16, 128], BF16)
    wgl = sb("s5wgl", [128, 4, 512], BF16)
    dsk = sb("s5dsk", [128, 4], F32)
    bgl = sb("s5bgl", [128, 4], F32)
    kb.dma("pool", cTr[:], I["s5_cT_re"][l], writes=R)
    kb.dma("pool", cTi[:], I["s5_cT_im"][l], writes=R)
    kb.dma("pool", wgl[:], I["s5_wglu"][l].rearrange("(k p) c -> p k c", p=128), writes=R)
    kb.dma("sp", dsk[:], I["s5_d"][:, l, :], writes=R)
    kb.dma("sp", bgl[:], I["s5_bglu"][:, l, :], writes=R)
    kb.op("act", lambda e: e.activation(out=cTi[:], in_=cTi[:], func=AF.Identity, scale=-1.0), reads=R, writes=R)
    uT = sb("s5uT", [128, 4, T], BF16)
    uT_res = Res()
    kb.dma("sp", uT[:], g.pj[b, 0:512, :].rearrange("(k p) t -> p k t", p=128), reads=g.pj_res[b][0:4], writes=[uT_res])
    X = [[sb(f"s5X{i}{c}", [128, T], F32) for c in range(2)] for i in range(2)]
    X_res = [mkres(2) for _ in range(2)]
    xsb = [[sb(f"s5xb{i}{c}", [128, T], BF16) for c in range(2)] for i in range(4)]
    xsb_res = [mkres(2) for _ in range(4)]
    yg = sb("s5yg", [128, 4, T], BF16)
    yg_res = mkres(4)
    ytmp = [sb(f"s5yt{i}", [128, 512], F32) for i in range(2)]
    ytmp_res = mkres(2)
    for st in range(16):
        ut, po = st // 4, 32 * (st % 4)
        for tc in range(4):
            cs = slice(tc * 512, (tc + 1) * 512)
            for c, bbt in enumerate((bbpr, bbpi)):
                bank = 2 * c + tc % 2
                ps = g.ps[bank]
                kb.op("pe", lambda e: e.matmul(ps[:], bbt[:, st, :], uT[:, ut, cs], start=True, stop=True),
                      reads=[pres, uT_res], writes=[g.ps_res[bank]])
                kb.op("act", lambda e: e.activation(out=X[0][c][:, cs], in_=ps[:], func=AF.Identity),
                      reads=[g.ps_res[bank]], writes=[X_res[0][c]])
        cur = 0
        for k in range(NK):
            d = 2 ** k
            s0, s1 = X[cur], X[1 - cur]
            r0, r1 = X_res[cur], X_res[1 - cur]
            ar, ai, nai = pw[:, 0, k, st:st + 1], pw[:, 1, k, st:st + 1], pw[:, 2, k, st:st + 1]
            kb.op("dve", lambda e: e.scalar_tensor_tensor(out=s1[0][:, d:], in0=s0[0][:, :T - d], scalar=ar, in1=s0[0][:, d:],
                                                          op0=ALU.mult, op1=ALU.add), reads=[r0[0], pres], writes=[r1[0]])
            kb.op("dve", lambda e: e.scalar_tensor_tensor(out=s1[0][:, d:], in0=s0[1][:, :T - d], scalar=nai, in1=s1[0][:, d:],
                                                          op0=ALU.mult, op1=ALU.add), reads=[r0[1], pres, r1[0]], writes=[r1[0]])
            kb.op("dve", lambda e: e.scalar_tensor_tensor(out=s1[1][:, d:], in0=s0[1][:, :T - d], scalar=ar, in1=s0[1][:, d:],
                                                          op0=ALU.mult, op1=ALU.add), reads=[r0[1], pres], writes=[r1[1]])
            kb.op("dve", lambda e: e.scalar_tensor_tensor(out=s1[1][:, d:], in0=s0[0][:, :T - d], scalar=ai, in1=s1[1][:, d:],
                                                          op0=ALU.mult, op1=ALU.add), reads=[r0[0], pres, r1[1]], writes=[r1[1]])
            for c in range(2):
                kb.op("act", lambda e, c=c: e.activation(out=s1[c][:, :d], in_=s0[c][:, :d], func=AF.Identity),
                      reads=[r0[c]], writes=[r1[c]])
            cur = 1 - cur
        for c in range(2):
            kb.op("act", lambda e, c=c: e.activation(out=xsb[st % 4][c][:], in_=X[cur][c][:], func=AF.Identity),
                  reads=[X_res[cur][c]], writes=[xsb_res[st % 4][c]])
        if st == 0:
            dump(g, "s5_xs0", X[cur][0][:], [X_res[cur][0]])
        if st % 4 == 3:
            ct = st // 4
            for tc in range(4):
                cs = slice(tc * 512, (tc + 1) * 512)
                bank = 4 + tc % 2
                ps = g.ps[bank]
                n = 0
                for j in range(4):
                    for c, cT in enumerate((cTr, cTi)):
                        kb.op("pe", lambda e: e.matmul(ps[:], cT[:, 4 * ct + j, :], xsb[j][c][:, cs], start=(n == 0), stop=(n == 7)),
                              reads=[pres, xsb_res[j][c]], writes=[g.ps_res[bank]])
                        n += 1
                yt, yt_res = ytmp[tc % 2], ytmp_res[tc % 2]
                kb.op("dve", lambda e: e.scalar_tensor_tensor(out=yt[:], in0=uT[:, ct, cs], scalar=dsk[:, ct:ct + 1], in1=ps[:],
                                                              op0=ALU.mult, op1=ALU.add),
                      reads=[uT_res, pres, g.ps_res[bank]], writes=[yt_res])
                kb.op("act", lambda e: e.activation(out=yg[:, ct, cs], in_=yt[:], func=AF.Gelu), reads=[yt_res], writes=[yg_res[ct]])
    sg = [sb(f"s5sg{i}", [128, 512], BF16) for i in range(2)]
    sg_res = mkres(2)
    yo = [sb(f"s5yo{i}", [128, T], BF16) for i in range(2)]
    yo_res = mkres(2)
    n = 0
    for co in range(4):
        for tc in range(4):
            cs = slice(tc * 512, (tc + 1) * 512)
            bank = 6 + n % 2
            i = n % 2
            n += 1
            ps = g.ps[bank]
            for ci in range(4):
                kb.op("pe", lambda e: e.matmul(ps[:], wgl[:, ci, co * 128:(co + 1) * 128], yg[:, ci, cs], start=(ci == 0), stop=(ci == 3)),
                      reads=[pres, yg_res[ci]], writes=[g.ps_res[bank]])
            kb.op("act", lambda e: e.activation(out=sg[i][:], in_=ps[:], func=AF.Sigmoid, bias=bgl[:, co:co + 1]),
                  reads=[g.ps_res[bank], pres], writes=[sg_res[i]])
            kb.op("dve", lambda e: e.tensor_tensor(out=yo[co % 2][:, cs], in0=yg[:, co, cs], in1=sg[i][:], op=ALU.mult),
                  reads=[yg_res[co], sg_res[i]], writes=[yo_res[co % 2]])
        kb.dma("sp", g.ybs[b, co * 128:(co + 1) * 128, :], yo[co % 2][:], reads=[yo_res[co % 2]], writes=[g.ybs_res[b][co]])


def prep_inputs(inputs, core):
    f = lambda a: np.ascontiguousarray(np.asarray(a, dtype=np.float32))
    b0 = core * NB
    m = {}
    m["x"] = f(inputs["x"][b0:b0 + NB])
    m["cT"] = f(np.transpose(_pk(inputs["c"][b0:b0 + NB]), (1, 2, 0)))
    m["gmix"] = f(np.transpose(_pk(inputs["norm_mix_gain"]), (1, 0, 2)))
    m["gffn"] = f(np.transpose(_pk(inputs["norm_ffn_gain"]), (1, 0, 2)))
    m["gfin"] = f(_pk(inputs["final_gain"]))
    m["pool_scale"] = f(np.transpose(_pk(inputs["pool_scale"]), (1, 0, 2)))
    m["hg_gain"] = f(np.transpose(_pk(inputs["hg_norm_gain"]), (1, 0, 2)))
    m["hg_lbl"] = f(np.transpose(_pk(inputs["hg_lb_logits"]), (1, 0, 2)))
    return m


def s5_host_layouts(inputs):
    f = lambda a: np.ascontiguousarray(np.asarray(a, dtype=np.float32))
    L = DEPTH
    m = {}
    lre, lim, lst = f(inputs["s5_lambda_re"]), f(inputs["s5_lambda_im"]), f(inputs["s5_log_step"])
    lstf = np.broadcast_to(lst[:, :, None], (L, 32, 64))
    def s_lay(a):
        return f(a.reshape(L, 16, 2, 64).transpose(2, 3, 0, 1).reshape(128, L, 16))
    def b_lay(a):
        t = a.reshape(L, 4, 4, 2, 64).transpose(0, 2, 1, 3, 4)
        t = np.broadcast_to(t[:, :, None], (L, 4, 32, 4, 2, 64))
        return f(t.reshape(L, 128, 512))
    m["s5_lre_s"], m["s5_lim_s"], m["s5_lst_s"] = s_lay(lre), s_lay(lim), s_lay(lstf)
    m["s5_lre_b"], m["s5_lim_b"], m["s5_lst_b"] = b_lay(lre), b_lay(lim), b_lay(lstf)
    for nm, key in (("s5_bT_re", "s5_b_re"), ("s5_bT_im", "s5_b_im")):
        bb = f(inputs[key]).reshape(L, 4, 4, 2, 64, 16)
        t = bb.transpose(0, 2, 3, 5, 1, 4)
        arr = np.zeros((L, 4, 2, 16, 4, 2, 64), np.float32)
        for gp in range(2):
            arr[:, :, gp, :, :, gp, :] = t[:, :, gp]
        m[nm] = f(arr.reshape(L, 128, 512))
    for nm, key in (("s5_cT_re", "s5_c_re"), ("s5_cT_im", "s5_c_im")):
        cc = f(inputs[key]).reshape(L, 16, 2, 16, 64)
        arr = np.zeros((L, 2, 64, 16, 4, 2, 16), np.float32)
        for st in range(16):
            for gp in range(2):
                arr[:, gp, :, st, st % 4, gp, :] = cc[:, st, gp].transpose(0, 2, 1)
        m[nm] = f(arr.reshape(L, 128, 16, 128))
    m["s5_d"] = f(np.transpose(_pk(inputs["s5_d"]), (1, 0, 2)))
    m["s5_bglu"] = f(np.transpose(_pk(inputs["s5_b_glu"]), (1, 0, 2)))
    m["s5_wglu"] = f(inputs["s5_w_glu"])
    return m


def shared_inputs(inputs):
    f = lambda a: np.ascontiguousarray(np.asarray(a, dtype=np.float32))
    m = {}
    m.update(s5_host_layouts(inputs))
    for l in range(DEPTH):
        m[f"ada_w{l}"] = f(inputs["ada_w"][l])
    m["ada_b"] = f(inputs["ada_b"]).reshape(DEPTH, 1, 6 * D)
    m["w_in"] = f(inputs["w_in"])
    m["pool_w"] = f(inputs["pool_w"])
    m["w_gate"] = f(inputs["w_gate"])
    m["w_branch"] = f(inputs["w_branch"])
    m["w_out"] = f(inputs["w_out"])
    m["w_query"] = f(inputs["peer_w_query"])
    m["keysT"] = f(np.transpose(np.asarray(inputs["peer_sub_keys"]), (0, 4, 1, 2, 3)).reshape(DEPTH, 128, 16, 128))
    for l in range(DEPTH):
        m[f"peer_uT{l}"] = f(np.asarray(inputs["peer_u"][l]).T)
        m[f"peer_v{l}"] = f(inputs["peer_v"][l])
    m.update(host_consts())
    return m


def kernel(**inputs):
    nc = build()
    sh = shared_inputs(inputs)
    in_maps = []
    for c in range(8):
        m = dict(sh)
        m.update(prep_inputs(inputs, c))
        in_maps.append(m)
    res = run_bass_kernel_spmd(nc, in_maps, core_ids=list(range(8)))
    return np.concatenate([np.asarray(r["out"]) for r in res.results], axis=0)
```
